# Optimizing a Trainium2 kernel written in Bass

```python
import math
import jax
import jax.numpy as jnp
from jax import lax
import numpy as np

D_MODEL = 4096
BATCH = 2
SEQ = 8192
DEPTH = 2

HEAD_DIM = 128
ROT_DIM = HEAD_DIM // 4
ROPE_THETA = 500000.0
ATTN_SCALE = HEAD_DIM ** -0.5
NEG_INF = -1e30
LN_EPS = 1e-5

NSA_HEADS = D_MODEL // 256
NSA_KV_HEADS = NSA_HEADS // 4
NSA_GROUP = NSA_HEADS // NSA_KV_HEADS
CMP_LEN = 32
CMP_STRIDE = 16
SEL_LEN = 64
SEL_TOPN = 16
WINDOW = 512
NSA_CHUNK = 64

DIFF_HEADS = D_MODEL // 1024
DIFF_VDIM = 2 * HEAD_DIM
DENSE_CHUNK = 128

MOBA_HEADS = D_MODEL // 512
MOBA_BLOCK = 256
MOBA_TOPK = 3
MOBA_CHUNK = 32

N_EXPERTS = 32
TOP_K = 4
D_EXPERT = D_MODEL // 8
SWIGLU_LIMIT = 7.0
SWIGLU_ALPHA = 1.702
MOE_BLOCK = 512

DEEPNORM_ALPHA = (2 * DEPTH) ** 0.25
DEEPNORM_BETA = (8 * DEPTH) ** -0.25

COL_SIZES = ((NSA_HEADS * HEAD_DIM,) + (NSA_KV_HEADS * HEAD_DIM,) * 6 + (3 * NSA_HEADS,)
             + (2 * DIFF_HEADS * HEAD_DIM,) * 2 + (DIFF_HEADS * DIFF_VDIM,)
             + (MOBA_HEADS * HEAD_DIM,) * 3)
IN_COLS = sum(COL_SIZES)
MIX_WIDTH = NSA_HEADS * HEAD_DIM + DIFF_HEADS * DIFF_VDIM + MOBA_HEADS * HEAD_DIM

kernel_name = 'hybrid_nsa_diff_moba_moe_deepnorm'


def layer_norm(x, g, b):
    xf = x.astype(jnp.float32)
    mu = jnp.mean(xf, -1, keepdims=True)
    var = jnp.mean(jnp.square(xf - mu), -1, keepdims=True)
    return ((xf - mu) * lax.rsqrt(var + LN_EPS) * g + b).astype(x.dtype)


def masked_softmax(s, mask):
    s = jnp.where(mask, s, NEG_INF)
    p = jnp.exp(s - jnp.max(s, -1, keepdims=True)) * mask
    return p / jnp.maximum(jnp.sum(p, -1, keepdims=True), 1e-30)


def rope_tables(positions):
    inv_freq = 1.0 / (ROPE_THETA ** (jnp.arange(0, ROT_DIM, 2, dtype=jnp.float32) / ROT_DIM))
    ang = positions.astype(jnp.float32)[..., None] * inv_freq
    return jnp.cos(ang), jnp.sin(ang)


def apply_rope(x, cos, sin):
    half = ROT_DIM // 2
    c = cos[:, None].astype(x.dtype)
    s = sin[:, None].astype(x.dtype)
    x1 = x[..., :half]
    x2 = x[..., half:ROT_DIM]
    return jnp.concatenate([x1 * c - x2 * s, x2 * c + x1 * s, x[..., ROT_DIM:]], -1)


def to_heads(t, n_heads, dim):
    b, s, _ = t.shape
    return t.reshape(b, s, n_heads, dim).transpose(0, 2, 1, 3)


def split_columns(h):
    offsets = np.cumsum(np.array(COL_SIZES))[:-1].tolist()
    return jnp.split(h, offsets, axis=-1)


def nsa_compress(k, pos_emb, w1, w2):
    b, hk, s, d = k.shape
    n_cmp = (s - CMP_LEN) // CMP_STRIDE + 1
    idx = np.arange(n_cmp)[:, None] * CMP_STRIDE + np.arange(CMP_LEN)[None, :]
    blocks = k[:, :, idx] + pos_emb.astype(k.dtype)
    flat = blocks.reshape(b, hk, n_cmp, CMP_LEN * d)
    return jax.nn.gelu(flat @ w1) @ w2


def nsa_attention(q, k_cmp, v_cmp, k_slc, v_slc, k_win, v_win, gate_logits,
                  cmp_pos, cmp_w1, cmp_w2, cos, sin):
    b, s, _ = q.shape
    hk, g, dh = NSA_KV_HEADS, NSA_GROUP, HEAD_DIM
    qg = apply_rope(to_heads(q, NSA_HEADS, dh), cos, sin).reshape(b, hk, g, s, dh)
    gates = jax.nn.sigmoid(gate_logits.astype(jnp.float32)).reshape(b, s, hk, g, 3)
    gates = gates.transpose(0, 2, 3, 1, 4).astype(q.dtype)

    kc = nsa_compress(apply_rope(to_heads(k_cmp, hk, dh), cos, sin), cmp_pos[0], cmp_w1[0], cmp_w2[0])
    vc = nsa_compress(to_heads(v_cmp, hk, dh), cmp_pos[1], cmp_w1[1], cmp_w2[1])
    n_cmp = kc.shape[2]
    cmp_end = np.arange(n_cmp) * CMP_STRIDE + CMP_LEN - 1

    n_sel = s // SEL_LEN
    n_sel_pad = max(n_sel, SEL_TOPN)
    ratio = SEL_LEN // CMP_STRIDE
    cover = np.arange(n_sel)[:, None] * ratio + np.arange(1 - CMP_LEN // CMP_STRIDE, ratio)[None, :]
    cover_ok = (cover >= 0) & (cover < n_cmp)
    cover = np.clip(cover, 0, n_cmp - 1)

    ks_blocks = apply_rope(to_heads(k_slc, hk, dh), cos, sin).reshape(b, hk, n_sel, SEL_LEN, dh)
    vs_blocks = to_heads(v_slc, hk, dh).reshape(b, hk, n_sel, SEL_LEN, dh)
    pad_w = ((0, 0), (0, 0), (WINDOW, 0), (0, 0))
    kw_all = jnp.pad(apply_rope(to_heads(k_win, hk, dh), cos, sin), pad_w)
    vw_all = jnp.pad(to_heads(v_win, hk, dh), pad_w)

    bi = jnp.arange(b)[:, None, None, None]
    hi = jnp.arange(hk)[None, :, None, None]
    blk = jnp.arange(n_sel_pad)
    c = NSA_CHUNK

    def chunk(ci):
        s0 = ci * c
        t = s0 + jnp.arange(c)
        qc = lax.dynamic_slice_in_dim(qg, s0, c, axis=3)
        gc = lax.dynamic_slice_in_dim(gates, s0, c, axis=3)

        sc = jnp.einsum('bkgcd,bknd->bkgcn', qc, kc).astype(jnp.float32) * ATTN_SCALE
        pc = masked_softmax(sc, cmp_end[None, :] <= t[:, None])
        o_cmp = jnp.einsum('bkgcn,bknd->bkgcd', pc.astype(vc.dtype), vc)

        p_grp = jnp.sum(pc, axis=2)
        imp = jnp.sum(jnp.where(cover_ok, p_grp[..., cover], 0.0), -1)
        imp = jnp.pad(imp, ((0, 0), (0, 0), (0, 0), (0, n_sel_pad - n_sel)))
        cur = t // SEL_LEN
        forced = (blk[None] == 0) | (blk[None] == cur[:, None]) | (blk[None] == cur[:, None] - 1)
        imp = jnp.where(forced, jnp.inf, imp)
        imp = jnp.where(blk[None] <= cur[:, None], imp, -jnp.inf)
        _, sel = lax.top_k(imp, SEL_TOPN)
        sel_ok = jnp.arange(SEL_TOPN)[None, :] < jnp.minimum(cur + 1, SEL_TOPN)[:, None]
        sel = jnp.minimum(sel, n_sel - 1)
        ks = ks_blocks[bi, hi, sel]
        vs = vs_blocks[bi, hi, sel]
        kpos = sel[..., None] * SEL_LEN + jnp.arange(SEL_LEN)
        m_sel = sel_ok[:, :, None] & (kpos <= t[:, None, None])
        ss = jnp.einsum('bkgcd,bkcnld->bkgcnl', qc, ks).astype(jnp.float32) * ATTN_SCALE
        ps = masked_softmax(ss.reshape(b, hk, g, c, SEL_TOPN * SEL_LEN),
                            m_sel.reshape(b, hk, 1, c, SEL_TOPN * SEL_LEN))
        o_slc = jnp.einsum('bkgcm,bkcmd->bkgcd', ps.astype(vs.dtype),
                           vs.reshape(b, hk, c, SEL_TOPN * SEL_LEN, dh))

        kw = lax.dynamic_slice_in_dim(kw_all, s0, WINDOW + c, axis=2)
        vw = lax.dynamic_slice_in_dim(vw_all, s0, WINDOW + c, axis=2)
        wpos = s0 - WINDOW + jnp.arange(WINDOW + c)
        m_win = (wpos[None] <= t[:, None]) & (wpos[None] > t[:, None] - WINDOW) & (wpos[None] >= 0)
        sw = jnp.einsum('bkgcd,bkjd->bkgcj', qc, kw).astype(jnp.float32) * ATTN_SCALE
        pw = masked_softmax(sw, m_win)
        o_win = jnp.einsum('bkgcj,bkjd->bkgcd', pw.astype(vw.dtype), vw)

        return gc[..., 0:1] * o_cmp + gc[..., 1:2] * o_slc + gc[..., 2:3] * o_win

    out = lax.map(chunk, jnp.arange(s // c))
    return out.transpose(1, 0, 4, 2, 3, 5).reshape(b, s, NSA_HEADS * dh)


def diff_attention(q, k, v, lam_vecs, subln_g, lambda_init, cos, sin):
    b, s, _ = q.shape
    dh = HEAD_DIM
    qh = apply_rope(to_heads(q, 2 * DIFF_HEADS, dh), cos, sin).reshape(b, DIFF_HEADS, 2, s, dh)
    kh = apply_rope(to_heads(k, 2 * DIFF_HEADS, dh), cos, sin).reshape(b, DIFF_HEADS, 2, s, dh)
    vh = to_heads(v, DIFF_HEADS, DIFF_VDIM)
    lv = lam_vecs.astype(jnp.float32)
    lam = jnp.exp(jnp.sum(lv[0] * lv[1])) - jnp.exp(jnp.sum(lv[2] * lv[3])) + lambda_init
    kpos = jnp.arange(s)
    cd = DENSE_CHUNK

    def block(ci):
        s0 = ci * cd
        t = s0 + jnp.arange(cd)
        qb = lax.dynamic_slice_in_dim(qh, s0, cd, axis=3)
        sc = jnp.einsum('bhmcd,bhmjd->bhmcj', qb, kh).astype(jnp.float32) * ATTN_SCALE
        p = masked_softmax(sc, kpos[None, :] <= t[:, None])
        a = p[:, :, 0] - lam * p[:, :, 1]
        return jnp.einsum('bhcj,bhjd->bhcd', a.astype(vh.dtype), vh)

    o = lax.map(block, jnp.arange(s // cd))
    o = o.transpose(1, 0, 3, 2, 4).reshape(b, s, DIFF_HEADS, DIFF_VDIM).astype(jnp.float32)
    o = o * lax.rsqrt(jnp.mean(jnp.square(o), -1, keepdims=True) + LN_EPS) * subln_g
    o = o * (1.0 - lambda_init)
    return o.astype(q.dtype).reshape(b, s, DIFF_HEADS * DIFF_VDIM)


def moba_attention(q, k, v, cos, sin):
    b, s, _ = q.shape
    h, dh, bl = MOBA_HEADS, HEAD_DIM, MOBA_BLOCK
    qh = apply_rope(to_heads(q, h, dh), cos, sin)
    kh = apply_rope(to_heads(k, h, dh), cos, sin)
    vh = to_heads(v, h, dh)
    n_blk = max(-(-s // bl), MOBA_TOPK)
    pad = ((0, 0), (0, 0), (0, n_blk * bl - s), (0, 0))
    kp = jnp.pad(kh, pad)
    vp = jnp.pad(vh, pad)
    kb = kp.reshape(b, h, n_blk, bl, dh)
    vb = vp.reshape(b, h, n_blk, bl, dh)
    k_mean = jnp.mean(kb.astype(jnp.float32), axis=3).astype(kh.dtype)
    bi = jnp.arange(b)[:, None, None, None]
    hi = jnp.arange(h)[None, :, None, None]
    blk = jnp.arange(n_blk)
    c = MOBA_CHUNK

    def chunk(ci):
        s0 = ci * c
        t = s0 + jnp.arange(c)
        j = s0 // bl
        qc = lax.dynamic_slice_in_dim(qh, s0, c, axis=2)
        score = jnp.einsum('bhcd,bhnd->bhcn', qc, k_mean).astype(jnp.float32)
        score = jnp.where(blk < j, score, -jnp.inf)
        _, sel = lax.top_k(score, MOBA_TOPK)
        sel_ok = jnp.arange(MOBA_TOPK) < j
        ks = kb[bi, hi, sel]
        vs = vb[bi, hi, sel]
        ko = lax.dynamic_slice_in_dim(kp, j * bl, bl, axis=2)
        vo = lax.dynamic_slice_in_dim(vp, j * bl, bl, axis=2)
        opos = j * bl + jnp.arange(bl)
        s_sel = jnp.einsum('bhcd,bhcnld->bhcnl', qc, ks).reshape(b, h, c, MOBA_TOPK * bl)
        s_own = jnp.einsum('bhcd,bhld->bhcl', qc, ko)
        sc = jnp.concatenate([s_sel, s_own], -1).astype(jnp.float32) * ATTN_SCALE
        mask = jnp.concatenate([
            jnp.broadcast_to(jnp.repeat(sel_ok, bl)[None, :], (c, MOBA_TOPK * bl)),
            opos[None, :] <= t[:, None]], -1)
        p = masked_softmax(sc, mask).astype(vh.dtype)
        o = jnp.einsum('bhcm,bhcmd->bhcd', p[..., :MOBA_TOPK * bl], vs.reshape(b, h, c, MOBA_TOPK * bl, dh))
        return o + jnp.einsum('bhcl,bhld->bhcd', p[..., MOBA_TOPK * bl:], vo)

    out = lax.map(chunk, jnp.arange(s // c))
    return out.transpose(1, 0, 3, 2, 4).reshape(b, s, h * dh)


def clamped_swiglu(hid):
    h_glu = jnp.minimum(hid[..., ::2], SWIGLU_LIMIT)
    h_lin = jnp.clip(hid[..., 1::2], -SWIGLU_LIMIT, SWIGLU_LIMIT)
    return h_glu * jax.nn.sigmoid(SWIGLU_ALPHA * h_glu) * (h_lin + 1.0)


def moe_ffn(x, w_router, b_router, w_gate_up, b_gate_up, w_down, b_down):
    n_tok, d = x.shape
    logits = (x @ w_router + b_router).astype(jnp.float32)
    top_logit, top_idx = lax.top_k(logits, TOP_K)
    gate = jax.nn.softmax(top_logit, axis=-1)
    n_assign = n_tok * TOP_K
    expert = top_idx.reshape(-1)
    token = jnp.arange(n_assign, dtype=jnp.int32) // TOP_K
    order = jnp.argsort(expert)
    expert_sorted = expert[order]
    counts = jnp.bincount(expert, length=N_EXPERTS)
    start = jnp.cumsum(counts) - counts
    padded = (counts + MOE_BLOCK - 1) // MOE_BLOCK * MOE_BLOCK
    pad_end = jnp.cumsum(padded)
    pad_start = pad_end - padded
    dest = pad_start[expert_sorted] + jnp.arange(n_assign) - start[expert_sorted]
    n_blocks = -(-n_assign // MOE_BLOCK) + N_EXPERTS
    n_rows = n_blocks * MOE_BLOCK
    row_token = jnp.full((n_rows,), n_tok, jnp.int32).at[dest].set(token[order])
    row_gate = jnp.zeros((n_rows,), jnp.float32).at[dest].set(gate.reshape(-1)[order])
    block_expert = jnp.minimum(
        jnp.searchsorted(pad_end, jnp.arange(n_blocks) * MOE_BLOCK, side='right'), N_EXPERTS - 1)
    x_pad = jnp.concatenate([x, jnp.zeros((1, d), x.dtype)], 0)

    def expert_block(args):
        rows, e = args
        hid = x_pad[rows] @ w_gate_up[e] + b_gate_up[e]
        return clamped_swiglu(hid) @ w_down[e] + b_down[e]

    out = lax.map(expert_block, (row_token.reshape(n_blocks, MOE_BLOCK), block_expert))
    out = out.reshape(n_rows, d) * row_gate[:, None].astype(out.dtype)
    return jnp.zeros((n_tok + 1, d), out.dtype).at[row_token].add(out)[:n_tok]


def setup_inputs(seed: int = 0) -> dict:
    key = jax.random.key(seed)
    k = jax.random.split(key, 19)
    f32 = jnp.float32
    L, D, E, F = DEPTH, D_MODEL, N_EXPERTS, D_EXPERT

    def normal(kk, shape, scale):
        return jax.random.normal(kk, shape, f32) * scale

    return {
        'x': normal(k[0], (BATCH, SEQ, D), 1.0),
        'positions': jnp.broadcast_to(jnp.arange(SEQ, dtype=jnp.int32)[None, :], (BATCH, SEQ)),
        'w_in': normal(k[1], (L, D, IN_COLS), D ** -0.5),
        'nsa_cmp_pos': normal(k[2], (L, 2, CMP_LEN, HEAD_DIM), 0.02),
        'nsa_cmp_w1': normal(k[3], (L, 2, CMP_LEN * HEAD_DIM, HEAD_DIM), (CMP_LEN * HEAD_DIM) ** -0.5),
        'nsa_cmp_w2': normal(k[4], (L, 2, HEAD_DIM, HEAD_DIM), HEAD_DIM ** -0.5),
        'diff_lambda': normal(k[5], (L, 4, HEAD_DIM), 0.1),
        'diff_subln_g': 1.0 + normal(k[6], (L, DIFF_VDIM), 0.02),
        'w_out': normal(k[7], (L, MIX_WIDTH, D), MIX_WIDTH ** -0.5 * DEEPNORM_BETA),
        'ln1_g': 1.0 + normal(k[8], (L, D), 0.02),
        'ln1_b': normal(k[9], (L, D), 0.02),
        'w_router': normal(k[10], (L, D, E), D ** -0.5),
        'b_router': normal(k[11], (L, E), 0.01),
        'w_gate_up': normal(k[12], (L, E, D, 2 * F), D ** -0.5),
        'b_gate_up': normal(k[13], (L, E, 2 * F), 0.01),
        'w_down': normal(k[14], (L, E, F, D), F ** -0.5 * DEEPNORM_BETA),
        'b_down': normal(k[15], (L, E, D), 0.01),
        'ln2_g': 1.0 + normal(k[16], (L, D), 0.02),
        'ln2_b': normal(k[17], (L, D), 0.02),
    }


def reference(x, positions, w_in, nsa_cmp_pos, nsa_cmp_w1, nsa_cmp_w2, diff_lambda, diff_subln_g,
              w_out, ln1_g, ln1_b, w_router, b_router, w_gate_up, b_gate_up, w_down, b_down,
              ln2_g, ln2_b):
    b, s, d = x.shape
    cos, sin = rope_tables(positions)
    for layer in range(DEPTH):
        h = x @ w_in[layer]
        (nq, nkc, nvc, nks, nvs, nkw, nvw, ngate,
         dq, dk, dv, mq, mk, mv) = split_columns(h)
        y_nsa = nsa_attention(nq, nkc, nvc, nks, nvs, nkw, nvw, ngate,
                              nsa_cmp_pos[layer], nsa_cmp_w1[layer], nsa_cmp_w2[layer], cos, sin)
        lambda_init = 0.8 - 0.6 * math.exp(-0.3 * layer)
        y_diff = diff_attention(dq, dk, dv, diff_lambda[layer], diff_subln_g[layer], lambda_init, cos, sin)
        y_moba = moba_attention(mq, mk, mv, cos, sin)
        mix = jnp.concatenate([y_nsa, y_diff, y_moba], -1) @ w_out[layer]
        x = layer_norm(DEEPNORM_ALPHA * x + mix, ln1_g[layer], ln1_b[layer])
        ffn = moe_ffn(x.reshape(b * s, d), w_router[layer], b_router[layer], w_gate_up[layer],
                      b_gate_up[layer], w_down[layer], b_down[layer]).reshape(b, s, d)
        x = layer_norm(DEEPNORM_ALPHA * x + ffn, ln2_g[layer], ln2_b[layer])
    return x
```

```python
import numpy as np
import os
SKIP = os.environ.get('SKIP', '')
import concourse.bass as bass
import concourse.mybir as mybir
from concourse.bass_utils import run_bass_kernel_spmd
from contextlib import ExitStack

F32 = mybir.dt.float32
BF16 = mybir.dt.bfloat16
I32 = mybir.dt.int32
ALU = mybir.AluOpType
AF = mybir.ActivationFunctionType
AX = mybir.AxisListType

ENGS = ('pe', 'dve', 'act', 'pool', 'sp')


class Buf:
    __slots__ = ('name', 'w', 'r', 'excl')

    def __init__(self, name='', excl=False):
        self.name = name
        self.excl = excl
        self.w = None
        self.r = {}


class Prog:
    EPOCH = 16000
    NDMA = 32

    def __init__(self, nc, stack):
        self.nc = nc
        self.stack = stack
        self.ops = {e: [] for e in ENGS}
        self.cnt = {e: 0 for e in ENGS}
        self.esems = {e: [] for e in ENGS}
        self.waited = {e: {} for e in ENGS}
        self.dma_sems = [self.new_sem(f"dq{i}") for i in range(self.NDMA)]
        self.dma_val = [0] * self.NDMA
        self.dma_pool = {'sp': list(range(0, 20)), 'pool': list(range(20, 32)), 'act': []}
        self.dma_next = {'sp': 0, 'pool': 0}
        self.nwaits = 0
        self.alloc_stack = stack
        self.eobj = {'pe': nc.tensor, 'dve': nc.vector, 'act': nc.scalar, 'pool': nc.gpsimd, 'sp': nc.sync}

    def new_sem(self, name):
        return self.stack.enter_context(self.nc.semaphore(name))

    def sb(self, name, shape, dt):
        self._uid = getattr(self, '_uid', 0) + 1
        name = f"{name}_{self._uid}"
        return self.alloc_stack.enter_context(self.nc.sbuf_tensor(name, list(shape), dt))

    def ps(self, name, shape, dt=F32):
        self._uid = getattr(self, '_uid', 0) + 1
        name = f"{name}_{self._uid}"
        return self.alloc_stack.enter_context(self.nc.psum_tensor(name, list(shape), dt))

    def op(self, eng, fn, reads=(), writes=(), dma=False):
        xr = [b for b in reads if b.excl]
        if xr:
            reads = [b for b in reads if not b.excl]
            writes = list(writes) + [b for b in xr if b not in writes]
        deps = []
        for b in reads:
            if b.w is not None:
                deps.append(b.w)
        for b in writes:
            if b.w is not None:
                deps.append(b.w)
            for ev in b.r.values():
                deps.append(ev)
        if dma:
            pl = self.dma_pool[eng]
            i = pl[self.dma_next[eng] % len(pl)]
            self.dma_next[eng] += 1
            sem = self.dma_sems[i]
            prev = self.dma_val[i]
            if prev > 0:
                deps.append((sem, prev, 'dma'))
            self.dma_val[i] = prev + 16
            ev = (sem, prev + 16, 'dma')
            inc = 16
        else:
            k = self.cnt[eng]
            ep = k // self.EPOCH
            if ep >= len(self.esems[eng]):
                self.esems[eng].append(self.new_sem(f"{eng}{ep}"))
            sem = self.esems[eng][ep]
            self.cnt[eng] = k + 1
            ev = (sem, k - ep * self.EPOCH + 1, eng)
            inc = 1
        wd = self.waited[eng]
        waits = []
        for (s, v, e) in deps:
            if e == 'pe' and eng == 'pe' and not dma:
                continue
            if wd.get(s.num, 0) < v:
                wd[s.num] = v
                waits.append((s, v))
        self.nwaits += len(waits)
        eo = self.eobj[eng]
        for (s, v) in waits:
            eo.wait_ge(s, v)
        fn(eo).then_inc(sem, inc)
        for b in reads:
            old = b.r.get(sem.num)
            if old is None or old[1] < ev[1]:
                b.r[sem.num] = ev
        for b in writes:
            b.w = ev
            b.r = {}
        return ev


    def barrier(self):
        evs = []
        for e in ENGS:
            k = self.cnt[e]
            if k == 0:
                continue
            ep = (k - 1) // self.EPOCH
            evs.append((self.esems[e][ep], k - ep * self.EPOCH))
        for i, s in enumerate(self.dma_sems):
            if self.dma_val[i] > 0:
                evs.append((s, self.dma_val[i]))
        for e in ENGS:
            wd = self.waited[e]
            waits = []
            for (s, v) in evs:
                if wd.get(s.num, 0) < v:
                    wd[s.num] = v
                    waits.append((s, v))
            for (s, v) in waits:
                self.eobj[e].wait_ge(s, v)

    def push_scope(self):
        st = ExitStack()
        st.__enter__()
        self._saved = getattr(self, '_saved', [])
        self._saved.append(self.alloc_stack)
        self.alloc_stack = st
        return st

    def pop_scope(self):
        self.barrier()
        st = self.alloc_stack
        self.alloc_stack = self._saved.pop()
        st.__exit__(None, None, None)

    def dma(self, out, in_, reads=(), writes=(), eng='sp', **kw):
        return self.op(eng, lambda e: e.dma_start(out=out, in_=in_, **kw), reads, writes, dma=True)

    def finish(self):
        wd = self.waited['sp']
        waits = []
        for i, s in enumerate(self.dma_sems):
            v = self.dma_val[i]
            if v > 0 and wd.get(s.num, 0) < v:
                waits.append((s, v))
        for e in ENGS:
            if e == 'sp' or self.cnt[e] == 0:
                continue
            k = self.cnt[e]
            ep = (k - 1) // self.EPOCH
            s = self.esems[e][ep]
            v = k - ep * self.EPOCH
            if wd.get(s.num, 0) < v:
                waits.append((s, v))
        self.final_waits = waits

    def emit(self):
        self.finish()
        for (s, v) in self.final_waits:
            self.eobj['sp'].wait_ge(s, v)


S_LEN = 8192
D_MODEL = 4096
SCALE = 128 ** -0.5
NEGM = 30000.0
PI = 3.141592653589793


def _sel_causal(q0, k0):
    return dict(pattern=[[1, 512]], cm=-1, base=q0 - k0)


def attn_block(P, nm, qT, tiles, S_ps, bS, PT, bPT, O_ps, bO, dv1, q_reads):
    n = len(tiles)
    first = {}
    last = {}
    for i, t in enumerate(tiles):
        for s in t['subs']:
            first.setdefault(s, i)
            last[s] = i

    def emitS(i):
        t = tiles[i]
        sb_ = S_ps[i % len(S_ps)]
        b = bS[i % len(S_ps)]
        mm = t.get('mm')
        P.op('pe', lambda e: e.matmul(sb_[:, :], lhsT=t['kT'], rhs=qT, start=True, stop=(mm is None)),
             list(t['reads']) + list(q_reads), [b])
        if mm is not None:
            P.op('pe', lambda e: e.matmul(sb_[:, :], lhsT=mm[0], rhs=mm[1], start=False, stop=True),
                 list(mm[2]), [b])

    emitS(0)
    for i in range(n):
        if i + 1 < n:
            emitS(i + 1)
        t = tiles[i]
        sb_ = S_ps[i % len(S_ps)]
        b = bS[i % len(S_ps)]
        pt = PT[i % len(PT)]
        bp = bPT[i % len(PT)]
        P.op('act', lambda e: e.activation(out=pt[:, :], in_=sb_[:, :], func=AF.Exp, scale=SCALE), [b], [bp])
        sel = t.get('sel')
        if sel is not None:
            P.op('pool', lambda e: e.affine_select(out=pt[:, :], in_=pt[:, :], pattern=sel['pattern'],
                                                   compare_op=ALU.is_ge, fill=0.0, base=sel['base'],
                                                   channel_multiplier=sel['cm']), [bp], [bp])
        for s in t['subs']:
            P.op('pe', (lambda s: lambda e: e.matmul(O_ps[s][:, 0:dv1], lhsT=pt[:, s * 128:(s + 1) * 128],
                                                     rhs=t['v'], start=(first[s] == i), stop=(last[s] == i)))(s),
                 [bp] + list(t['reads']), [bO[s]])


def build_mixer(debug=False, stop_after=None, n_tt=16, n_ph=3, n_qt=16, mixers='NDM'):
    nc = bass.Bass("TRN2", target_bir_lowering=False)
    S = S_LEN
    xT = nc.dram_tensor("xT", [D_MODEL, S], F32, kind="ExternalInput").ap()
    wA = nc.dram_tensor("wA", [D_MODEL, 2828], F32, kind="ExternalInput").ap()
    pos = nc.dram_tensor("pos", [1, S], I32, kind="ExternalInput").ap()
    cpos = nc.dram_tensor("cpos", [2, 32, 128], F32, kind="ExternalInput").ap()
    cw1 = nc.dram_tensor("cw1", [2, 4096, 128], F32, kind="ExternalInput").ap()
    cw2 = nc.dram_tensor("cw2", [2, 128, 128], F32, kind="ExternalInput").ap()
    dlam = nc.dram_tensor("dlam", [1, 512], F32, kind="ExternalInput").ap()
    subg = nc.dram_tensor("subg", [1, 256], F32, kind="ExternalInput").ap()
    c_invf = nc.dram_tensor("c_invf", [32, 1], F32, kind="ExternalInput").ap()
    c_sw = nc.dram_tensor("c_sw", [128, 32], F32, kind="ExternalInput").ap()
    c_lam = nc.dram_tensor("c_lam", [1, 2], F32, kind="ExternalInput").ap()
    c_fbig = nc.dram_tensor("c_fbig", [128, 256], F32, kind="ExternalInput").ap()
    mix = nc.dram_tensor("mix", [S, 1024], F32, kind="ExternalOutput").ap()
    kd = "ExternalOutput" if debug else "Internal"
    cos_d = nc.dram_tensor("cos_d", [32, S], F32, kind=kd).ap()
    sin_d = nc.dram_tensor("sin_d", [32, S], F32, kind=kd).ap()
    pT_d = nc.dram_tensor("pT_d", [16, 128, S], BF16, kind=kd).ap()
    pV_d = nc.dram_tensor("pV_d", [3, S, 256], BF16, kind=kd).ap()
    gate_d = nc.dram_tensor("gate_d", [S, 12], F32, kind=kd).ap()
    b_cs = Buf()
    b_pT = [Buf() for _ in range(16)]
    b_pV = [Buf() for _ in range(3)]
    b_gate = Buf()

    with ExitStack() as st:
        P = Prog(nc, st)
        ident = P.sb("ident", [128, 128], F32); b_ident = Buf()
        P.op('pool', lambda e: e.memset(ident[:, :], 1.0), [], [b_ident])
        P.op('pool', lambda e: e.affine_select(out=ident[:, :], in_=ident[:, :], pattern=[[-1, 128]],
                                               compare_op=ALU.is_equal, fill=0.0, base=0, channel_multiplier=1),
             [b_ident], [b_ident])
        swm = P.sb("swm", [128, 32], BF16); b_swm = Buf()
        P.dma(swm[:, :], c_sw, writes=[b_swm], eng='pool')

        P.push_scope()
        invf = P.sb("invf", [32, 1], F32); b_invf = Buf()
        P.dma(invf[:, :], c_invf, writes=[b_invf])
        CH = 2048
        posi = P.sb("posi", [32, CH], I32); b_posi = Buf()
        ang = P.sb("ang", [32, CH], F32); b_ang = Buf()
        m1 = P.sb("m1", [32, CH], F32); b_m1 = Buf()
        tb = P.sb("tb", [32, CH], F32); b_tb = Buf()
        a2 = P.sb("a2", [32, CH], F32); b_a2 = Buf()
        for c in range(S // CH):
            sl = slice(c * CH, (c + 1) * CH)
            P.dma(posi[:, :], pos[:, sl].to_broadcast([32, CH]), writes=[b_posi])
            P.op('dve', lambda e: e.tensor_copy(out=ang[:, :], in_=posi[:, :]), [b_posi], [b_ang])
            P.op('dve', lambda e: e.tensor_scalar(out=ang[:, :], in0=ang[:, :], scalar1=invf[:, 0:1], scalar2=None,
                                                  op0=ALU.mult), [b_ang, b_invf], [b_ang])
            for (shift, dst) in ((0.0, sin_d), (0.5 * PI, cos_d)):
                P.op('dve', lambda e: e.tensor_scalar(out=a2[:, :], in0=ang[:, :], scalar1=shift, scalar2=None,
                                                      op0=ALU.add), [b_ang], [b_a2])
                P.op('dve', lambda e: e.tensor_scalar(out=m1[:, :], in0=a2[:, :], scalar1=1.0 / (2 * PI), scalar2=None,
                                                      op0=ALU.mult), [b_a2], [b_m1])
                P.op('dve', lambda e: e.tensor_copy(out=posi[:, :], in_=m1[:, :]), [b_m1], [b_posi])
                P.op('dve', lambda e: e.tensor_copy(out=m1[:, :], in_=posi[:, :]), [b_posi], [b_m1])
                P.op('dve', lambda e: e.scalar_tensor_tensor(out=m1[:, :], in0=m1[:, :], scalar=-2 * PI, in1=a2[:, :],
                                                             op0=ALU.mult, op1=ALU.add), [b_m1, b_a2], [b_m1])
                P.op('dve', lambda e: e.tensor_scalar(out=a2[:, :], in0=m1[:, :], scalar1=PI, scalar2=-2 * PI,
                                                      op0=ALU.is_gt, op1=ALU.mult), [b_m1], [b_a2])
                P.op('dve', lambda e: e.tensor_tensor(out=m1[:, :], in0=m1[:, :], in1=a2[:, :], op=ALU.add), [b_m1, b_a2], [b_m1])
                P.op('dve', lambda e: e.tensor_scalar(out=a2[:, :], in0=m1[:, :], scalar1=-PI, scalar2=2 * PI,
                                                      op0=ALU.is_lt, op1=ALU.mult), [b_m1], [b_a2])
                P.op('dve', lambda e: e.tensor_tensor(out=m1[:, :], in0=m1[:, :], in1=a2[:, :], op=ALU.add), [b_m1, b_a2], [b_m1])
                P.op('dve', lambda e: e.tensor_scalar(out=m1[:, :], in0=m1[:, :], scalar1=0.999999, scalar2=None,
                                                      op0=ALU.mult), [b_m1], [b_m1])
                P.op('act', lambda e: e.activation(out=tb[:, :], in_=m1[:, :], func=AF.Sin), [b_m1], [b_tb])
                P.dma(dst[:, sl], tb[:, :], reads=[b_tb], writes=[b_cs])
        P.pop_scope()
        if stop_after == 'rope':
            P.emit()
            return nc

        phases = [
            (0, 8, [1, 1, 1, 1, 1, 0, 1, 1], 256, True, 0, 0),
            (1292, 4, [1, 1, 1, 1], 256, False, 8, 1),
            (2060, 4, [1, 1, 1, 1], 256, False, 12, 2),
        ]
        xTv = xT.rearrange("(k p) t -> p k t", p=128)
        wAv = wA.rearrange("(k p) n -> p k n", p=128)
        for (col0, nT, ropef, nV, has_gate, pTb, pVi) in [p for p, mch in zip(phases, 'NDM') if mch in mixers][:n_ph]:
            ncols = nT * 128 + nV + (12 if has_gate else 0)
            nVg = nV + (12 if has_gate else 0)
            P.push_scope()
            wb = P.sb("wb", [128, 32, ncols], BF16); b_wb = Buf()
            for q in range(4):
                P.dma(wb[:, q * 8:(q + 1) * 8, :], wAv[:, q * 8:(q + 1) * 8, col0:col0 + ncols], writes=[b_wb], eng='pool')
            xb = [P.sb(f"xb{i}", [128, 32, 512], BF16) for i in range(2)]; b_xb = [Buf(), Buf()]
            cs = [P.sb(f"cs{i}", [32, 2, 512], F32) for i in range(2)]; b_csb = [Buf(), Buf()]
            stg = [P.sb(f"stg{i}", [128, 512], BF16) for i in range(3)]; b_stg = [Buf() for _ in range(3)]
            vst = [P.sb(f"vst{i}", [128, 256], BF16) for i in range(2)]; b_vst = [Buf(), Buf()]
            gst = [P.sb(f"gst{i}", [128, 12], F32) for i in range(2)]; b_gst = [Buf(), Buf()]
            t1 = P.sb("t1", [32, 512], F32); b_t1 = Buf()
            t2 = P.sb("t2", [32, 512], F32); b_t2 = Buf()
            pp = [P.ps(f"pp{i}", [128, 512]) for i in range(3)]; b_pp = [Buf(excl=True) for _ in range(3)]
            psw = [P.ps(f"psw{i}", [128, 512]) for i in range(2)]; b_psw = [Buf(excl=True), Buf(excl=True)]
            pv = [P.ps(f"pv{i}", [128, 512]) for i in range(2)]; b_pv = [Buf(excl=True), Buf(excl=True)]
            ic = 0
            iv = 0
            for tt in range(n_tt):
                t0 = tt * 512
                x_ = xb[tt % 2]; bx = b_xb[tt % 2]
                for q in range(2):
                    P.dma(x_[:, q * 16:(q + 1) * 16, :], xTv[:, q * 16:(q + 1) * 16, t0:t0 + 512], writes=[bx], eng='pool')
                c_ = cs[tt % 2]; bc = b_csb[tt % 2]
                P.dma(c_[:, 0, :], cos_d[:, t0:t0 + 512], reads=[b_cs], writes=[bc])
                P.dma(c_[:, 1, :], sin_d[:, t0:t0 + 512], reads=[b_cs], writes=[bc])
                for c in range(nT if 'T' not in SKIP else 0):
                    ps_ = pp[ic % 3]; bp = b_pp[ic % 3]
                    sg = stg[ic % 3]; bs = b_stg[ic % 3]
                    for k in range(32):
                        P.op('pe', lambda e: e.matmul(ps_[:, :], lhsT=wb[:, k, c * 128:(c + 1) * 128], rhs=x_[:, k, :],
                                                      start=(k == 0), stop=(k == 31)), [b_wb, bx], [bp])
                    P.op('act', lambda e: e.copy(out=sg[:, :], in_=ps_[:, :]), [bp], [bs])
                    if ropef[c] and 'R' not in SKIP:
                        sw_ = psw[ic % 2]; bw_ = b_psw[ic % 2]
                        if '1' not in SKIP:
                            P.op('pe', lambda e: e.matmul(sw_[0:32, :], lhsT=swm[:, :], rhs=sg[:, :], start=True, stop=True),
                                 [bs, b_swm], [bw_])
                        if '2' not in SKIP:
                            P.op('dve', lambda e: e.tensor_tensor(out=t1[:, :], in0=ps_[0:32, :], in1=c_[:, 0, :], op=ALU.mult),
                                 [bp, bc], [b_t1])
                        if '3' not in SKIP:
                            P.op('dve', lambda e: e.tensor_tensor(out=t2[:, :], in0=sw_[0:32, :], in1=c_[:, 1, :], op=ALU.mult),
                                 [bw_, bc], [b_t2])
                        if '4' not in SKIP:
                            P.op('dve', lambda e: e.tensor_tensor(out=sg[0:32, :], in0=t1[:, :], in1=t2[:, :], op=ALU.add),
                                 [b_t1, b_t2], [bs])
                    P.dma(pT_d[pTb + c, :, t0:t0 + 512], sg[:, :], reads=[bs], writes=[b_pT[pTb + c]])
                    ic += 1
                for sub in range(0 if 'V' not in SKIP else 4, 4):
                    pv_ = pv[iv % 2]; bpv = b_pv[iv % 2]
                    vs_ = vst[iv % 2]; bvs = b_vst[iv % 2]
                    for k in range(32):
                        P.op('pe', lambda e: e.matmul(pv_[:, 0:nVg], lhsT=x_[:, k, sub * 128:(sub + 1) * 128],
                                                      rhs=wb[:, k, nT * 128:nT * 128 + nVg],
                                                      start=(k == 0), stop=(k == 31)), [b_wb, bx], [bpv])
                    P.op('act', lambda e: e.copy(out=vs_[:, 0:nV], in_=pv_[:, 0:nV]), [bpv], [bvs])
                    r0 = t0 + sub * 128
                    P.dma(pV_d[pVi, r0:r0 + 128, :], vs_[:, :], reads=[bvs], writes=[b_pV[pVi]])
                    if has_gate and 'G' not in SKIP:
                        g_ = gst[iv % 2]; bg = b_gst[iv % 2]
                        P.op('act', lambda e: e.activation(out=g_[:, :], in_=pv_[:, nV:nV + 12], func=AF.Sigmoid), [bpv], [bg])
                        P.dma(gate_d[r0:r0 + 128, :], g_[:, :], reads=[bg], writes=[b_gate])
                    iv += 1
            P.pop_scope()

        if stop_after == 'proj':
            P.emit()
            return nc

        def ld_v(v_sb, b_v, pvi, c0, dv):
            src = pV_d[pvi].rearrange("(t p) c -> p t c", p=128)
            for q in range(4):
                P.dma(v_sb[:, q * 16:(q + 1) * 16, 0:dv], src[:, q * 16:(q + 1) * 16, c0:c0 + dv], reads=[b_pV[pvi]], writes=[b_v])
            P.op('pool', lambda e: e.memset(v_sb[:, :, dv:dv + 1], 1.0), [], [b_v])

        def causal_tiles(qt, kTs, b_k, v_sb, b_v, mm_fn=None):
            tl = []
            for kt in range(4 * qt + 4):
                a = kt - 4 * qt
                t = dict(kT=kTs[:, kt * 128:(kt + 1) * 128], v=v_sb[:, kt, :], reads=[b_k, b_v],
                         sel=_sel_causal(qt * 512, kt * 128) if a >= 0 else None,
                         subs=[s_ for s_ in range(4) if a <= s_])
                if mm_fn is not None:
                    t['mm'] = mm_fn(kt)
                tl.append(t)
            return tl

        def recip_col(rz, b_rz, src_ap, src_bufs):
            P.op('dve', lambda e: e.tensor_scalar(out=rz[:, 0:1], in0=src_ap, scalar1=1e-30, scalar2=None, op0=ALU.max),
                 src_bufs, [b_rz])
            P.op('dve', lambda e: e.reciprocal(out=rz[:, 0:1], in_=rz[:, 0:1]), [b_rz], [b_rz])

        qts = range(n_qt)

        if 'D' in mixers:
            P.push_scope()
            S_ps = [P.ps(f"S{i}", [128, 512]) for i in range(2)]; bS = [Buf(excl=True) for _ in range(2)]
            O_ps = [P.ps(f"O{i}", [128, 512]) for i in range(4)]; bO = [Buf(excl=True) for _ in range(4)]
            PT = [P.sb(f"PT{i}", [128, 512], BF16) for i in range(3)]; bPT = [Buf() for _ in range(3)]
            kT = [P.sb(f"dk{m}", [128, S], BF16) for m in range(2)]; b_kT = [Buf(), Buf()]
            for m in range(2):
                P.dma(kT[m][:, :], pT_d[10 + m], reads=[b_pT[10 + m]], writes=[b_kT[m]])
            v_sb = P.sb("dv", [128, 64, 257], BF16); b_v = Buf()
            ld_v(v_sb, b_v, 1, 0, 256)
            lv = P.sb("lv", [128, 512], F32); b_lv = Buf()
            P.dma(lv[:, :], dlam.to_broadcast([128, 512]), writes=[b_lv])
            lc = P.sb("lc", [128, 2], F32); b_lc = Buf()
            P.dma(lc[:, :], c_lam.to_broadcast([128, 2]), writes=[b_lc])
            gsc = P.sb("gsc", [128, 256], F32); b_gsc = Buf()
            P.dma(gsc[:, :], subg.to_broadcast([128, 256]), writes=[b_gsc])
            P.op('dve', lambda e: e.tensor_scalar(out=gsc[:, :], in0=gsc[:, :], scalar1=lc[:, 1:2], scalar2=None, op0=ALU.mult),
                 [b_gsc, b_lc], [b_gsc])
            pr = P.sb("pr", [128, 256], F32); b_pr = Buf()
            sm = P.sb("sm", [128, 4], F32); b_sm = Buf()
            P.op('dve', lambda e: e.tensor_tensor(out=pr[:, 0:128], in0=lv[:, 0:128], in1=lv[:, 128:256], op=ALU.mult), [b_lv], [b_pr])
            P.op('dve', lambda e: e.tensor_tensor(out=pr[:, 128:256], in0=lv[:, 256:384], in1=lv[:, 384:512], op=ALU.mult), [b_lv], [b_pr])
            P.op('dve', lambda e: e.reduce_sum(out=sm[:, 0:1], in_=pr[:, 0:128], axis=AX.X), [b_pr], [b_sm])
            P.op('dve', lambda e: e.reduce_sum(out=sm[:, 1:2], in_=pr[:, 128:256], axis=AX.X), [b_pr], [b_sm])
            P.op('act', lambda e: e.activation(out=sm[:, 0:2], in_=sm[:, 0:2], func=AF.Exp), [b_sm], [b_sm])
            P.op('dve', lambda e: e.tensor_tensor(out=sm[:, 2:3], in0=sm[:, 1:2], in1=sm[:, 0:1], op=ALU.subtract), [b_sm], [b_sm])
            P.op('dve', lambda e: e.tensor_tensor(out=sm[:, 2:3], in0=sm[:, 2:3], in1=lc[:, 0:1], op=ALU.subtract), [b_sm, b_lc], [b_sm])
            qsb = [P.sb(f"dq{i}", [128, 2, 512], BF16) for i in range(2)]; b_q = [Buf(), Buf()]
            o0 = P.sb("o0", [128, 4, 256], F32); b_o0 = Buf()
            ot = P.sb("ot", [128, 4, 256], F32); b_ot = Buf()
            ss4 = P.sb("ss4", [128, 4], F32); b_ss4 = Buf()
            sq = P.sb("sq", [128, 256], F32); b_sq = Buf()
            rz = P.sb("rz", [128, 4], F32); b_rz = Buf()
            outst = [P.sb(f"dout{i}", [128, 4, 256], F32) for i in range(2)]; b_out = [Buf(), Buf()]
            for qt in qts:
                q_ = qsb[qt % 2]; bq = b_q[qt % 2]
                for m in range(2):
                    P.dma(q_[:, m, :], pT_d[8 + m, :, qt * 512:(qt + 1) * 512], reads=[b_pT[8 + m]], writes=[bq])
                ost = outst[qt % 2]; bost = b_out[qt % 2]
                for m in range(2):
                    tl = causal_tiles(qt, kT[m], b_kT[m], v_sb, b_v)
                    attn_block(P, "d", q_[:, m, :], tl, S_ps, bS, PT, bPT, O_ps, bO, 257, [bq])
                    for s_ in range(4):
                        recip_col(rz, b_rz, O_ps[s_][:, 256:257], [bO[s_]])
                        if m == 0:
                            P.op('dve', lambda e: e.tensor_scalar(out=o0[:, s_, :], in0=O_ps[s_][:, 0:256], scalar1=rz[:, 0:1],
                                                                  scalar2=None, op0=ALU.mult), [bO[s_], b_rz], [b_o0])
                        else:
                            P.op('dve', lambda e: e.tensor_tensor(out=rz[:, 1:2], in0=rz[:, 0:1], in1=sm[:, 2:3], op=ALU.mult),
                                 [b_rz, b_sm], [b_rz])
                            P.op('dve', lambda e: e.scalar_tensor_tensor(out=ot[:, s_, :], in0=O_ps[s_][:, 0:256], scalar=rz[:, 1:2],
                                                                         in1=o0[:, s_, :], op0=ALU.mult, op1=ALU.add),
                                 [bO[s_], b_rz, b_o0], [b_ot])
                            P.op('dve', lambda e: e.tensor_tensor(out=sq[:, :], in0=ot[:, s_, :], in1=ot[:, s_, :], op=ALU.mult), [b_ot], [b_sq])
                            P.op('dve', lambda e: e.reduce_sum(out=ss4[:, s_:s_ + 1], in_=sq[:, :], axis=AX.X), [b_sq], [b_ss4])
                    if m == 1:
                        P.op('dve', lambda e: e.tensor_scalar(out=ss4[:, :], in0=ss4[:, :], scalar1=1.0 / 256, scalar2=1e-5,
                                                              op0=ALU.mult, op1=ALU.add), [b_ss4], [b_ss4])
                        P.op('act', lambda e: e.sqrt(out=ss4[:, :], in_=ss4[:, :]), [b_ss4], [b_ss4])
                        P.op('dve', lambda e: e.reciprocal(out=ss4[:, :], in_=ss4[:, :]), [b_ss4], [b_ss4])
                        for s_ in range(4):
                            P.op('dve', lambda e: e.scalar_tensor_tensor(out=ost[:, s_, :], in0=ot[:, s_, :], scalar=ss4[:, s_:s_ + 1],
                                                                         in1=gsc[:, :], op0=ALU.mult, op1=ALU.mult),
                                 [b_ot, b_ss4, b_gsc], [bost])
                P.dma(mix[qt * 512:(qt + 1) * 512, 512:768].rearrange("(s p) c -> p s c", p=128), ost[:, :, :], reads=[bost])
            P.pop_scope()
        if stop_after == 'diff':
            P.emit()
            return nc

        if 'M' in mixers:
            P.push_scope()
            S_ps = [P.ps(f"S{i}", [128, 512]) for i in range(2)]; bS = [Buf(excl=True) for _ in range(2)]
            O_ps = [P.ps(f"O{i}", [128, 512]) for i in range(4)]; bO = [Buf(excl=True) for _ in range(4)]
            aux = P.ps("aux", [128, 512]); b_aux = Buf(excl=True)
            aux2 = P.ps("aux2", [128, 512]); b_aux2 = Buf(excl=True)
            PT = [P.sb(f"PT{i}", [128, 512], BF16) for i in range(3)]; bPT = [Buf() for _ in range(3)]
            kT = [P.sb(f"mk{m}", [128, S], BF16) for m in range(2)]; b_kT = [Buf(), Buf()]
            v_sb = [P.sb(f"mv{m}", [128, 64, 129], BF16) for m in range(2)]; b_v = [Buf(), Buf()]
            kmf = P.sb("kmf", [128, 32], F32); b_kmf = Buf()
            kmb = [P.sb(f"kmb{m}", [128, 32], BF16) for m in range(2)]; b_kmb = [Buf(), Buf()]
            for m in range(2):
                P.dma(kT[m][:, :], pT_d[14 + m], reads=[b_pT[14 + m]], writes=[b_kT[m]])
                ld_v(v_sb[m], b_v[m], 2, m * 128, 128)
                P.op('dve', lambda e: e.tensor_reduce(out=kmf[:, :], in_=kT[m][:, :].rearrange("p (b k) -> p b k", k=256),
                                                      axis=AX.X, op=ALU.add), [b_kT[m]], [b_kmf])
                P.op('dve', lambda e: e.tensor_scalar(out=kmb[m][:, :], in0=kmf[:, :], scalar1=1.0 / 256, scalar2=None, op0=ALU.mult),
                     [b_kmf], [b_kmb[m]])
            indm = P.sb("indm", [32, S], BF16); b_indm = Buf()
            P.op('pool', lambda e: e.memset(indm[:, :], 1.0), [], [b_indm])
            P.op('pool', lambda e: e.affine_select(out=indm[:, :], in_=indm[:, :], pattern=[[1, S]], compare_op=ALU.is_ge, fill=0.0,
                                                   base=0, channel_multiplier=-256), [b_indm], [b_indm])
            P.op('pool', lambda e: e.affine_select(out=indm[:, :], in_=indm[:, :], pattern=[[-1, S]], compare_op=ALU.is_ge, fill=0.0,
                                                   base=255, channel_multiplier=256), [b_indm], [b_indm])
            qsb = [P.sb(f"mq{i}", [128, 2, 512], BF16) for i in range(2)]; b_q = [Buf(), Buf()]
            sc = P.sb("sc", [128, 32], F32); b_sc = Buf()
            m8 = P.sb("m8", [128, 8], F32); b_m8 = Buf()
            nm = P.sb("nm", [128, 32], F32); b_nm = Buf()
            negT = [P.sb(f"negT{i}", [32, 512], BF16) for i in range(2)]; b_negT = [Buf(), Buf()]
            rz = P.sb("rz", [128, 4], F32); b_rz = Buf()
            outst = [P.sb(f"mout{i}", [128, 4, 256], F32) for i in range(2)]; b_out = [Buf(), Buf()]
            for qt in qts:
                q_ = qsb[qt % 2]; bq = b_q[qt % 2]
                for m in range(2):
                    P.dma(q_[:, m, :], pT_d[12 + m, :, qt * 512:(qt + 1) * 512], reads=[b_pT[12 + m]], writes=[bq])
                ost = outst[qt % 2]; bost = b_out[qt % 2]
                for m in range(2):
                    ng = negT[m]; bng = b_negT[m]
                    for s_ in range(4):
                        jb = (qt * 512 + s_ * 128) // 256
                        P.op('pe', lambda e: e.matmul(aux[:, 0:32], lhsT=q_[:, m, s_ * 128:(s_ + 1) * 128], rhs=kmb[m][:, :],
                                                      start=True, stop=True), [bq, b_kmb[m]], [b_aux])
                        P.op('dve', lambda e: e.tensor_copy(out=sc[:, :], in_=aux[:, 0:32]), [b_aux], [b_sc])
                        P.op('dve', lambda e: e.memset(sc[:, jb:32], -1e30), [], [b_sc])
                        P.op('dve', lambda e: e.max(out=m8[:, :], in_=sc[:, :]), [b_sc], [b_m8])
                        P.op('dve', lambda e: e.tensor_scalar(out=nm[:, :], in0=sc[:, :], scalar1=m8[:, 2:3], scalar2=None,
                                                              op0=ALU.is_ge), [b_sc, b_m8], [b_nm])
                        P.op('dve', lambda e: e.memset(nm[:, jb:32], 0.0), [], [b_nm])
                        P.op('dve', lambda e: e.memset(nm[:, jb:jb + 1], 1.0), [], [b_nm])
                        P.op('dve', lambda e: e.tensor_scalar(out=nm[:, :], in0=nm[:, :], scalar1=NEGM, scalar2=-NEGM,
                                                              op0=ALU.mult, op1=ALU.add), [b_nm], [b_nm])
                        P.op('pe', lambda e: e.transpose(out=aux2[0:32, 0:128], in_=nm[:, :], identity=ident[:, :]),
                             [b_nm, b_ident], [b_aux2])
                        P.op('act', lambda e: e.copy(out=ng[:, s_ * 128:(s_ + 1) * 128], in_=aux2[0:32, 0:128]), [b_aux2], [bng])
                    tl = causal_tiles(qt, kT[m], b_kT[m], v_sb[m], b_v[m],
                                      mm_fn=lambda kt: (indm[:, kt * 128:(kt + 1) * 128], ng[:, :], [b_indm, bng]))
                    attn_block(P, "m", q_[:, m, :], tl, S_ps, bS, PT, bPT, O_ps, bO, 129, [bq])
                    for s_ in range(4):
                        recip_col(rz, b_rz, O_ps[s_][:, 128:129], [bO[s_]])
                        P.op('dve', lambda e: e.tensor_scalar(out=ost[:, s_, m * 128:(m + 1) * 128], in0=O_ps[s_][:, 0:128],
                                                              scalar1=rz[:, 0:1], scalar2=None, op0=ALU.mult), [bO[s_], b_rz], [bost])
                P.dma(mix[qt * 512:(qt + 1) * 512, 768:1024].rearrange("(s p) c -> p s c", p=128), ost[:, :, :], reads=[bost])
            P.pop_scope()
        if stop_after == 'moba':
            P.emit()
            return nc

        if 'N' in mixers:
            P.push_scope()
            S_ps = [P.ps(f"S{i}", [128, 512]) for i in range(2)]; bS = [Buf(excl=True) for _ in range(2)]
            O_ps = [P.ps(f"O{i}", [128, 512]) for i in range(4)]; bO = [Buf(excl=True) for _ in range(4)]
            aux = P.ps("aux", [128, 512]); b_aux = Buf(excl=True)
            aux2 = P.ps("aux2", [128, 512]); b_aux2 = Buf(excl=True)
            PT = [P.sb(f"PT{i}", [128, 512], BF16) for i in range(3)]; bPT = [Buf() for _ in range(3)]
            kcT = P.sb("kcT", [128, 512], BF16); b_kcT = Buf()
            vc = P.sb("vc", [128, 4, 129], BF16); b_vc = Buf()
            P.push_scope()
            for which in range(2):
                cin = P.sb("cin", [128, S], BF16); b_cin = Buf()
                P.dma(cin[:, :], pT_d[4 + which], reads=[b_pT[4 + which]], writes=[b_cin])
                w1 = P.sb("w1", [128, 32, 128], BF16); b_w1 = Buf()
                P.dma(w1[:, :, :], cw1[which].rearrange("(l d) h -> d l h", d=128), writes=[b_w1], eng='pool')
                w2 = P.sb("w2", [128, 128], BF16); b_w2 = Buf()
                P.dma(w2[:, :], cw2[which], writes=[b_w2], eng='pool')
                posT = P.sb("posT", [128, 32], BF16); b_posT = Buf()
                P.dma(posT[:, :], cpos[which].rearrange("l d -> d l"), writes=[b_posT], eng='pool', allow_slow_non_contiguous=True)
                for l in range(32):
                    P.op('pe', lambda e: e.matmul(aux[:, 0:511], lhsT=w1[:, l, :], rhs=cin[:, l:l + 16 * 510 + 1:16],
                                                  start=(l == 0), stop=(l == 31)), [b_w1, b_cin], [b_aux])
                for l in range(32):
                    P.op('pe', lambda e: e.matmul(aux2[:, 0:1], lhsT=w1[:, l, :], rhs=posT[:, l:l + 1],
                                                  start=(l == 0), stop=(l == 31)), [b_w1, b_posT], [b_aux2])
                pb = P.sb("pb", [128, 1], F32); b_pb = Buf()
                P.op('dve', lambda e: e.tensor_copy(out=pb[:, :], in_=aux2[:, 0:1]), [b_aux2], [b_pb])
                u = P.sb("u", [128, 512], F32); b_u = Buf()
                w_ = P.sb("w_", [128, 512], F32); b_w_ = Buf()
                gel = P.sb("gel", [128, 512], BF16); b_gel = Buf()
                P.op('dve', lambda e: e.memset(gel[:, 511:512], 0.0), [], [b_gel])
                P.op('dve', lambda e: e.tensor_scalar(out=u[:, 0:511], in0=aux[:, 0:511], scalar1=pb[:, 0:1], scalar2=None, op0=ALU.add),
                     [b_aux, b_pb], [b_u])
                P.op('dve', lambda e: e.tensor_tensor(out=w_[:, 0:511], in0=u[:, 0:511], in1=u[:, 0:511], op=ALU.mult), [b_u], [b_w_])
                P.op('dve', lambda e: e.tensor_scalar(out=w_[:, 0:511], in0=w_[:, 0:511], scalar1=0.044715, scalar2=1.0,
                                                      op0=ALU.mult, op1=ALU.add), [b_w_], [b_w_])
                P.op('dve', lambda e: e.tensor_tensor(out=w_[:, 0:511], in0=w_[:, 0:511], in1=u[:, 0:511], op=ALU.mult), [b_w_, b_u], [b_w_])
                P.op('act', lambda e: e.activation(out=w_[:, 0:511], in_=w_[:, 0:511], func=AF.Tanh, scale=0.7978845608028654),
                     [b_w_], [b_w_])
                P.op('dve', lambda e: e.scalar_tensor_tensor(out=w_[:, 0:511], in0=w_[:, 0:511], scalar=1.0, in1=u[:, 0:511],
                                                             op0=ALU.add, op1=ALU.mult), [b_w_, b_u], [b_w_])
                P.op('dve', lambda e: e.tensor_scalar(out=gel[:, 0:511], in0=w_[:, 0:511], scalar1=0.5, scalar2=None, op0=ALU.mult),
                     [b_w_], [b_gel])
                if which == 0:
                    P.op('pe', lambda e: e.matmul(aux[:, 0:512], lhsT=w2[:, :], rhs=gel[:, :], start=True, stop=True), [b_w2, b_gel], [b_aux])
                    P.op('act', lambda e: e.copy(out=kcT[:, :], in_=aux[:, 0:512]), [b_aux], [b_kcT])
                else:
                    for nt in range(4):
                        P.op('pe', lambda e: e.matmul(aux[:, 0:128], lhsT=gel[:, nt * 128:(nt + 1) * 128], rhs=w2[:, :], start=True, stop=True),
                             [b_w2, b_gel], [b_aux])
                        P.op('act', lambda e: e.copy(out=vc[:, nt, 0:128], in_=aux[:, 0:128]), [b_aux], [b_vc])
                    P.op('pool', lambda e: e.memset(vc[:, :, 128:129], 1.0), [], [b_vc])
            P.pop_scope()
            ksT = P.sb("ksT", [128, S], BF16); b_ksT = Buf()
            kwT = P.sb("kwT", [128, S], BF16); b_kwT = Buf()
            P.dma(ksT[:, :], pT_d[6], reads=[b_pT[6]], writes=[b_ksT])
            P.dma(kwT[:, :], pT_d[7], reads=[b_pT[7]], writes=[b_kwT])
            vs = P.sb("vs", [128, 64, 129], BF16); b_vs = Buf()
            vw = P.sb("vw", [128, 64, 129], BF16); b_vw = Buf()
            ld_v(vs, b_vs, 0, 0, 128)
            ld_v(vw, b_vw, 0, 128, 128)
            indb = P.sb("indb", [128, S], BF16); b_indb = Buf()
            P.op('pool', lambda e: e.memset(indb[:, :], 1.0), [], [b_indb])
            P.op('pool', lambda e: e.affine_select(out=indb[:, :], in_=indb[:, :], pattern=[[1, S]], compare_op=ALU.is_ge, fill=0.0,
                                                   base=0, channel_multiplier=-64), [b_indb], [b_indb])
            P.op('pool', lambda e: e.affine_select(out=indb[:, :], in_=indb[:, :], pattern=[[-1, S]], compare_op=ALU.is_ge, fill=0.0,
                                                   base=63, channel_multiplier=64), [b_indb], [b_indb])
            fbig = P.sb("fbig", [128, 256], F32); b_fbig = Buf()
            P.dma(fbig[:, :], c_fbig, writes=[b_fbig])
            qsb = [P.sb(f"nq{i}", [128, 4, 512], BF16) for i in range(2)]; b_q = [Buf(), Buf()]
            gsb = [P.sb(f"ng{i}", [128, 4, 12], F32) for i in range(2)]; b_g = [Buf(), Buf()]
            E = [P.sb(f"E{i}", [128, 512], F32) for i in range(2)]; b_E = [Buf(), Buf()]
            pg = P.sb("pg", [128, 512], F32); b_pg = Buf()
            imp = P.sb("imp", [128, 128], F32); b_imp = Buf()
            wk = P.sb("wk", [128, 128], F32); b_wk = Buf()
            m8 = P.sb("m8", [128, 16], F32); b_m8 = Buf()
            rz = P.sb("rz", [128, 4], F32); b_rz = Buf()
            negT = P.sb("negT", [128, 512], BF16); b_negT = Buf()
            outst = [P.sb(f"nout{i}", [128, 4, 512], F32) for i in range(2)]; b_out = [Buf(), Buf()]
            for qt in qts:
                q_ = qsb[qt % 2]; bq = b_q[qt % 2]
                g_ = gsb[qt % 2]; bg = b_g[qt % 2]
                for g in range(4):
                    P.dma(q_[:, g, :], pT_d[g, :, qt * 512:(qt + 1) * 512], reads=[b_pT[g]], writes=[bq])
                P.dma(g_[:, :, :], gate_d[qt * 512:(qt + 1) * 512, :].rearrange("(s p) c -> p s c", p=128), reads=[b_gate], writes=[bg])
                ost = outst[qt % 2]; bost = b_out[qt % 2]
                for s_ in range(4):
                    q0 = qt * 512 + s_ * 128
                    for g in range(4):
                        e_ = E[g % 2]; be = b_E[g % 2]
                        P.op('pe', lambda e: e.matmul(aux[:, :], lhsT=q_[:, g, s_ * 128:(s_ + 1) * 128], rhs=kcT[:, :], start=True, stop=True),
                             [bq, b_kcT], [b_aux])
                        P.op('act', lambda e: e.activation(out=e_[:, :], in_=aux[:, :], func=AF.Exp, scale=SCALE), [b_aux], [be])
                        P.op('pool', lambda e: e.affine_select(out=e_[:, :], in_=e_[:, :], pattern=[[-16, 512]], compare_op=ALU.is_ge,
                                                               fill=0.0, base=q0 - 31, channel_multiplier=1), [be], [be])
                        P.op('dve', lambda e: e.reduce_sum(out=rz[:, 0:1], in_=e_[:, :], axis=AX.X), [be], [b_rz])
                        recip_col(rz, b_rz, rz[:, 0:1], [b_rz])
                        if g == 0:
                            P.op('dve', lambda e: e.tensor_scalar(out=pg[:, :], in0=e_[:, :], scalar1=rz[:, 0:1], scalar2=None, op0=ALU.mult),
                                 [be, b_rz], [b_pg])
                        else:
                            P.op('dve', lambda e: e.scalar_tensor_tensor(out=pg[:, :], in0=e_[:, :], scalar=rz[:, 0:1], in1=pg[:, :],
                                                                         op0=ALU.mult, op1=ALU.add), [be, b_rz, b_pg], [b_pg])
                    P.op('dve', lambda e: e.tensor_reduce(out=imp[:, :], in_=pg[:, :].rearrange("p (b k) -> p b k", k=4), axis=AX.X, op=ALU.add),
                         [b_pg], [b_imp])
                    P.op('dve', lambda e: e.tensor_tensor(out=imp[:, 1:128], in0=imp[:, 1:128], in1=pg[:, 3:508:4], op=ALU.add),
                         [b_imp, b_pg], [b_imp])
                    st0 = 128 - q0 // 64
                    P.op('dve', lambda e: e.tensor_tensor(out=imp[:, :], in0=imp[:, :], in1=fbig[:, st0:st0 + 128], op=ALU.add),
                         [b_imp, b_fbig], [b_imp])
                    P.op('dve', lambda e: e.tensor_scalar(out=imp[:, 0:1], in0=imp[:, 0:1], scalar1=100.0, scalar2=None, op0=ALU.add),
                         [b_imp], [b_imp])
                    P.op('dve', lambda e: e.max(out=m8[:, 0:8], in_=imp[:, :]), [b_imp], [b_m8])
                    P.op('dve', lambda e: e.match_replace(out=wk[:, :], in_to_replace=m8[:, 0:8], in_values=imp[:, :], imm_value=-1e30),
                         [b_imp, b_m8], [b_wk])
                    P.op('dve', lambda e: e.max(out=m8[:, 8:16], in_=wk[:, :]), [b_wk], [b_m8])
                    P.op('dve', lambda e: e.tensor_scalar(out=wk[:, :], in0=imp[:, :], scalar1=m8[:, 15:16], scalar2=NEGM,
                                                          op0=ALU.is_ge, op1=ALU.mult), [b_imp, b_m8], [b_wk])
                    P.op('dve', lambda e: e.tensor_scalar(out=wk[:, :], in0=wk[:, :], scalar1=-NEGM, scalar2=None, op0=ALU.add),
                         [b_wk], [b_wk])
                    P.op('pe', lambda e: e.transpose(out=aux2[:, 0:128], in_=wk[:, :], identity=ident[:, :]), [b_wk, b_ident], [b_aux2])
                    P.op('act', lambda e: e.copy(out=negT[:, s_ * 128:(s_ + 1) * 128], in_=aux2[:, 0:128]), [b_aux2], [b_negT])
                for g in range(4):
                    qg = q_[:, g, :]
                    for br in range(3):
                        if br == 0:
                            tl = []
                            for nt in range(4):
                                if qt * 512 + 511 < 2048 * nt + 31:
                                    continue
                                full = qt * 512 >= 16 * (nt * 128 + 127) + 31
                                tl.append(dict(kT=kcT[:, nt * 128:(nt + 1) * 128], v=vc[:, nt, :], reads=[b_kcT, b_vc],
                                               sel=None if full else dict(pattern=[[1, 512]], cm=-16, base=qt * 512 - 2048 * nt - 31),
                                               subs=[x_ for x_ in range(4) if qt * 512 + x_ * 128 + 127 >= 2048 * nt + 31]))
                        elif br == 1:
                            tl = causal_tiles(qt, ksT, b_ksT, vs, b_vs,
                                              mm_fn=lambda kt: (indb[:, kt * 128:(kt + 1) * 128], negT[:, :], [b_indb, b_negT]))
                        else:
                            tl = []
                            for a in range(8):
                                kt = 4 * qt - 4 + a
                                if kt < 0:
                                    continue
                                if a < 4:
                                    sel = dict(pattern=[[-1, 512]], cm=1, base=kt * 128 - qt * 512 + 511)
                                    subs = [x_ for x_ in range(4) if a >= x_]
                                else:
                                    sel = _sel_causal(qt * 512, kt * 128)
                                    subs = [x_ for x_ in range(4) if a - 4 <= x_]
                                tl.append(dict(kT=kwT[:, kt * 128:(kt + 1) * 128], v=vw[:, kt, :], reads=[b_kwT, b_vw], sel=sel, subs=subs))
                        attn_block(P, "n", qg, tl, S_ps, bS, PT, bPT, O_ps, bO, 129, [bq])
                        for s_ in range(4):
                            recip_col(rz, b_rz, O_ps[s_][:, 128:129], [bO[s_]])
                            P.op('dve', lambda e: e.tensor_tensor(out=rz[:, 1:2], in0=rz[:, 0:1], in1=g_[:, s_, 3 * g + br:3 * g + br + 1],
                                                                  op=ALU.mult), [b_rz, bg], [b_rz])
                            dst = ost[:, s_, g * 128:(g + 1) * 128]
                            if br == 0:
                                P.op('dve', lambda e: e.tensor_scalar(out=dst, in0=O_ps[s_][:, 0:128], scalar1=rz[:, 1:2], scalar2=None,
                                                                      op0=ALU.mult), [bO[s_], b_rz], [bost])
                            else:
                                P.op('dve', lambda e: e.scalar_tensor_tensor(out=dst, in0=O_ps[s_][:, 0:128], scalar=rz[:, 1:2], in1=dst,
                                                                             op0=ALU.mult, op1=ALU.add), [bO[s_], b_rz, bost], [bost])
                P.dma(mix[qt * 512:(qt + 1) * 512, 0:512].rearrange("(s p) c -> p s c", p=128), ost[:, :, :], reads=[bost])
            P.pop_scope()
        P.emit()
        return nc


ALPHA = 4 ** 0.25
TOK = 2048


def build_ffn(n_pass=4, n_exp=32, debug=False):
    nc = bass.Bass("TRN2", target_bir_lowering=False)
    xres = nc.dram_tensor("xres", [TOK, 4096], F32, kind="ExternalInput").ap()
    mixT = nc.dram_tensor("mixT", [4096, TOK], F32, kind="ExternalInput").ap()
    wout = nc.dram_tensor("wout", [4096, 4096], F32, kind="ExternalInput").ap()
    lnp = nc.dram_tensor("lnp", [4, 4096], F32, kind="ExternalInput").ap()
    wr = nc.dram_tensor("wr", [4096, 32], F32, kind="ExternalInput").ap()
    br = nc.dram_tensor("br", [1, 32], F32, kind="ExternalInput").ap()
    wgu = nc.dram_tensor("wgu", [32, 4, 128, 32 * 256], F32, kind="ExternalInput").ap()
    bgu = nc.dram_tensor("bgu", [128, 256], F32, kind="ExternalInput").ap()
    wdn = nc.dram_tensor("wdn", [32, 4, 128, 4 * 1024], F32, kind="ExternalInput").ap()
    bdn = nc.dram_tensor("bdn", [32, 4096], F32, kind="ExternalInput").ap()
    y = nc.dram_tensor("y", [TOK, 4096], F32, kind="ExternalOutput").ap()
    dbg = nc.dram_tensor("dbg", [TOK, 32], F32, kind="ExternalOutput").ap() if debug else None
    with ExitStack() as st:
        P = Prog(nc, st)
        ident = P.sb("ident", [128, 128], F32); b_ident = Buf()
        P.op('pool', lambda e: e.memset(ident[:, :], 1.0), [], [b_ident])
        P.op('pool', lambda e: e.affine_select(out=ident[:, :], in_=ident[:, :], pattern=[[-1, 128]],
                                               compare_op=ALU.is_equal, fill=0.0, base=0, channel_multiplier=1),
             [b_ident], [b_ident])
        bgs = P.sb("bgs", [128, 256], F32); b_bgs = Buf()
        P.dma(bgs[:, :], bgu, writes=[b_bgs])
        brs = P.sb("brs", [128, 32], F32); b_brs = Buf()
        P.dma(brs[:, :], br.to_broadcast([128, 32]), writes=[b_brs])
        wrs = P.sb("wrs", [128, 32, 32], F32); b_wrs = Buf()
        P.dma(wrs[:, :, :], wr.rearrange("(k p) e -> p k e", p=128), writes=[b_wrs])
        acc = P.sb("acc", [128, 4, 4096], F32); b_acc = [Buf() for _ in range(4)]
        x1T = P.sb("x1T", [128, 32, 512], BF16); b_x1T = Buf()
        G = P.sb("G", [128, 4, 32], F32); b_G = Buf()
        moutv = mixT.rearrange("(k p) t -> p k t", p=128)
        woutv = wout.rearrange("(k p) n -> p k n", p=128)

        def layer_norm(sub, gi, stat, b_stat, sq, b_sq, gb, b_gb):
            a_ = acc[:, sub, :]
            ba = b_acc[sub]
            P.op('dve', lambda e: e.reduce_sum(out=stat[:, 0:1], in_=a_, axis=AX.X), [ba], [b_stat])
            P.op('dve', lambda e: e.tensor_scalar(out=stat[:, 0:1], in0=stat[:, 0:1], scalar1=-1.0 / 4096, scalar2=None, op0=ALU.mult),
                 [b_stat], [b_stat])
            P.op('dve', lambda e: e.tensor_scalar(out=a_, in0=a_, scalar1=stat[:, 0:1], scalar2=None, op0=ALU.add), [ba, b_stat], [ba])
            P.op('act', lambda e: e.activation(out=sq[:, :], in_=a_, func=AF.Square, accum_out=stat[:, 1:2]), [ba, b_stat], [b_sq, b_stat])
            P.op('dve', lambda e: e.tensor_scalar(out=stat[:, 1:2], in0=stat[:, 1:2], scalar1=1.0 / 4096, scalar2=1e-5,
                                                  op0=ALU.mult, op1=ALU.add), [b_stat], [b_stat])
            P.op('act', lambda e: e.sqrt(out=stat[:, 1:2], in_=stat[:, 1:2]), [b_stat], [b_stat])
            P.op('dve', lambda e: e.reciprocal(out=stat[:, 1:2], in_=stat[:, 1:2]), [b_stat], [b_stat])
            for ch in range(4):
                cs_ = slice(ch * 1024, (ch + 1) * 1024)
                P.op('dve', lambda e: e.scalar_tensor_tensor(out=acc[:, sub, cs_], in0=acc[:, sub, cs_], scalar=stat[:, 1:2],
                                                             in1=gb[:, 0, cs_], op0=ALU.mult, op1=ALU.mult), [ba, b_stat, b_gb], [ba])
                P.op('pool', lambda e: e.tensor_tensor(out=acc[:, sub, cs_], in0=acc[:, sub, cs_], in1=gb[:, 1, cs_], op=ALU.add),
                     [ba, b_gb], [ba])

        for ps_i in range(n_pass):
            t0 = ps_i * 512
            P.push_scope()
            mT = P.sb("mT", [128, 32, 512], BF16); b_mT = Buf()
            for q in range(2):
                P.dma(mT[:, q * 16:(q + 1) * 16, :], moutv[:, q * 16:(q + 1) * 16, t0:t0 + 512], writes=[b_mT], eng='pool')
            wo = [P.sb(f"wo{i}", [128, 32, 256], BF16) for i in range(2)]; b_wo = [Buf(), Buf()]
            xr = [P.sb(f"xr{i}", [128, 4, 256], F32) for i in range(2)]; b_xr = [Buf(), Buf()]
            pso = [P.ps(f"pso{i}", [128, 512]) for i in range(2)]; b_pso = [Buf(excl=True), Buf(excl=True)]
            io = 0
            for cc in range(16):
                w_ = wo[cc % 2]; bw = b_wo[cc % 2]
                P.dma(w_[:, :, :], woutv[:, :, cc * 256:(cc + 1) * 256], writes=[bw], eng='pool')
                x_ = xr[cc % 2]; bx = b_xr[cc % 2]
                P.dma(x_[:, :, :], xres[t0:t0 + 512, cc * 256:(cc + 1) * 256].rearrange("(s p) c -> p s c", p=128), writes=[bx])
                for sub in range(4):
                    po = pso[io % 2]; bpo = b_pso[io % 2]
                    for k in range(32):
                        P.op('pe', lambda e: e.matmul(po[:, 0:256], lhsT=mT[:, k, sub * 128:(sub + 1) * 128], rhs=w_[:, k, :],
                                                      start=(k == 0), stop=(k == 31)), [b_mT, bw], [bpo])
                    P.op('dve', lambda e: e.scalar_tensor_tensor(out=acc[:, sub, cc * 256:(cc + 1) * 256], in0=x_[:, sub, :], scalar=ALPHA,
                                                                 in1=po[:, 0:256], op0=ALU.mult, op1=ALU.add), [bx, bpo], [b_acc[sub]])
                    io += 1
            P.pop_scope()
            P.push_scope()
            gb = P.sb("gb", [128, 2, 4096], F32); b_gb = Buf()
            P.dma(gb[:, 0, :], lnp[0:1, :].to_broadcast([128, 4096]), writes=[b_gb])
            P.dma(gb[:, 1, :], lnp[1:2, :].to_broadcast([128, 4096]), writes=[b_gb])
            ptr = [P.ps(f"ptr{i}", [128, 512]) for i in range(2)]; b_ptr = [Buf(excl=True), Buf(excl=True)]
            plg = P.ps("plg", [128, 512]); b_plg = Buf(excl=True)
            pbd = [P.ps(f"pbd{i}", [128, 512]) for i in range(2)]; b_pbd = [Buf(excl=True), Buf(excl=True)]
            stat = P.sb("stat", [128, 2], F32); b_stat = Buf()
            sq = P.sb("sq", [128, 4096], BF16); b_sq = Buf()
            xTf = P.sb("xTf", [128, 32, 128], F32); b_xTf = Buf()
            lg = P.sb("lg", [128, 32], F32); b_lg = Buf()
            m8 = P.sb("m8", [128, 8], F32); b_m8 = Buf()
            ex = P.sb("ex", [128, 32], F32); b_ex = Buf()
            GT = P.sb("GT", [32, 128], F32); b_GT = Buf()
            bds = P.sb("bds", [32, 4096], F32); b_bds = Buf()
            P.dma(bds[:, :], bdn, writes=[b_bds])
            for sub in range(4):
                layer_norm(sub, 0, stat, b_stat, sq, b_sq, gb, b_gb)
                for k4 in range(8):
                    pt_ = ptr[k4 % 2]; bpt = b_ptr[k4 % 2]
                    for i4 in range(4):
                        k = k4 * 4 + i4
                        P.op('pe', lambda e: e.transpose(out=pt_[:, i4 * 128:(i4 + 1) * 128], in_=acc[:, sub, k * 128:(k + 1) * 128],
                                                         identity=ident[:, :]), [b_acc[sub], b_ident], [bpt])
                    P.op('act', lambda e: e.copy(out=x1T[:, k4 * 4:(k4 + 1) * 4, sub * 128:(sub + 1) * 128],
                                                 in_=pt_[:, :].rearrange("p (a b) -> p a b", b=128)), [bpt], [b_x1T])
                    P.op('dve', lambda e: e.tensor_copy(out=xTf[:, k4 * 4:(k4 + 1) * 4, :], in_=pt_[:, :].rearrange("p (a b) -> p a b", b=128)),
                         [bpt], [b_xTf])
                for k in range(32):
                    P.op('pe', lambda e: e.matmul(plg[:, 0:32], lhsT=xTf[:, k, :], rhs=wrs[:, k, :], start=(k == 0), stop=(k == 31)),
                         [b_xTf, b_wrs], [b_plg])
                P.op('dve', lambda e: e.tensor_tensor(out=lg[:, :], in0=plg[:, 0:32], in1=brs[:, :], op=ALU.add), [b_plg, b_brs], [b_lg])
                P.op('dve', lambda e: e.max(out=m8[:, :], in_=lg[:, :]), [b_lg], [b_m8])
                P.op('dve', lambda e: e.tensor_scalar(out=ex[:, :], in0=lg[:, :], scalar1=m8[:, 0:1], scalar2=None, op0=ALU.subtract),
                     [b_lg, b_m8], [b_ex])
                P.op('act', lambda e: e.activation(out=ex[:, :], in_=ex[:, :], func=AF.Exp), [b_ex], [b_ex])
                P.op('dve', lambda e: e.scalar_tensor_tensor(out=ex[:, :], in0=lg[:, :], scalar=m8[:, 3:4], in1=ex[:, :],
                                                             op0=ALU.is_ge, op1=ALU.mult), [b_lg, b_m8, b_ex], [b_ex])
                P.op('dve', lambda e: e.reduce_sum(out=m8[:, 4:5], in_=ex[:, :], axis=AX.X), [b_ex], [b_m8])
                P.op('dve', lambda e: e.reciprocal(out=m8[:, 4:5], in_=m8[:, 4:5]), [b_m8], [b_m8])
                P.op('dve', lambda e: e.tensor_scalar(out=G[:, sub, :], in0=ex[:, :], scalar1=m8[:, 4:5], scalar2=None, op0=ALU.mult),
                     [b_ex, b_m8], [b_G])
                if dbg is not None:
                    P.dma(dbg[t0 + sub * 128:t0 + (sub + 1) * 128, :], G[:, sub, :], reads=[b_G])
                P.op('pe', lambda e: e.transpose(out=plg[0:32, 128:256], in_=G[:, sub, :], identity=ident[:, :]), [b_G, b_ident], [b_plg])
                P.op('dve', lambda e: e.tensor_copy(out=GT[:, :], in_=plg[0:32, 128:256]), [b_plg], [b_GT])
                for ch in range(8):
                    pb_ = pbd[ch % 2]; bpb = b_pbd[ch % 2]
                    P.op('pe', lambda e: e.matmul(pb_[:, :], lhsT=GT[:, :], rhs=bds[:, ch * 512:(ch + 1) * 512], start=True, stop=True),
                         [b_GT, b_bds], [bpb])
                    P.op('dve', lambda e: e.scalar_tensor_tensor(out=acc[:, sub, ch * 512:(ch + 1) * 512], in0=acc[:, sub, ch * 512:(ch + 1) * 512],
                                                                 scalar=ALPHA, in1=pb_[:, :], op0=ALU.mult, op1=ALU.add),
                         [b_acc[sub], bpb], [b_acc[sub]])
            P.pop_scope()
            P.push_scope()
            wg = [P.sb(f"wg{i}", [128, 32, 256], BF16) for i in range(2)]; b_wg = [Buf(), Buf()]
            wd = [P.sb(f"wd{i}", [128, 4, 1024], BF16) for i in range(2)]; b_wd = [Buf(), Buf()]
            pg_ = [P.ps(f"pg{i}", [128, 512]) for i in range(4)]; b_pg = [Buf(excl=True) for _ in range(4)]
            pd_ = [P.ps(f"pd{i}", [128, 512]) for i in range(3)]; b_pd = [Buf(excl=True) for _ in range(3)]
            actT = [P.sb(f"actT{i}", [128, 4, 512], BF16) for i in range(2)]; b_act = [Buf(), Buf()]
            gl = [P.sb(f"gl{i}", [128, 512], F32) for i in range(2)]; b_gl = [Buf(), Buf()]
            sg_ = [P.sb(f"sg{i}", [128, 512], F32) for i in range(2)]; b_sg = [Buf(), Buf()]
            ln_ = [P.sb(f"ln{i}", [128, 512], F32) for i in range(2)]; b_ln = [Buf(), Buf()]
            ig = 0
            idn = 0
            iw = 0
            for ex_i in range(n_exp):
                a_ = actT[ex_i % 2]; ba = b_act[ex_i % 2]
                for f in range(4):
                    w_ = wg[ig % 2]; bw = b_wg[ig % 2]
                    P.dma(w_[:, :, :], wgu[ex_i, f].rearrange("p (k c) -> p k c", c=256), writes=[bw], eng='pool')
                    pgl = pg_[(ig % 2) * 2]; bpgl = b_pg[(ig % 2) * 2]
                    pln = pg_[(ig % 2) * 2 + 1]; bpln = b_pg[(ig % 2) * 2 + 1]
                    for k in range(32):
                        P.op('pe', lambda e: e.matmul(pgl[:, :], lhsT=w_[:, k, 0:128], rhs=x1T[:, k, :], start=(k == 0), stop=(k == 31)),
                             [bw, b_x1T], [bpgl])
                    for k in range(32):
                        P.op('pe', lambda e: e.matmul(pln[:, :], lhsT=w_[:, k, 128:256], rhs=x1T[:, k, :], start=(k == 0), stop=(k == 31)),
                             [bw, b_x1T], [bpln])
                    g_ = gl[ig % 2]; bg = b_gl[ig % 2]
                    s_ = sg_[ig % 2]; bs = b_sg[ig % 2]
                    l_ = ln_[ig % 2]; bl = b_ln[ig % 2]
                    bc = (ex_i * 4 + f) * 2
                    P.op('dve', lambda e: e.tensor_scalar(out=g_[:, :], in0=pgl[:, :], scalar1=bgs[:, bc:bc + 1], scalar2=7.0,
                                                          op0=ALU.add, op1=ALU.min), [bpgl, b_bgs], [bg])
                    P.op('act', lambda e: e.activation(out=s_[:, :], in_=g_[:, :], func=AF.Sigmoid, scale=1.702), [bg], [bs])
                    P.op('dve', lambda e: e.tensor_scalar(out=l_[:, :], in0=pln[:, :], scalar1=bgs[:, bc + 1:bc + 2], scalar2=7.0,
                                                          op0=ALU.add, op1=ALU.min), [bpln, b_bgs], [bl])
                    P.op('pool', lambda e: e.tensor_scalar(out=l_[:, :], in0=l_[:, :], scalar1=-7.0, scalar2=1.0,
                                                           op0=ALU.max, op1=ALU.add), [bl], [bl])
                    P.op('pool', lambda e: e.tensor_tensor(out=g_[:, :], in0=g_[:, :], in1=s_[:, :], op=ALU.mult), [bg, bs], [bg])
                    P.op('pool', lambda e: e.tensor_tensor(out=a_[:, f, :], in0=g_[:, :], in1=l_[:, :], op=ALU.mult), [bg, bl], [ba])
                    ig += 1
                for cc in range(4):
                    d_ = wd[iw % 2]; bd = b_wd[iw % 2]
                    P.dma(d_[:, :, :], wdn[ex_i, cc].rearrange("p (f c) -> p f c", c=1024), writes=[bd], eng='pool')
                    iw += 1
                    for sub in range(4):
                        for hf in range(2):
                            pp_ = pd_[idn % 3]; bpp = b_pd[idn % 3]
                            for ft in range(4):
                                P.op('pe', lambda e: e.matmul(pp_[:, :], lhsT=a_[:, ft, sub * 128:(sub + 1) * 128],
                                                              rhs=d_[:, ft, hf * 512:(hf + 1) * 512], start=(ft == 0), stop=(ft == 3)),
                                     [ba, bd], [bpp])
                            c0 = cc * 1024 + hf * 512
                            P.op('dve', lambda e: e.scalar_tensor_tensor(out=acc[:, sub, c0:c0 + 512], in0=pp_[:, :],
                                                                         scalar=G[:, sub, ex_i:ex_i + 1], in1=acc[:, sub, c0:c0 + 512],
                                                                         op0=ALU.mult, op1=ALU.add), [bpp, b_G, b_acc[sub]], [b_acc[sub]])
                            idn += 1
            P.pop_scope()
            P.push_scope()
            gb = P.sb("gb2", [128, 2, 4096], F32); b_gb = Buf()
            P.dma(gb[:, 0, :], lnp[2:3, :].to_broadcast([128, 4096]), writes=[b_gb])
            P.dma(gb[:, 1, :], lnp[3:4, :].to_broadcast([128, 4096]), writes=[b_gb])
            stat = P.sb("stat2", [128, 2], F32); b_stat = Buf()
            sq = P.sb("sq2", [128, 4096], BF16); b_sq = Buf()
            for sub in range(4):
                layer_norm(sub, 2, stat, b_stat, sq, b_sq, gb, b_gb)
                P.dma(y[t0 + sub * 128:t0 + (sub + 1) * 128, :], acc[:, sub, :], reads=[b_acc[sub]])
            P.pop_scope()
        P.emit()
    return nc


def prep_ffn_weights(w_out, ln1_g, ln1_b, ln2_g, ln2_b, w_router, b_router, w_gate_up, b_gate_up, w_down, b_down):
    wg = w_gate_up.reshape(32, 32, 128, 4, 128, 2)
    wg = np.ascontiguousarray(wg.transpose(0, 3, 2, 1, 5, 4)).reshape(32, 4, 128, 32 * 256)
    bg = b_gate_up.reshape(32, 4, 128, 2)
    bg = np.ascontiguousarray(bg.transpose(2, 0, 1, 3)).reshape(128, 256)
    wd = w_down.reshape(32, 4, 128, 4, 1024)
    wd = np.ascontiguousarray(wd.transpose(0, 3, 2, 1, 4)).reshape(32, 4, 128, 4 * 1024)
    return dict(wout=np.ascontiguousarray(w_out), lnp=np.ascontiguousarray(np.stack([ln1_g, ln1_b, ln2_g, ln2_b])),
                wr=np.ascontiguousarray(w_router), br=np.ascontiguousarray(b_router.reshape(1, 32)),
                wgu=wg, bgu=bg, wdn=wd, bdn=np.ascontiguousarray(b_down))


_COLS = [2048, 512, 512, 512, 512, 512, 512, 48, 1024, 1024, 1024, 1024, 1024, 1024]
_OFF = [0]
for _c in _COLS:
    _OFF.append(_OFF[-1] + _c)


def mixer_cols(j):
    r = lambda seg, a, n: list(range(_OFF[seg] + a, _OFF[seg] + a + n))
    cols = []
    cols += r(0, 4 * j * 128, 512)
    for seg in (1, 2, 3, 5):
        cols += r(seg, j * 128, 128)
    cols += r(4, j * 128, 128)
    cols += r(6, j * 128, 128)
    cols += r(7, j * 12, 12)
    cols += r(8, 2 * j * 128, 256)
    cols += r(9, 2 * j * 128, 256)
    cols += r(10, j * 256, 256)
    cols += r(11, 2 * j * 128, 256)
    cols += r(12, 2 * j * 128, 256)
    cols += r(13, 2 * j * 128, 256)
    return np.array(cols)


def mixer_consts(layer):
    import math
    invf = (1.0 / (500000.0 ** (np.arange(0, 32, 2, dtype=np.float32) / 32))).astype(np.float32)
    c_invf = np.concatenate([invf, invf]).reshape(32, 1).astype(np.float32)
    sw = np.zeros((128, 32), np.float32)
    for m in range(16):
        sw[m + 16, m] = -1.0
        sw[m, m + 16] = 1.0
    li = 0.8 - 0.6 * math.exp(-0.3 * layer)
    c_lam = np.array([[li, 1.0 - li]], np.float32)
    fb = np.zeros((128, 256), np.float32)
    for p in range(128):
        cr = p // 64
        for c in range(256):
            rel = c - 128
            if rel > cr:
                fb[p, c] = -1e30
            elif rel == cr or rel == cr - 1:
                fb[p, c] = 100.0
    return dict(c_invf=c_invf, c_sw=sw, c_lam=c_lam, c_fbig=fb)


_PROGS = {}


def _get_prog(name):
    if name not in _PROGS:
        _PROGS[name] = build_mixer() if name == 'mixer' else build_ffn()
    return _PROGS[name]


def kernel(x, positions, w_in, nsa_cmp_pos, nsa_cmp_w1, nsa_cmp_w2, diff_lambda, diff_subln_g,
           w_out, ln1_g, ln1_b, w_router, b_router, w_gate_up, b_gate_up, w_down, b_down, ln2_g, ln2_b):
    x = np.asarray(x, np.float32)
    positions = np.asarray(positions, np.int32)
    B, S, D = x.shape
    cores = list(range(8))
    for layer in range(2):
        ncA = _get_prog('mixer')
        consts = mixer_consts(layer)
        xTs = [np.ascontiguousarray(x[b].T) for b in range(B)]
        wAs = [np.ascontiguousarray(np.asarray(w_in[layer])[:, mixer_cols(j)]) for j in range(4)]
        in_maps = []
        for c in cores:
            b, j = c // 4, c % 4
            in_maps.append(dict(xT=xTs[b], wA=wAs[j], pos=np.ascontiguousarray(positions[b:b + 1]),
                                cpos=np.ascontiguousarray(nsa_cmp_pos[layer]), cw1=np.ascontiguousarray(nsa_cmp_w1[layer]),
                                cw2=np.ascontiguousarray(nsa_cmp_w2[layer]),
                                dlam=np.ascontiguousarray(np.asarray(diff_lambda[layer]).reshape(1, 512)),
                                subg=np.ascontiguousarray(np.asarray(diff_subln_g[layer]).reshape(1, 256)), **consts))
        resA = run_bass_kernel_spmd(ncA, in_maps, core_ids=cores)
        del xTs, wAs, in_maps
        mix_full = np.empty((B, S, 4096), np.float32)
        for c in cores:
            b, j = c // 4, c % 4
            m = resA.results[c]["mix"]
            mix_full[b, :, j * 512:(j + 1) * 512] = m[:, 0:512]
            mix_full[b, :, 2048 + j * 256:2048 + (j + 1) * 256] = m[:, 512:768]
            mix_full[b, :, 3072 + j * 256:3072 + (j + 1) * 256] = m[:, 768:1024]
        ncB = _get_prog('ffn')
        W = prep_ffn_weights(np.asarray(w_out[layer]), np.asarray(ln1_g[layer]), np.asarray(ln1_b[layer]), np.asarray(ln2_g[layer]),
                             np.asarray(ln2_b[layer]), np.asarray(w_router[layer]), np.asarray(b_router[layer]),
                             np.asarray(w_gate_up[layer]), np.asarray(b_gate_up[layer]), np.asarray(w_down[layer]),
                             np.asarray(b_down[layer]))
        in_maps = []
        for c in cores:
            b, s0 = c // 4, (c % 4) * TOK
            in_maps.append(dict(xres=np.ascontiguousarray(x[b, s0:s0 + TOK]), mixT=np.ascontiguousarray(mix_full[b, s0:s0 + TOK].T), **W))
        resB = run_bass_kernel_spmd(ncB, in_maps, core_ids=cores)
        del in_maps, W, mix_full
        xn = np.empty_like(x)
        for c in cores:
            b, s0 = c // 4, (c % 4) * TOK
            xn[b, s0:s0 + TOK] = resB.results[c]["y"]
        x = xn
    return x
```

```python
import numpy as np
import os
SKIP = os.environ.get('SKIP', '')
import concourse.bass as bass
import concourse.mybir as mybir
from concourse.bass_utils import run_bass_kernel_spmd
from contextlib import ExitStack

F32 = mybir.dt.float32
BF16 = mybir.dt.bfloat16
I32 = mybir.dt.int32
ALU = mybir.AluOpType
AF = mybir.ActivationFunctionType
AX = mybir.AxisListType

ENGS = ('pe', 'dve', 'act', 'pool', 'sp')


class Buf:
    __slots__ = ('name', 'w', 'r', 'excl')

    def __init__(self, name='', excl=False):
        self.name = name
        self.excl = excl
        self.w = None
        self.r = {}


class Prog:
    EPOCH = 16000
    NDMA = 32

    def __init__(self, nc, stack):
        self.nc = nc
        self.stack = stack
        self.ops = {e: [] for e in ENGS}
        self.cnt = {e: 0 for e in ENGS}
        self.esems = {e: [] for e in ENGS}
        self.waited = {e: {} for e in ENGS}
        self.dma_sems = [self.new_sem(f"dq{i}") for i in range(self.NDMA)]
        self.dma_val = [0] * self.NDMA
        self.dma_pool = {'sp': list(range(0, 20)), 'pool': list(range(20, 32)), 'act': []}
        self.dma_next = {'sp': 0, 'pool': 0}
        self.nwaits = 0
        self.alloc_stack = stack
        self.eobj = {'pe': nc.tensor, 'dve': nc.vector, 'act': nc.scalar, 'pool': nc.gpsimd, 'sp': nc.sync}

    def new_sem(self, name):
        return self.stack.enter_context(self.nc.semaphore(name))

    def sb(self, name, shape, dt):
        self._uid = getattr(self, '_uid', 0) + 1
        name = f"{name}_{self._uid}"
        return self.alloc_stack.enter_context(self.nc.sbuf_tensor(name, list(shape), dt))

    def ps(self, name, shape, dt=F32):
        self._uid = getattr(self, '_uid', 0) + 1
        name = f"{name}_{self._uid}"
        return self.alloc_stack.enter_context(self.nc.psum_tensor(name, list(shape), dt))

    def op(self, eng, fn, reads=(), writes=(), dma=False):
        xr = [b for b in reads if b.excl]
        if xr:
            reads = [b for b in reads if not b.excl]
            writes = list(writes) + [b for b in xr if b not in writes]
        deps = []
        for b in reads:
            if b.w is not None:
                deps.append(b.w)
        for b in writes:
            if b.w is not None:
                deps.append(b.w)
            for ev in b.r.values():
                deps.append(ev)
        if dma:
            pl = self.dma_pool[eng]
            i = pl[self.dma_next[eng] % len(pl)]
            self.dma_next[eng] += 1
            sem = self.dma_sems[i]
            prev = self.dma_val[i]
            if prev > 0:
                deps.append((sem, prev, 'dma'))
            self.dma_val[i] = prev + 16
            ev = (sem, prev + 16, 'dma')
            inc = 16
        else:
            k = self.cnt[eng]
            ep = k // self.EPOCH
            if ep >= len(self.esems[eng]):
                self.esems[eng].append(self.new_sem(f"{eng}{ep}"))
            sem = self.esems[eng][ep]
            self.cnt[eng] = k + 1
            ev = (sem, k - ep * self.EPOCH + 1, eng)
            inc = 1
        wd = self.waited[eng]
        waits = []
        for (s, v, e) in deps:
            if e == 'pe' and eng == 'pe' and not dma:
                continue
            if wd.get(s.num, 0) < v:
                wd[s.num] = v
                waits.append((s, v))
        self.nwaits += len(waits)
        eo = self.eobj[eng]
        for (s, v) in waits:
            eo.wait_ge(s, v)
        fn(eo).then_inc(sem, inc)
        for b in reads:
            old = b.r.get(sem.num)
            if old is None or old[1] < ev[1]:
                b.r[sem.num] = ev
        for b in writes:
            b.w = ev
            b.r = {}
        return ev


    def barrier(self):
        evs = []
        for e in ENGS:
            k = self.cnt[e]
            if k == 0:
                continue
            ep = (k - 1) // self.EPOCH
            evs.append((self.esems[e][ep], k - ep * self.EPOCH))
        for i, s in enumerate(self.dma_sems):
            if self.dma_val[i] > 0:
                evs.append((s, self.dma_val[i]))
        for e in ENGS:
            wd = self.waited[e]
            waits = []
            for (s, v) in evs:
                if wd.get(s.num, 0) < v:
                    wd[s.num] = v
                    waits.append((s, v))
            for (s, v) in waits:
                self.eobj[e].wait_ge(s, v)

    def push_scope(self):
        st = ExitStack()
        st.__enter__()
        self._saved = getattr(self, '_saved', [])
        self._saved.append(self.alloc_stack)
        self.alloc_stack = st
        return st

    def pop_scope(self):
        self.barrier()
        st = self.alloc_stack
        self.alloc_stack = self._saved.pop()
        st.__exit__(None, None, None)

    def dma(self, out, in_, reads=(), writes=(), eng='sp', **kw):
        return self.op(eng, lambda e: e.dma_start(out=out, in_=in_, **kw), reads, writes, dma=True)

    def finish(self):
        wd = self.waited['sp']
        waits = []
        for i, s in enumerate(self.dma_sems):
            v = self.dma_val[i]
            if v > 0 and wd.get(s.num, 0) < v:
                waits.append((s, v))
        for e in ENGS:
            if e == 'sp' or self.cnt[e] == 0:
                continue
            k = self.cnt[e]
            ep = (k - 1) // self.EPOCH
            s = self.esems[e][ep]
            v = k - ep * self.EPOCH
            if wd.get(s.num, 0) < v:
                waits.append((s, v))
        self.final_waits = waits

    def emit(self):
        self.finish()
        for (s, v) in self.final_waits:
            self.eobj['sp'].wait_ge(s, v)


S_LEN = 8192
D_MODEL = 4096
SCALE = 128 ** -0.5
NEGM = 30000.0
PI = 3.141592653589793


def _sel_causal(q0, k0):
    return dict(pattern=[[1, 512]], cm=-1, base=q0 - k0)


def attn_block(P, nm, qT, tiles, S_ps, bS, PT, bPT, O_ps, bO, dv1, q_reads):
    n = len(tiles)
    first = {}
    last = {}
    for i, t in enumerate(tiles):
        for s in t['subs']:
            first.setdefault(s, i)
            last[s] = i

    def emitS(i):
        t = tiles[i]
        sb_ = S_ps[i % len(S_ps)]
        b = bS[i % len(S_ps)]
        mm = t.get('mm')
        P.op('pe', lambda e: e.matmul(sb_[:, :], lhsT=t['kT'], rhs=qT, start=True, stop=(mm is None)),
             list(t['reads']) + list(q_reads), [b])
        if mm is not None:
            P.op('pe', lambda e: e.matmul(sb_[:, :], lhsT=mm[0], rhs=mm[1], start=False, stop=True),
                 list(mm[2]), [b])

    emitS(0)
    for i in range(n):
        if i + 1 < n:
            emitS(i + 1)
        t = tiles[i]
        sb_ = S_ps[i % len(S_ps)]
        b = bS[i % len(S_ps)]
        pt = PT[i % len(PT)]
        bp = bPT[i % len(PT)]
        P.op('act', lambda e: e.activation(out=pt[:, :], in_=sb_[:, :], func=AF.Exp, scale=SCALE), [b], [bp])
        sel = t.get('sel')
        if sel is not None:
            P.op('pool', lambda e: e.affine_select(out=pt[:, :], in_=pt[:, :], pattern=sel['pattern'],
                                                   compare_op=ALU.is_ge, fill=0.0, base=sel['base'],
                                                   channel_multiplier=sel['cm']), [bp], [bp])
        for s in t['subs']:
            P.op('pe', (lambda s: lambda e: e.matmul(O_ps[s][:, 0:dv1], lhsT=pt[:, s * 128:(s + 1) * 128],
                                                     rhs=t['v'], start=(first[s] == i), stop=(last[s] == i)))(s),
                 [bp] + list(t['reads']), [bO[s]])


def build_mixer(debug=False, stop_after=None, n_tt=16, n_ph=3, n_qt=16, mixers='NDM'):
    nc = bass.Bass("TRN2", target_bir_lowering=False)
    S = S_LEN
    xT = nc.dram_tensor("xT", [D_MODEL, S], F32, kind="ExternalInput").ap()
    wA = nc.dram_tensor("wA", [D_MODEL, 2828], F32, kind="ExternalInput").ap()
    pos = nc.dram_tensor("pos", [1, S], I32, kind="ExternalInput").ap()
    cpos = nc.dram_tensor("cpos", [2, 32, 128], F32, kind="ExternalInput").ap()
    cw1 = nc.dram_tensor("cw1", [2, 4096, 128], F32, kind="ExternalInput").ap()
    cw2 = nc.dram_tensor("cw2", [2, 128, 128], F32, kind="ExternalInput").ap()
    dlam = nc.dram_tensor("dlam", [1, 512], F32, kind="ExternalInput").ap()
    subg = nc.dram_tensor("subg", [1, 256], F32, kind="ExternalInput").ap()
    c_invf = nc.dram_tensor("c_invf", [32, 1], F32, kind="ExternalInput").ap()
    c_sw = nc.dram_tensor("c_sw", [128, 32], F32, kind="ExternalInput").ap()
    c_lam = nc.dram_tensor("c_lam", [1, 2], F32, kind="ExternalInput").ap()
    c_fbig = nc.dram_tensor("c_fbig", [128, 256], F32, kind="ExternalInput").ap()
    mix = nc.dram_tensor("mix", [S, 1024], F32, kind="ExternalOutput").ap()
    kd = "ExternalOutput" if debug else "Internal"
    cos_d = nc.dram_tensor("cos_d", [32, S], F32, kind=kd).ap()
    sin_d = nc.dram_tensor("sin_d", [32, S], F32, kind=kd).ap()
    pT_d = nc.dram_tensor("pT_d", [16, 128, S], BF16, kind=kd).ap()
    pV_d = nc.dram_tensor("pV_d", [3, S, 256], BF16, kind=kd).ap()
    gate_d = nc.dram_tensor("gate_d", [S, 12], F32, kind=kd).ap()
    b_cs = Buf()
    b_pT = [Buf() for _ in range(16)]
    b_pV = [Buf() for _ in range(3)]
    b_gate = Buf()

    with ExitStack() as st:
        P = Prog(nc, st)
        ident = P.sb("ident", [128, 128], F32); b_ident = Buf()
        P.op('pool', lambda e: e.memset(ident[:, :], 1.0), [], [b_ident])
        P.op('pool', lambda e: e.affine_select(out=ident[:, :], in_=ident[:, :], pattern=[[-1, 128]],
                                               compare_op=ALU.is_equal, fill=0.0, base=0, channel_multiplier=1),
             [b_ident], [b_ident])
        swm = P.sb("swm", [128, 32], BF16); b_swm = Buf()
        P.dma(swm[:, :], c_sw, writes=[b_swm], eng='pool')

        P.push_scope()
        invf = P.sb("invf", [32, 1], F32); b_invf = Buf()
        P.dma(invf[:, :], c_invf, writes=[b_invf])
        CH = 2048
        posi = P.sb("posi", [32, CH], I32); b_posi = Buf()
        ang = P.sb("ang", [32, CH], F32); b_ang = Buf()
        m1 = P.sb("m1", [32, CH], F32); b_m1 = Buf()
        tb = P.sb("tb", [32, CH], F32); b_tb = Buf()
        a2 = P.sb("a2", [32, CH], F32); b_a2 = Buf()
        for c in range(S // CH):
            sl = slice(c * CH, (c + 1) * CH)
            P.dma(posi[:, :], pos[:, sl].to_broadcast([32, CH]), writes=[b_posi])
            P.op('dve', lambda e: e.tensor_copy(out=ang[:, :], in_=posi[:, :]), [b_posi], [b_ang])
            P.op('dve', lambda e: e.tensor_scalar(out=ang[:, :], in0=ang[:, :], scalar1=invf[:, 0:1], scalar2=None,
                                                  op0=ALU.mult), [b_ang, b_invf], [b_ang])
            for (shift, dst) in ((0.0, sin_d), (0.5 * PI, cos_d)):
                P.op('dve', lambda e: e.tensor_scalar(out=a2[:, :], in0=ang[:, :], scalar1=shift, scalar2=None,
                                                      op0=ALU.add), [b_ang], [b_a2])
                P.op('dve', lambda e: e.tensor_scalar(out=m1[:, :], in0=a2[:, :], scalar1=1.0 / (2 * PI), scalar2=None,
                                                      op0=ALU.mult), [b_a2], [b_m1])
                P.op('dve', lambda e: e.tensor_copy(out=posi[:, :], in_=m1[:, :]), [b_m1], [b_posi])
                P.op('dve', lambda e: e.tensor_copy(out=m1[:, :], in_=posi[:, :]), [b_posi], [b_m1])
                P.op('dve', lambda e: e.scalar_tensor_tensor(out=m1[:, :], in0=m1[:, :], scalar=-2 * PI, in1=a2[:, :],
                                                             op0=ALU.mult, op1=ALU.add), [b_m1, b_a2], [b_m1])
                P.op('dve', lambda e: e.tensor_scalar(out=a2[:, :], in0=m1[:, :], scalar1=PI, scalar2=-2 * PI,
                                                      op0=ALU.is_gt, op1=ALU.mult), [b_m1], [b_a2])
                P.op('dve', lambda e: e.tensor_tensor(out=m1[:, :], in0=m1[:, :], in1=a2[:, :], op=ALU.add), [b_m1, b_a2], [b_m1])
                P.op('dve', lambda e: e.tensor_scalar(out=a2[:, :], in0=m1[:, :], scalar1=-PI, scalar2=2 * PI,
                                                      op0=ALU.is_lt, op1=ALU.mult), [b_m1], [b_a2])
                P.op('dve', lambda e: e.tensor_tensor(out=m1[:, :], in0=m1[:, :], in1=a2[:, :], op=ALU.add), [b_m1, b_a2], [b_m1])
                P.op('dve', lambda e: e.tensor_scalar(out=m1[:, :], in0=m1[:, :], scalar1=0.999999, scalar2=None,
                                                      op0=ALU.mult), [b_m1], [b_m1])
                P.op('act', lambda e: e.activation(out=tb[:, :], in_=m1[:, :], func=AF.Sin), [b_m1], [b_tb])
                P.dma(dst[:, sl], tb[:, :], reads=[b_tb], writes=[b_cs])
        P.pop_scope()
        if stop_after == 'rope':
            P.emit()
            return nc

        phases = [
            (0, 8, [1, 1, 1, 1, 1, 0, 1, 1], 256, True, 0, 0),
            (1292, 4, [1, 1, 1, 1], 256, False, 8, 1),
            (2060, 4, [1, 1, 1, 1], 256, False, 12, 2),
        ]
        xTv = xT.rearrange("(k p) t -> p k t", p=128)
        wAv = wA.rearrange("(k p) n -> p k n", p=128)
        for (col0, nT, ropef, nV, has_gate, pTb, pVi) in [p for p, mch in zip(phases, 'NDM') if mch in mixers][:n_ph]:
            ncols = nT * 128 + nV + (12 if has_gate else 0)
            nVg = nV + (12 if has_gate else 0)
            P.push_scope()
            wb = P.sb("wb", [128, 32, ncols], BF16); b_wb = Buf()
            for q in range(4):
                P.dma(wb[:, q * 8:(q + 1) * 8, :], wAv[:, q * 8:(q + 1) * 8, col0:col0 + ncols], writes=[b_wb], eng='pool')
            xb = [P.sb(f"xb{i}", [128, 32, 512], BF16) for i in range(2)]; b_xb = [Buf(), Buf()]
            cs = [P.sb(f"cs{i}", [32, 2, 512], F32) for i in range(2)]; b_csb = [Buf(), Buf()]
            stg = [P.sb(f"stg{i}", [128, 512], BF16) for i in range(3)]; b_stg = [Buf() for _ in range(3)]
            vst = [P.sb(f"vst{i}", [128, 256], BF16) for i in range(2)]; b_vst = [Buf(), Buf()]
            gst = [P.sb(f"gst{i}", [128, 12], F32) for i in range(2)]; b_gst = [Buf(), Buf()]
            t1 = P.sb("t1", [32, 512], F32); b_t1 = Buf()
            t2 = P.sb("t2", [32, 512], F32); b_t2 = Buf()
            pp = [P.ps(f"pp{i}", [128, 512]) for i in range(3)]; b_pp = [Buf(excl=True) for _ in range(3)]
            psw = [P.ps(f"psw{i}", [128, 512]) for i in range(2)]; b_psw = [Buf(excl=True), Buf(excl=True)]
            pv = [P.ps(f"pv{i}", [128, 512]) for i in range(2)]; b_pv = [Buf(excl=True), Buf(excl=True)]
            ic = 0
            iv = 0
            for tt in range(n_tt):
                t0 = tt * 512
                x_ = xb[tt % 2]; bx = b_xb[tt % 2]
                for q in range(2):
                    P.dma(x_[:, q * 16:(q + 1) * 16, :], xTv[:, q * 16:(q + 1) * 16, t0:t0 + 512], writes=[bx], eng='pool')
                c_ = cs[tt % 2]; bc = b_csb[tt % 2]
                P.dma(c_[:, 0, :], cos_d[:, t0:t0 + 512], reads=[b_cs], writes=[bc])
                P.dma(c_[:, 1, :], sin_d[:, t0:t0 + 512], reads=[b_cs], writes=[bc])
                for c in range(nT if 'T' not in SKIP else 0):
                    ps_ = pp[ic % 3]; bp = b_pp[ic % 3]
                    sg = stg[ic % 3]; bs = b_stg[ic % 3]
                    for k in range(32):
                        P.op('pe', lambda e: e.matmul(ps_[:, :], lhsT=wb[:, k, c * 128:(c + 1) * 128], rhs=x_[:, k, :],
                                                      start=(k == 0), stop=(k == 31)), [b_wb, bx], [bp])
                    P.op('act', lambda e: e.copy(out=sg[:, :], in_=ps_[:, :]), [bp], [bs])
                    if ropef[c] and 'R' not in SKIP:
                        sw_ = psw[ic % 2]; bw_ = b_psw[ic % 2]
                        if '1' not in SKIP:
                            P.op('pe', lambda e: e.matmul(sw_[0:32, :], lhsT=swm[:, :], rhs=sg[:, :], start=True, stop=True),
                                 [bs, b_swm], [bw_])
                        if '2' not in SKIP:
                            P.op('dve', lambda e: e.tensor_tensor(out=t1[:, :], in0=ps_[0:32, :], in1=c_[:, 0, :], op=ALU.mult),
                                 [bp, bc], [b_t1])
                        if '3' not in SKIP:
                            P.op('dve', lambda e: e.tensor_tensor(out=t2[:, :], in0=sw_[0:32, :], in1=c_[:, 1, :], op=ALU.mult),
                                 [bw_, bc], [b_t2])
                        if '4' not in SKIP:
                            P.op('dve', lambda e: e.tensor_tensor(out=sg[0:32, :], in0=t1[:, :], in1=t2[:, :], op=ALU.add),
                                 [b_t1, b_t2], [bs])
                    P.dma(pT_d[pTb + c, :, t0:t0 + 512], sg[:, :], reads=[bs], writes=[b_pT[pTb + c]])
                    ic += 1
                for sub in range(0 if 'V' not in SKIP else 4, 4):
                    pv_ = pv[iv % 2]; bpv = b_pv[iv % 2]
                    vs_ = vst[iv % 2]; bvs = b_vst[iv % 2]
                    for k in range(32):
                        P.op('pe', lambda e: e.matmul(pv_[:, 0:nVg], lhsT=x_[:, k, sub * 128:(sub + 1) * 128],
                                                      rhs=wb[:, k, nT * 128:nT * 128 + nVg],
                                                      start=(k == 0), stop=(k == 31)), [b_wb, bx], [bpv])
                    P.op('act', lambda e: e.copy(out=vs_[:, 0:nV], in_=pv_[:, 0:nV]), [bpv], [bvs])
                    r0 = t0 + sub * 128
                    P.dma(pV_d[pVi, r0:r0 + 128, :], vs_[:, :], reads=[bvs], writes=[b_pV[pVi]])
                    if has_gate and 'G' not in SKIP:
                        g_ = gst[iv % 2]; bg = b_gst[iv % 2]
                        P.op('act', lambda e: e.activation(out=g_[:, :], in_=pv_[:, nV:nV + 12], func=AF.Sigmoid), [bpv], [bg])
                        P.dma(gate_d[r0:r0 + 128, :], g_[:, :], reads=[bg], writes=[b_gate])
                    iv += 1
            P.pop_scope()

        if stop_after == 'proj':
            P.emit()
            return nc

        def ld_v(v_sb, b_v, pvi, c0, dv):
            src = pV_d[pvi].rearrange("(t p) c -> p t c", p=128)
            for q in range(4):
                P.dma(v_sb[:, q * 16:(q + 1) * 16, 0:dv], src[:, q * 16:(q + 1) * 16, c0:c0 + dv], reads=[b_pV[pvi]], writes=[b_v])
            P.op('pool', lambda e: e.memset(v_sb[:, :, dv:dv + 1], 1.0), [], [b_v])

        def causal_tiles(qt, kTs, b_k, v_sb, b_v, mm_fn=None):
            tl = []
            for kt in range(4 * qt + 4):
                a = kt - 4 * qt
                t = dict(kT=kTs[:, kt * 128:(kt + 1) * 128], v=v_sb[:, kt, :], reads=[b_k, b_v],
                         sel=_sel_causal(qt * 512, kt * 128) if a >= 0 else None,
                         subs=[s_ for s_ in range(4) if a <= s_])
                if mm_fn is not None:
                    t['mm'] = mm_fn(kt)
                tl.append(t)
            return tl

        def recip_col(rz, b_rz, src_ap, src_bufs):
            P.op('dve', lambda e: e.tensor_scalar(out=rz[:, 0:1], in0=src_ap, scalar1=1e-30, scalar2=None, op0=ALU.max),
                 src_bufs, [b_rz])
            P.op('dve', lambda e: e.reciprocal(out=rz[:, 0:1], in_=rz[:, 0:1]), [b_rz], [b_rz])

        qts = range(n_qt)

        if 'D' in mixers:
            P.push_scope()
            S_ps = [P.ps(f"S{i}", [128, 512]) for i in range(2)]; bS = [Buf(excl=True) for _ in range(2)]
            O_ps = [P.ps(f"O{i}", [128, 512]) for i in range(4)]; bO = [Buf(excl=True) for _ in range(4)]
            PT = [P.sb(f"PT{i}", [128, 512], BF16) for i in range(3)]; bPT = [Buf() for _ in range(3)]
            kT = [P.sb(f"dk{m}", [128, S], BF16) for m in range(2)]; b_kT = [Buf(), Buf()]
            for m in range(2):
                P.dma(kT[m][:, :], pT_d[10 + m], reads=[b_pT[10 + m]], writes=[b_kT[m]])
            v_sb = P.sb("dv", [128, 64, 257], BF16); b_v = Buf()
            ld_v(v_sb, b_v, 1, 0, 256)
            lv = P.sb("lv", [128, 512], F32); b_lv = Buf()
            P.dma(lv[:, :], dlam.to_broadcast([128, 512]), writes=[b_lv])
            lc = P.sb("lc", [128, 2], F32); b_lc = Buf()
            P.dma(lc[:, :], c_lam.to_broadcast([128, 2]), writes=[b_lc])
            gsc = P.sb("gsc", [128, 256], F32); b_gsc = Buf()
            P.dma(gsc[:, :], subg.to_broadcast([128, 256]), writes=[b_gsc])
            P.op('dve', lambda e: e.tensor_scalar(out=gsc[:, :], in0=gsc[:, :], scalar1=lc[:, 1:2], scalar2=None, op0=ALU.mult),
                 [b_gsc, b_lc], [b_gsc])
            pr = P.sb("pr", [128, 256], F32); b_pr = Buf()
            sm = P.sb("sm", [128, 4], F32); b_sm = Buf()
            P.op('dve', lambda e: e.tensor_tensor(out=pr[:, 0:128], in0=lv[:, 0:128], in1=lv[:, 128:256], op=ALU.mult), [b_lv], [b_pr])
            P.op('dve', lambda e: e.tensor_tensor(out=pr[:, 128:256], in0=lv[:, 256:384], in1=lv[:, 384:512], op=ALU.mult), [b_lv], [b_pr])
            P.op('dve', lambda e: e.reduce_sum(out=sm[:, 0:1], in_=pr[:, 0:128], axis=AX.X), [b_pr], [b_sm])
            P.op('dve', lambda e: e.reduce_sum(out=sm[:, 1:2], in_=pr[:, 128:256], axis=AX.X), [b_pr], [b_sm])
            P.op('act', lambda e: e.activation(out=sm[:, 0:2], in_=sm[:, 0:2], func=AF.Exp), [b_sm], [b_sm])
            P.op('dve', lambda e: e.tensor_tensor(out=sm[:, 2:3], in0=sm[:, 1:2], in1=sm[:, 0:1], op=ALU.subtract), [b_sm], [b_sm])
            P.op('dve', lambda e: e.tensor_tensor(out=sm[:, 2:3], in0=sm[:, 2:3], in1=lc[:, 0:1], op=ALU.subtract), [b_sm, b_lc], [b_sm])
            qsb = [P.sb(f"dq{i}", [128, 2, 512], BF16) for i in range(2)]; b_q = [Buf(), Buf()]
            o0 = P.sb("o0", [128, 4, 256], F32); b_o0 = Buf()
            ot = P.sb("ot", [128, 4, 256], F32); b_ot = Buf()
            ss4 = P.sb("ss4", [128, 4], F32); b_ss4 = Buf()
            sq = P.sb("sq", [128, 256], F32); b_sq = Buf()
            rz = P.sb("rz", [128, 4], F32); b_rz = Buf()
            outst = [P.sb(f"dout{i}", [128, 4, 256], F32) for i in range(2)]; b_out = [Buf(), Buf()]
            for qt in qts:
                q_ = qsb[qt % 2]; bq = b_q[qt % 2]
                for m in range(2):
                    P.dma(q_[:, m, :], pT_d[8 + m, :, qt * 512:(qt + 1) * 512], reads=[b_pT[8 + m]], writes=[bq])
                ost = outst[qt % 2]; bost = b_out[qt % 2]
                for m in range(2):
                    tl = causal_tiles(qt, kT[m], b_kT[m], v_sb, b_v)
                    attn_block(P, "d", q_[:, m, :], tl, S_ps, bS, PT, bPT, O_ps, bO, 257, [bq])
                    for s_ in range(4):
                        recip_col(rz, b_rz, O_ps[s_][:, 256:257], [bO[s_]])
                        if m == 0:
                            P.op('dve', lambda e: e.tensor_scalar(out=o0[:, s_, :], in0=O_ps[s_][:, 0:256], scalar1=rz[:, 0:1],
                                                                  scalar2=None, op0=ALU.mult), [bO[s_], b_rz], [b_o0])
                        else:
                            P.op('dve', lambda e: e.tensor_tensor(out=rz[:, 1:2], in0=rz[:, 0:1], in1=sm[:, 2:3], op=ALU.mult),
                                 [b_rz, b_sm], [b_rz])
                            P.op('dve', lambda e: e.scalar_tensor_tensor(out=ot[:, s_, :], in0=O_ps[s_][:, 0:256], scalar=rz[:, 1:2],
                                                                         in1=o0[:, s_, :], op0=ALU.mult, op1=ALU.add),
                                 [bO[s_], b_rz, b_o0], [b_ot])
                            P.op('dve', lambda e: e.tensor_tensor(out=sq[:, :], in0=ot[:, s_, :], in1=ot[:, s_, :], op=ALU.mult), [b_ot], [b_sq])
                            P.op('dve', lambda e: e.reduce_sum(out=ss4[:, s_:s_ + 1], in_=sq[:, :], axis=AX.X), [b_sq], [b_ss4])
                    if m == 1:
                        P.op('dve', lambda e: e.tensor_scalar(out=ss4[:, :], in0=ss4[:, :], scalar1=1.0 / 256, scalar2=1e-5,
                                                              op0=ALU.mult, op1=ALU.add), [b_ss4], [b_ss4])
                        P.op('act', lambda e: e.sqrt(out=ss4[:, :], in_=ss4[:, :]), [b_ss4], [b_ss4])
                        P.op('dve', lambda e: e.reciprocal(out=ss4[:, :], in_=ss4[:, :]), [b_ss4], [b_ss4])
                        for s_ in range(4):
                            P.op('dve', lambda e: e.scalar_tensor_tensor(out=ost[:, s_, :], in0=ot[:, s_, :], scalar=ss4[:, s_:s_ + 1],
                                                                         in1=gsc[:, :], op0=ALU.mult, op1=ALU.mult),
                                 [b_ot, b_ss4, b_gsc], [bost])
                P.dma(mix[qt * 512:(qt + 1) * 512, 512:768].rearrange("(s p) c -> p s c", p=128), ost[:, :, :], reads=[bost])
            P.pop_scope()
        if stop_after == 'diff':
            P.emit()
            return nc

        if 'M' in mixers:
            P.push_scope()
            S_ps = [P.ps(f"S{i}", [128, 512]) for i in range(2)]; bS = [Buf(excl=True) for _ in range(2)]
            O_ps = [P.ps(f"O{i}", [128, 512]) for i in range(4)]; bO = [Buf(excl=True) for _ in range(4)]
            aux = P.ps("aux", [128, 512]); b_aux = Buf(excl=True)
            aux2 = P.ps("aux2", [128, 512]); b_aux2 = Buf(excl=True)
            PT = [P.sb(f"PT{i}", [128, 512], BF16) for i in range(3)]; bPT = [Buf() for _ in range(3)]
            kT = [P.sb(f"mk{m}", [128, S], BF16) for m in range(2)]; b_kT = [Buf(), Buf()]
            v_sb = [P.sb(f"mv{m}", [128, 64, 129], BF16) for m in range(2)]; b_v = [Buf(), Buf()]
            kmf = P.sb("kmf", [128, 32], F32); b_kmf = Buf()
            kmb = [P.sb(f"kmb{m}", [128, 32], BF16) for m in range(2)]; b_kmb = [Buf(), Buf()]
            for m in range(2):
                P.dma(kT[m][:, :], pT_d[14 + m], reads=[b_pT[14 + m]], writes=[b_kT[m]])
                ld_v(v_sb[m], b_v[m], 2, m * 128, 128)
                P.op('dve', lambda e: e.tensor_reduce(out=kmf[:, :], in_=kT[m][:, :].rearrange("p (b k) -> p b k", k=256),
                                                      axis=AX.X, op=ALU.add), [b_kT[m]], [b_kmf])
                P.op('dve', lambda e: e.tensor_scalar(out=kmb[m][:, :], in0=kmf[:, :], scalar1=1.0 / 256, scalar2=None, op0=ALU.mult),
                     [b_kmf], [b_kmb[m]])
            indm = P.sb("indm", [32, S], BF16); b_indm = Buf()
            P.op('pool', lambda e: e.memset(indm[:, :], 1.0), [], [b_indm])
            P.op('pool', lambda e: e.affine_select(out=indm[:, :], in_=indm[:, :], pattern=[[1, S]], compare_op=ALU.is_ge, fill=0.0,
                                                   base=0, channel_multiplier=-256), [b_indm], [b_indm])
            P.op('pool', lambda e: e.affine_select(out=indm[:, :], in_=indm[:, :], pattern=[[-1, S]], compare_op=ALU.is_ge, fill=0.0,
                                                   base=255, channel_multiplier=256), [b_indm], [b_indm])
            qsb = [P.sb(f"mq{i}", [128, 2, 512], BF16) for i in range(2)]; b_q = [Buf(), Buf()]
            sc = P.sb("sc", [128, 32], F32); b_sc = Buf()
            m8 = P.sb("m8", [128, 8], F32); b_m8 = Buf()
            nm = P.sb("nm", [128, 32], F32); b_nm = Buf()
            negT = [P.sb(f"negT{i}", [32, 512], BF16) for i in range(2)]; b_negT = [Buf(), Buf()]
            rz = P.sb("rz", [128, 4], F32); b_rz = Buf()
            outst = [P.sb(f"mout{i}", [128, 4, 256], F32) for i in range(2)]; b_out = [Buf(), Buf()]
            for qt in qts:
                q_ = qsb[qt % 2]; bq = b_q[qt % 2]
                for m in range(2):
                    P.dma(q_[:, m, :], pT_d[12 + m, :, qt * 512:(qt + 1) * 512], reads=[b_pT[12 + m]], writes=[bq])
                ost = outst[qt % 2]; bost = b_out[qt % 2]
                for m in range(2):
                    ng = negT[m]; bng = b_negT[m]
                    for s_ in range(4):
                        jb = (qt * 512 + s_ * 128) // 256
                        P.op('pe', lambda e: e.matmul(aux[:, 0:32], lhsT=q_[:, m, s_ * 128:(s_ + 1) * 128], rhs=kmb[m][:, :],
                                                      start=True, stop=True), [bq, b_kmb[m]], [b_aux])
                        P.op('dve', lambda e: e.tensor_copy(out=sc[:, :], in_=aux[:, 0:32]), [b_aux], [b_sc])
                        P.op('dve', lambda e: e.memset(sc[:, jb:32], -1e30), [], [b_sc])
                        P.op('dve', lambda e: e.max(out=m8[:, :], in_=sc[:, :]), [b_sc], [b_m8])
                        P.op('dve', lambda e: e.tensor_scalar(out=nm[:, :], in0=sc[:, :], scalar1=m8[:, 2:3], scalar2=None,
                                                              op0=ALU.is_ge), [b_sc, b_m8], [b_nm])
                        P.op('dve', lambda e: e.memset(nm[:, jb:32], 0.0), [], [b_nm])
                        P.op('dve', lambda e: e.memset(nm[:, jb:jb + 1], 1.0), [], [b_nm])
                        P.op('dve', lambda e: e.tensor_scalar(out=nm[:, :], in0=nm[:, :], scalar1=NEGM, scalar2=-NEGM,
                                                              op0=ALU.mult, op1=ALU.add), [b_nm], [b_nm])
                        P.op('pe', lambda e: e.transpose(out=aux2[0:32, 0:128], in_=nm[:, :], identity=ident[:, :]),
                             [b_nm, b_ident], [b_aux2])
                        P.op('act', lambda e: e.copy(out=ng[:, s_ * 128:(s_ + 1) * 128], in_=aux2[0:32, 0:128]), [b_aux2], [bng])
                    tl = causal_tiles(qt, kT[m], b_kT[m], v_sb[m], b_v[m],
                                      mm_fn=lambda kt: (indm[:, kt * 128:(kt + 1) * 128], ng[:, :], [b_indm, bng]))
                    attn_block(P, "m", q_[:, m, :], tl, S_ps, bS, PT, bPT, O_ps, bO, 129, [bq])
                    for s_ in range(4):
                        recip_col(rz, b_rz, O_ps[s_][:, 128:129], [bO[s_]])
                        P.op('dve', lambda e: e.tensor_scalar(out=ost[:, s_, m * 128:(m + 1) * 128], in0=O_ps[s_][:, 0:128],
                                                              scalar1=rz[:, 0:1], scalar2=None, op0=ALU.mult), [bO[s_], b_rz], [bost])
                P.dma(mix[qt * 512:(qt + 1) * 512, 768:1024].rearrange("(s p) c -> p s c", p=128), ost[:, :, :], reads=[bost])
            P.pop_scope()
        if stop_after == 'moba':
            P.emit()
            return nc

        if 'N' in mixers:
            P.push_scope()
            S_ps = [P.ps(f"S{i}", [128, 512]) for i in range(2)]; bS = [Buf(excl=True) for _ in range(2)]
            O_ps = [P.ps(f"O{i}", [128, 512]) for i in range(4)]; bO = [Buf(excl=True) for _ in range(4)]
            aux = P.ps("aux", [128, 512]); b_aux = Buf(excl=True)
            aux2 = P.ps("aux2", [128, 512]); b_aux2 = Buf(excl=True)
            PT = [P.sb(f"PT{i}", [128, 512], BF16) for i in range(3)]; bPT = [Buf() for _ in range(3)]
            kcT = P.sb("kcT", [128, 512], BF16); b_kcT = Buf()
            vc = P.sb("vc", [128, 4, 129], BF16); b_vc = Buf()
            P.push_scope()
            for which in range(2):
                cin = P.sb("cin", [128, S], BF16); b_cin = Buf()
                P.dma(cin[:, :], pT_d[4 + which], reads=[b_pT[4 + which]], writes=[b_cin])
                w1 = P.sb("w1", [128, 32, 128], BF16); b_w1 = Buf()
                P.dma(w1[:, :, :], cw1[which].rearrange("(l d) h -> d l h", d=128), writes=[b_w1], eng='pool')
                w2 = P.sb("w2", [128, 128], BF16); b_w2 = Buf()
                P.dma(w2[:, :], cw2[which], writes=[b_w2], eng='pool')
                posT = P.sb("posT", [128, 32], BF16); b_posT = Buf()
                P.dma(posT[:, :], cpos[which].rearrange("l d -> d l"), writes=[b_posT], eng='pool', allow_slow_non_contiguous=True)
                for l in range(32):
                    P.op('pe', lambda e: e.matmul(aux[:, 0:511], lhsT=w1[:, l, :], rhs=cin[:, l:l + 16 * 510 + 1:16],
                                                  start=(l == 0), stop=(l == 31)), [b_w1, b_cin], [b_aux])
                for l in range(32):
                    P.op('pe', lambda e: e.matmul(aux2[:, 0:1], lhsT=w1[:, l, :], rhs=posT[:, l:l + 1],
                                                  start=(l == 0), stop=(l == 31)), [b_w1, b_posT], [b_aux2])
                pb = P.sb("pb", [128, 1], F32); b_pb = Buf()
                P.op('dve', lambda e: e.tensor_copy(out=pb[:, :], in_=aux2[:, 0:1]), [b_aux2], [b_pb])
                u = P.sb("u", [128, 512], F32); b_u = Buf()
                w_ = P.sb("w_", [128, 512], F32); b_w_ = Buf()
                gel = P.sb("gel", [128, 512], BF16); b_gel = Buf()
                P.op('dve', lambda e: e.memset(gel[:, 511:512], 0.0), [], [b_gel])
                P.op('dve', lambda e: e.tensor_scalar(out=u[:, 0:511], in0=aux[:, 0:511], scalar1=pb[:, 0:1], scalar2=None, op0=ALU.add),
                     [b_aux, b_pb], [b_u])
                P.op('dve', lambda e: e.tensor_tensor(out=w_[:, 0:511], in0=u[:, 0:511], in1=u[:, 0:511], op=ALU.mult), [b_u], [b_w_])
                P.op('dve', lambda e: e.tensor_scalar(out=w_[:, 0:511], in0=w_[:, 0:511], scalar1=0.044715, scalar2=1.0,
                                                      op0=ALU.mult, op1=ALU.add), [b_w_], [b_w_])
                P.op('dve', lambda e: e.tensor_tensor(out=w_[:, 0:511], in0=w_[:, 0:511], in1=u[:, 0:511], op=ALU.mult), [b_w_, b_u], [b_w_])
                P.op('act', lambda e: e.activation(out=w_[:, 0:511], in_=w_[:, 0:511], func=AF.Tanh, scale=0.7978845608028654),
                     [b_w_], [b_w_])
                P.op('dve', lambda e: e.scalar_tensor_tensor(out=w_[:, 0:511], in0=w_[:, 0:511], scalar=1.0, in1=u[:, 0:511],
                                                             op0=ALU.add, op1=ALU.mult), [b_w_, b_u], [b_w_])
                P.op('dve', lambda e: e.tensor_scalar(out=gel[:, 0:511], in0=w_[:, 0:511], scalar1=0.5, scalar2=None, op0=ALU.mult),
                     [b_w_], [b_gel])
                if which == 0:
                    P.op('pe', lambda e: e.matmul(aux[:, 0:512], lhsT=w2[:, :], rhs=gel[:, :], start=True, stop=True), [b_w2, b_gel], [b_aux])
                    P.op('act', lambda e: e.copy(out=kcT[:, :], in_=aux[:, 0:512]), [b_aux], [b_kcT])
                else:
                    for nt in range(4):
                        P.op('pe', lambda e: e.matmul(aux[:, 0:128], lhsT=gel[:, nt * 128:(nt + 1) * 128], rhs=w2[:, :], start=True, stop=True),
                             [b_w2, b_gel], [b_aux])
                        P.op('act', lambda e: e.copy(out=vc[:, nt, 0:128], in_=aux[:, 0:128]), [b_aux], [b_vc])
                    P.op('pool', lambda e: e.memset(vc[:, :, 128:129], 1.0), [], [b_vc])
            P.pop_scope()
            ksT = P.sb("ksT", [128, S], BF16); b_ksT = Buf()
            kwT = P.sb("kwT", [128, S], BF16); b_kwT = Buf()
            P.dma(ksT[:, :], pT_d[6], reads=[b_pT[6]], writes=[b_ksT])
            P.dma(kwT[:, :], pT_d[7], reads=[b_pT[7]], writes=[b_kwT])
            vs = P.sb("vs", [128, 64, 129], BF16); b_vs = Buf()
            vw = P.sb("vw", [128, 64, 129], BF16); b_vw = Buf()
            ld_v(vs, b_vs, 0, 0, 128)
            ld_v(vw, b_vw, 0, 128, 128)
            indb = P.sb("indb", [128, S], BF16); b_indb = Buf()
            P.op('pool', lambda e: e.memset(indb[:, :], 1.0), [], [b_indb])
            P.op('pool', lambda e: e.affine_select(out=indb[:, :], in_=indb[:, :], pattern=[[1, S]], compare_op=ALU.is_ge, fill=0.0,
                                                   base=0, channel_multiplier=-64), [b_indb], [b_indb])
            P.op('pool', lambda e: e.affine_select(out=indb[:, :], in_=indb[:, :], pattern=[[-1, S]], compare_op=ALU.is_ge, fill=0.0,
                                                   base=63, channel_multiplier=64), [b_indb], [b_indb])
            fbig = P.sb("fbig", [128, 256], F32); b_fbig = Buf()
            P.dma(fbig[:, :], c_fbig, writes=[b_fbig])
            qsb = [P.sb(f"nq{i}", [128, 4, 512], BF16) for i in range(2)]; b_q = [Buf(), Buf()]
            gsb = [P.sb(f"ng{i}", [128, 4, 12], F32) for i in range(2)]; b_g = [Buf(), Buf()]
            E = [P.sb(f"E{i}", [128, 512], F32) for i in range(2)]; b_E = [Buf(), Buf()]
            pg = P.sb("pg", [128, 512], F32); b_pg = Buf()
            imp = P.sb("imp", [128, 128], F32); b_imp = Buf()
            wk = P.sb("wk", [128, 128], F32); b_wk = Buf()
            m8 = P.sb("m8", [128, 16], F32); b_m8 = Buf()
            rz = P.sb("rz", [128, 4], F32); b_rz = Buf()
            negT = P.sb("negT", [128, 512], BF16); b_negT = Buf()
            outst = [P.sb(f"nout{i}", [128, 4, 512], F32) for i in range(2)]; b_out = [Buf(), Buf()]
            for qt in qts:
                q_ = qsb[qt % 2]; bq = b_q[qt % 2]
                g_ = gsb[qt % 2]; bg = b_g[qt % 2]
                for g in range(4):
                    P.dma(q_[:, g, :], pT_d[g, :, qt * 512:(qt + 1) * 512], reads=[b_pT[g]], writes=[bq])
                P.dma(g_[:, :, :], gate_d[qt * 512:(qt + 1) * 512, :].rearrange("(s p) c -> p s c", p=128), reads=[b_gate], writes=[bg])
                ost = outst[qt % 2]; bost = b_out[qt % 2]
                for s_ in range(4):
                    q0 = qt * 512 + s_ * 128
                    for g in range(4):
                        e_ = E[g % 2]; be = b_E[g % 2]
                        P.op('pe', lambda e: e.matmul(aux[:, :], lhsT=q_[:, g, s_ * 128:(s_ + 1) * 128], rhs=kcT[:, :], start=True, stop=True),
                             [bq, b_kcT], [b_aux])
                        P.op('act', lambda e: e.activation(out=e_[:, :], in_=aux[:, :], func=AF.Exp, scale=SCALE), [b_aux], [be])
                        P.op('pool', lambda e: e.affine_select(out=e_[:, :], in_=e_[:, :], pattern=[[-16, 512]], compare_op=ALU.is_ge,
                                                               fill=0.0, base=q0 - 31, channel_multiplier=1), [be], [be])
                        P.op('dve', lambda e: e.reduce_sum(out=rz[:, 0:1], in_=e_[:, :], axis=AX.X), [be], [b_rz])
                        recip_col(rz, b_rz, rz[:, 0:1], [b_rz])
                        if g == 0:
                            P.op('dve', lambda e: e.tensor_scalar(out=pg[:, :], in0=e_[:, :], scalar1=rz[:, 0:1], scalar2=None, op0=ALU.mult),
                                 [be, b_rz], [b_pg])
                        else:
                            P.op('dve', lambda e: e.scalar_tensor_tensor(out=pg[:, :], in0=e_[:, :], scalar=rz[:, 0:1], in1=pg[:, :],
                                                                         op0=ALU.mult, op1=ALU.add), [be, b_rz, b_pg], [b_pg])
                    P.op('dve', lambda e: e.tensor_reduce(out=imp[:, :], in_=pg[:, :].rearrange("p (b k) -> p b k", k=4), axis=AX.X, op=ALU.add),
                         [b_pg], [b_imp])
                    P.op('dve', lambda e: e.tensor_tensor(out=imp[:, 1:128], in0=imp[:, 1:128], in1=pg[:, 3:508:4], op=ALU.add),
                         [b_imp, b_pg], [b_imp])
                    st0 = 128 - q0 // 64
                    P.op('dve', lambda e: e.tensor_tensor(out=imp[:, :], in0=imp[:, :], in1=fbig[:, st0:st0 + 128], op=ALU.add),
                         [b_imp, b_fbig], [b_imp])
                    P.op('dve', lambda e: e.tensor_scalar(out=imp[:, 0:1], in0=imp[:, 0:1], scalar1=100.0, scalar2=None, op0=ALU.add),
                         [b_imp], [b_imp])
                    P.op('dve', lambda e: e.max(out=m8[:, 0:8], in_=imp[:, :]), [b_imp], [b_m8])
                    P.op('dve', lambda e: e.match_replace(out=wk[:, :], in_to_replace=m8[:, 0:8], in_values=imp[:, :], imm_value=-1e30),
                         [b_imp, b_m8], [b_wk])
                    P.op('dve', lambda e: e.max(out=m8[:, 8:16], in_=wk[:, :]), [b_wk], [b_m8])
                    P.op('dve', lambda e: e.tensor_scalar(out=wk[:, :], in0=imp[:, :], scalar1=m8[:, 15:16], scalar2=NEGM,
                                                          op0=ALU.is_ge, op1=ALU.mult), [b_imp, b_m8], [b_wk])
                    P.op('dve', lambda e: e.tensor_scalar(out=wk[:, :], in0=wk[:, :], scalar1=-NEGM, scalar2=None, op0=ALU.add),
                         [b_wk], [b_wk])
                    P.op('pe', lambda e: e.transpose(out=aux2[:, 0:128], in_=wk[:, :], identity=ident[:, :]), [b_wk, b_ident], [b_aux2])
                    P.op('act', lambda e: e.copy(out=negT[:, s_ * 128:(s_ + 1) * 128], in_=aux2[:, 0:128]), [b_aux2], [b_negT])
                for g in range(4):
                    qg = q_[:, g, :]
                    for br in range(3):
                        if br == 0:
                            tl = []
                            for nt in range(4):
                                if qt * 512 + 511 < 2048 * nt + 31:
                                    continue
                                full = qt * 512 >= 16 * (nt * 128 + 127) + 31
                                tl.append(dict(kT=kcT[:, nt * 128:(nt + 1) * 128], v=vc[:, nt, :], reads=[b_kcT, b_vc],
                                               sel=None if full else dict(pattern=[[1, 512]], cm=-16, base=qt * 512 - 2048 * nt - 31),
                                               subs=[x_ for x_ in range(4) if qt * 512 + x_ * 128 + 127 >= 2048 * nt + 31]))
                        elif br == 1:
                            tl = causal_tiles(qt, ksT, b_ksT, vs, b_vs,
                                              mm_fn=lambda kt: (indb[:, kt * 128:(kt + 1) * 128], negT[:, :], [b_indb, b_negT]))
                        else:
                            tl = []
                            for a in range(8):
                                kt = 4 * qt - 4 + a
                                if kt < 0:
                                    continue
                                if a < 4:
                                    sel = dict(pattern=[[-1, 512]], cm=1, base=kt * 128 - qt * 512 + 511)
                                    subs = [x_ for x_ in range(4) if a >= x_]
                                else:
                                    sel = _sel_causal(qt * 512, kt * 128)
                                    subs = [x_ for x_ in range(4) if a - 4 <= x_]
                                tl.append(dict(kT=kwT[:, kt * 128:(kt + 1) * 128], v=vw[:, kt, :], reads=[b_kwT, b_vw], sel=sel, subs=subs))
                        attn_block(P, "n", qg, tl, S_ps, bS, PT, bPT, O_ps, bO, 129, [bq])
                        for s_ in range(4):
                            recip_col(rz, b_rz, O_ps[s_][:, 128:129], [bO[s_]])
                            P.op('dve', lambda e: e.tensor_tensor(out=rz[:, 1:2], in0=rz[:, 0:1], in1=g_[:, s_, 3 * g + br:3 * g + br + 1],
                                                                  op=ALU.mult), [b_rz, bg], [b_rz])
                            dst = ost[:, s_, g * 128:(g + 1) * 128]
                            if br == 0:
                                P.op('dve', lambda e: e.tensor_scalar(out=dst, in0=O_ps[s_][:, 0:128], scalar1=rz[:, 1:2], scalar2=None,
                                                                      op0=ALU.mult), [bO[s_], b_rz], [bost])
                            else:
                                P.op('dve', lambda e: e.scalar_tensor_tensor(out=dst, in0=O_ps[s_][:, 0:128], scalar=rz[:, 1:2], in1=dst,
                                                                             op0=ALU.mult, op1=ALU.add), [bO[s_], b_rz, bost], [bost])
                P.dma(mix[qt * 512:(qt + 1) * 512, 0:512].rearrange("(s p) c -> p s c", p=128), ost[:, :, :], reads=[bost])
            P.pop_scope()
        P.emit()
        return nc


ALPHA = 4 ** 0.25
TOK = 2048


def build_ffn(n_pass=4, n_exp=32, debug=False):
    nc = bass.Bass("TRN2", target_bir_lowering=False)
    xres = nc.dram_tensor("xres", [TOK, 4096], F32, kind="ExternalInput").ap()
    mixT = nc.dram_tensor("mixT", [4096, TOK], F32, kind="ExternalInput").ap()
    wout = nc.dram_tensor("wout", [4096, 4096], F32, kind="ExternalInput").ap()
    lnp = nc.dram_tensor("lnp", [4, 4096], F32, kind="ExternalInput").ap()
    wr = nc.dram_tensor("wr", [4096, 32], F32, kind="ExternalInput").ap()
    br = nc.dram_tensor("br", [1, 32], F32, kind="ExternalInput").ap()
    wgu = nc.dram_tensor("wgu", [32, 4, 128, 32 * 256], F32, kind="ExternalInput").ap()
    bgu = nc.dram_tensor("bgu", [128, 256], F32, kind="ExternalInput").ap()
    wdn = nc.dram_tensor("wdn", [32, 4, 128, 4 * 1024], F32, kind="ExternalInput").ap()
    bdn = nc.dram_tensor("bdn", [32, 4096], F32, kind="ExternalInput").ap()
    y = nc.dram_tensor("y", [TOK, 4096], F32, kind="ExternalOutput").ap()
    dbg = nc.dram_tensor("dbg", [TOK, 32], F32, kind="ExternalOutput").ap() if debug else None
    with ExitStack() as st:
        P = Prog(nc, st)
        ident = P.sb("ident", [128, 128], F32); b_ident = Buf()
        P.op('pool', lambda e: e.memset(ident[:, :], 1.0), [], [b_ident])
        P.op('pool', lambda e: e.affine_select(out=ident[:, :], in_=ident[:, :], pattern=[[-1, 128]],
                                               compare_op=ALU.is_equal, fill=0.0, base=0, channel_multiplier=1),
             [b_ident], [b_ident])
        bgs = P.sb("bgs", [128, 256], F32); b_bgs = Buf()
        P.dma(bgs[:, :], bgu, writes=[b_bgs])
        brs = P.sb("brs", [128, 32], F32); b_brs = Buf()
        P.dma(brs[:, :], br.to_broadcast([128, 32]), writes=[b_brs])
        wrs = P.sb("wrs", [128, 32, 32], F32); b_wrs = Buf()
        P.dma(wrs[:, :, :], wr.rearrange("(k p) e -> p k e", p=128), writes=[b_wrs])
        acc = P.sb("acc", [128, 4, 4096], F32); b_acc = [Buf() for _ in range(4)]
        x1T = P.sb("x1T", [128, 32, 512], BF16); b_x1T = Buf()
        G = P.sb("G", [128, 4, 32], F32); b_G = Buf()
        moutv = mixT.rearrange("(k p) t -> p k t", p=128)
        woutv = wout.rearrange("(k p) n -> p k n", p=128)

        def layer_norm(sub, gi, stat, b_stat, sq, b_sq, gb, b_gb):
            a_ = acc[:, sub, :]
            ba = b_acc[sub]
            P.op('dve', lambda e: e.reduce_sum(out=stat[:, 0:1], in_=a_, axis=AX.X), [ba], [b_stat])
            P.op('dve', lambda e: e.tensor_scalar(out=stat[:, 0:1], in0=stat[:, 0:1], scalar1=-1.0 / 4096, scalar2=None, op0=ALU.mult),
                 [b_stat], [b_stat])
            P.op('dve', lambda e: e.tensor_scalar(out=a_, in0=a_, scalar1=stat[:, 0:1], scalar2=None, op0=ALU.add), [ba, b_stat], [ba])
            P.op('act', lambda e: e.activation(out=sq[:, :], in_=a_, func=AF.Square, accum_out=stat[:, 1:2]), [ba, b_stat], [b_sq, b_stat])
            P.op('dve', lambda e: e.tensor_scalar(out=stat[:, 1:2], in0=stat[:, 1:2], scalar1=1.0 / 4096, scalar2=1e-5,
                                                  op0=ALU.mult, op1=ALU.add), [b_stat], [b_stat])
            P.op('act', lambda e: e.sqrt(out=stat[:, 1:2], in_=stat[:, 1:2]), [b_stat], [b_stat])
            P.op('dve', lambda e: e.reciprocal(out=stat[:, 1:2], in_=stat[:, 1:2]), [b_stat], [b_stat])
            for ch in range(4):
                cs_ = slice(ch * 1024, (ch + 1) * 1024)
                P.op('dve', lambda e: e.scalar_tensor_tensor(out=acc[:, sub, cs_], in0=acc[:, sub, cs_], scalar=stat[:, 1:2],
                                                             in1=gb[:, 0, cs_], op0=ALU.mult, op1=ALU.mult), [ba, b_stat, b_gb], [ba])
                P.op('pool', lambda e: e.tensor_tensor(out=acc[:, sub, cs_], in0=acc[:, sub, cs_], in1=gb[:, 1, cs_], op=ALU.add),
                     [ba, b_gb], [ba])

        for ps_i in range(n_pass):
            t0 = ps_i * 512
            P.push_scope()
            mT = P.sb("mT", [128, 32, 512], BF16); b_mT = Buf()
            for q in range(2):
                P.dma(mT[:, q * 16:(q + 1) * 16, :], moutv[:, q * 16:(q + 1) * 16, t0:t0 + 512], writes=[b_mT], eng='pool')
            wo = [P.sb(f"wo{i}", [128, 32, 256], BF16) for i in range(2)]; b_wo = [Buf(), Buf()]
            xr = [P.sb(f"xr{i}", [128, 4, 256], F32) for i in range(2)]; b_xr = [Buf(), Buf()]
            pso = [P.ps(f"pso{i}", [128, 512]) for i in range(2)]; b_pso = [Buf(excl=True), Buf(excl=True)]
            io = 0
            for cc in range(16):
                w_ = wo[cc % 2]; bw = b_wo[cc % 2]
                P.dma(w_[:, :, :], woutv[:, :, cc * 256:(cc + 1) * 256], writes=[bw], eng='pool')
                x_ = xr[cc % 2]; bx = b_xr[cc % 2]
                P.dma(x_[:, :, :], xres[t0:t0 + 512, cc * 256:(cc + 1) * 256].rearrange("(s p) c -> p s c", p=128), writes=[bx])
                for sub in range(4):
                    po = pso[io % 2]; bpo = b_pso[io % 2]
                    for k in range(32):
                        P.op('pe', lambda e: e.matmul(po[:, 0:256], lhsT=mT[:, k, sub * 128:(sub + 1) * 128], rhs=w_[:, k, :],
                                                      start=(k == 0), stop=(k == 31)), [b_mT, bw], [bpo])
                    P.op('dve', lambda e: e.scalar_tensor_tensor(out=acc[:, sub, cc * 256:(cc + 1) * 256], in0=x_[:, sub, :], scalar=ALPHA,
                                                                 in1=po[:, 0:256], op0=ALU.mult, op1=ALU.add), [bx, bpo], [b_acc[sub]])
                    io += 1
            P.pop_scope()
            P.push_scope()
            gb = P.sb("gb", [128, 2, 4096], F32); b_gb = Buf()
            P.dma(gb[:, 0, :], lnp[0:1, :].to_broadcast([128, 4096]), writes=[b_gb])
            P.dma(gb[:, 1, :], lnp[1:2, :].to_broadcast([128, 4096]), writes=[b_gb])
            ptr = [P.ps(f"ptr{i}", [128, 512]) for i in range(2)]; b_ptr = [Buf(excl=True), Buf(excl=True)]
            plg = P.ps("plg", [128, 512]); b_plg = Buf(excl=True)
            pbd = [P.ps(f"pbd{i}", [128, 512]) for i in range(2)]; b_pbd = [Buf(excl=True), Buf(excl=True)]
            stat = P.sb("stat", [128, 2], F32); b_stat = Buf()
            sq = P.sb("sq", [128, 4096], BF16); b_sq = Buf()
            xTf = P.sb("xTf", [128, 32, 128], F32); b_xTf = Buf()
            lg = P.sb("lg", [128, 32], F32); b_lg = Buf()
            m8 = P.sb("m8", [128, 8], F32); b_m8 = Buf()
            ex = P.sb("ex", [128, 32], F32); b_ex = Buf()
            GT = P.sb("GT", [32, 128], F32); b_GT = Buf()
            bds = P.sb("bds", [32, 4096], F32); b_bds = Buf()
            P.dma(bds[:, :], bdn, writes=[b_bds])
            for sub in range(4):
                layer_norm(sub, 0, stat, b_stat, sq, b_sq, gb, b_gb)
                for k4 in range(8):
                    pt_ = ptr[k4 % 2]; bpt = b_ptr[k4 % 2]
                    for i4 in range(4):
                        k = k4 * 4 + i4
                        P.op('pe', lambda e: e.transpose(out=pt_[:, i4 * 128:(i4 + 1) * 128], in_=acc[:, sub, k * 128:(k + 1) * 128],
                                                         identity=ident[:, :]), [b_acc[sub], b_ident], [bpt])
                    P.op('act', lambda e: e.copy(out=x1T[:, k4 * 4:(k4 + 1) * 4, sub * 128:(sub + 1) * 128],
                                                 in_=pt_[:, :].rearrange("p (a b) -> p a b", b=128)), [bpt], [b_x1T])
                    P.op('dve', lambda e: e.tensor_copy(out=xTf[:, k4 * 4:(k4 + 1) * 4, :], in_=pt_[:, :].rearrange("p (a b) -> p a b", b=128)),
                         [bpt], [b_xTf])
                for k in range(32):
                    P.op('pe', lambda e: e.matmul(plg[:, 0:32], lhsT=xTf[:, k, :], rhs=wrs[:, k, :], start=(k == 0), stop=(k == 31)),
                         [b_xTf, b_wrs], [b_plg])
                P.op('dve', lambda e: e.tensor_tensor(out=lg[:, :], in0=plg[:, 0:32], in1=brs[:, :], op=ALU.add), [b_plg, b_brs], [b_lg])
                P.op('dve', lambda e: e.max(out=m8[:, :], in_=lg[:, :]), [b_lg], [b_m8])
                P.op('dve', lambda e: e.tensor_scalar(out=ex[:, :], in0=lg[:, :], scalar1=m8[:, 0:1], scalar2=None, op0=ALU.subtract),
                     [b_lg, b_m8], [b_ex])
                P.op('act', lambda e: e.activation(out=ex[:, :], in_=ex[:, :], func=AF.Exp), [b_ex], [b_ex])
                P.op('dve', lambda e: e.scalar_tensor_tensor(out=ex[:, :], in0=lg[:, :], scalar=m8[:, 3:4], in1=ex[:, :],
                                                             op0=ALU.is_ge, op1=ALU.mult), [b_lg, b_m8, b_ex], [b_ex])
                P.op('dve', lambda e: e.reduce_sum(out=m8[:, 4:5], in_=ex[:, :], axis=AX.X), [b_ex], [b_m8])
                P.op('dve', lambda e: e.reciprocal(out=m8[:, 4:5], in_=m8[:, 4:5]), [b_m8], [b_m8])
                P.op('dve', lambda e: e.tensor_scalar(out=G[:, sub, :], in0=ex[:, :], scalar1=m8[:, 4:5], scalar2=None, op0=ALU.mult),
                     [b_ex, b_m8], [b_G])
                if dbg is not None:
                    P.dma(dbg[t0 + sub * 128:t0 + (sub + 1) * 128, :], G[:, sub, :], reads=[b_G])
                P.op('pe', lambda e: e.transpose(out=plg[0:32, 128:256], in_=G[:, sub, :], identity=ident[:, :]), [b_G, b_ident], [b_plg])
                P.op('dve', lambda e: e.tensor_copy(out=GT[:, :], in_=plg[0:32, 128:256]), [b_plg], [b_GT])
                for ch in range(8):
                    pb_ = pbd[ch % 2]; bpb = b_pbd[ch % 2]
                    P.op('pe', lambda e: e.matmul(pb_[:, :], lhsT=GT[:, :], rhs=bds[:, ch * 512:(ch + 1) * 512], start=True, stop=True),
                         [b_GT, b_bds], [bpb])
                    P.op('dve', lambda e: e.scalar_tensor_tensor(out=acc[:, sub, ch * 512:(ch + 1) * 512], in0=acc[:, sub, ch * 512:(ch + 1) * 512],
                                                                 scalar=ALPHA, in1=pb_[:, :], op0=ALU.mult, op1=ALU.add),
                         [b_acc[sub], bpb], [b_acc[sub]])
            P.pop_scope()
            P.push_scope()
            NWB = 3
            wg = [P.sb(f"wg{i}", [128, 32, 256], BF16) for i in range(NWB)]; b_wg = [Buf() for _ in range(NWB)]
            wd = [P.sb(f"wd{i}", [128, 4, 1024], BF16) for i in range(NWB)]; b_wd = [Buf() for _ in range(NWB)]
            pg_ = [P.ps(f"pg{i}", [128, 512]) for i in range(4)]; b_pg = [Buf(excl=True) for _ in range(4)]
            pd_ = [P.ps(f"pd{i}", [128, 512]) for i in range(3)]; b_pd = [Buf(excl=True) for _ in range(3)]
            actT = [P.sb(f"actT{i}", [128, 4, 512], BF16) for i in range(2)]; b_act = [Buf(), Buf()]
            gl = [P.sb(f"gl{i}", [128, 512], F32) for i in range(2)]; b_gl = [Buf(), Buf()]
            sg_ = [P.sb(f"sg{i}", [128, 512], F32) for i in range(2)]; b_sg = [Buf(), Buf()]
            ln_ = [P.sb(f"ln{i}", [128, 512], F32) for i in range(2)]; b_ln = [Buf(), Buf()]
            ig = 0
            idn = 0
            iw = 0
            for ex_i in range(n_exp):
                a_ = actT[ex_i % 2]; ba = b_act[ex_i % 2]
                for f in range(4):
                    w_ = wg[ig % NWB]; bw = b_wg[ig % NWB]
                    P.dma(w_[:, :, :], wgu[ex_i, f].rearrange("p (k c) -> p k c", c=256), writes=[bw], eng='pool')
                    pgl = pg_[(ig % 2) * 2]; bpgl = b_pg[(ig % 2) * 2]
                    pln = pg_[(ig % 2) * 2 + 1]; bpln = b_pg[(ig % 2) * 2 + 1]
                    for k in range(32):
                        P.op('pe', lambda e: e.matmul(pgl[:, :], lhsT=w_[:, k, 0:128], rhs=x1T[:, k, :], start=(k == 0), stop=(k == 31)),
                             [bw, b_x1T], [bpgl])
                    for k in range(32):
                        P.op('pe', lambda e: e.matmul(pln[:, :], lhsT=w_[:, k, 128:256], rhs=x1T[:, k, :], start=(k == 0), stop=(k == 31)),
                             [bw, b_x1T], [bpln])
                    g_ = gl[ig % 2]; bg = b_gl[ig % 2]
                    s_ = sg_[ig % 2]; bs = b_sg[ig % 2]
                    l_ = ln_[ig % 2]; bl = b_ln[ig % 2]
                    bc = (ex_i * 4 + f) * 2
                    P.op('dve', lambda e: e.tensor_scalar(out=g_[:, :], in0=pgl[:, :], scalar1=bgs[:, bc:bc + 1], scalar2=7.0,
                                                          op0=ALU.add, op1=ALU.min), [bpgl, b_bgs], [bg])
                    P.op('act', lambda e: e.activation(out=s_[:, :], in_=g_[:, :], func=AF.Sigmoid, scale=1.702), [bg], [bs])
                    P.op('dve', lambda e: e.tensor_scalar(out=l_[:, :], in0=pln[:, :], scalar1=bgs[:, bc + 1:bc + 2], scalar2=7.0,
                                                          op0=ALU.add, op1=ALU.min), [bpln, b_bgs], [bl])
                    P.op('dve', lambda e: e.tensor_scalar(out=l_[:, :], in0=l_[:, :], scalar1=-7.0, scalar2=1.0,
                                                          op0=ALU.max, op1=ALU.add), [bl], [bl])
                    P.op('dve', lambda e: e.tensor_tensor(out=g_[:, :], in0=g_[:, :], in1=s_[:, :], op=ALU.mult), [bg, bs], [bg])
                    P.op('dve', lambda e: e.tensor_tensor(out=a_[:, f, :], in0=g_[:, :], in1=l_[:, :], op=ALU.mult), [bg, bl], [ba])
                    ig += 1
                for cc in range(4):
                    d_ = wd[iw % NWB]; bd = b_wd[iw % NWB]
                    P.dma(d_[:, :, :], wdn[ex_i, cc].rearrange("p (f c) -> p f c", c=1024), writes=[bd], eng='pool')
                    iw += 1
                    for sub in range(4):
                        for hf in range(2):
                            pp_ = pd_[idn % 3]; bpp = b_pd[idn % 3]
                            for ft in range(4):
                                P.op('pe', lambda e: e.matmul(pp_[:, :], lhsT=a_[:, ft, sub * 128:(sub + 1) * 128],
                                                              rhs=d_[:, ft, hf * 512:(hf + 1) * 512], start=(ft == 0), stop=(ft == 3)),
                                     [ba, bd], [bpp])
                            c0 = cc * 1024 + hf * 512
                            P.op('dve', lambda e: e.scalar_tensor_tensor(out=acc[:, sub, c0:c0 + 512], in0=pp_[:, :],
                                                                         scalar=G[:, sub, ex_i:ex_i + 1], in1=acc[:, sub, c0:c0 + 512],
                                                                         op0=ALU.mult, op1=ALU.add), [bpp, b_G, b_acc[sub]], [b_acc[sub]])
                            idn += 1
            P.pop_scope()
            P.push_scope()
            gb = P.sb("gb2", [128, 2, 4096], F32); b_gb = Buf()
            P.dma(gb[:, 0, :], lnp[2:3, :].to_broadcast([128, 4096]), writes=[b_gb])
            P.dma(gb[:, 1, :], lnp[3:4, :].to_broadcast([128, 4096]), writes=[b_gb])
            stat = P.sb("stat2", [128, 2], F32); b_stat = Buf()
            sq = P.sb("sq2", [128, 4096], BF16); b_sq = Buf()
            for sub in range(4):
                layer_norm(sub, 2, stat, b_stat, sq, b_sq, gb, b_gb)
                P.dma(y[t0 + sub * 128:t0 + (sub + 1) * 128, :], acc[:, sub, :], reads=[b_acc[sub]])
            P.pop_scope()
        P.emit()
    return nc


def prep_ffn_weights(w_out, ln1_g, ln1_b, ln2_g, ln2_b, w_router, b_router, w_gate_up, b_gate_up, w_down, b_down):
    wg = w_gate_up.reshape(32, 32, 128, 4, 128, 2)
    wg = np.ascontiguousarray(wg.transpose(0, 3, 2, 1, 5, 4)).reshape(32, 4, 128, 32 * 256)
    bg = b_gate_up.reshape(32, 4, 128, 2)
    bg = np.ascontiguousarray(bg.transpose(2, 0, 1, 3)).reshape(128, 256)
    wd = w_down.reshape(32, 4, 128, 4, 1024)
    wd = np.ascontiguousarray(wd.transpose(0, 3, 2, 1, 4)).reshape(32, 4, 128, 4 * 1024)
    return dict(wout=np.ascontiguousarray(w_out), lnp=np.ascontiguousarray(np.stack([ln1_g, ln1_b, ln2_g, ln2_b])),
                wr=np.ascontiguousarray(w_router), br=np.ascontiguousarray(b_router.reshape(1, 32)),
                wgu=wg, bgu=bg, wdn=wd, bdn=np.ascontiguousarray(b_down))


_COLS = [2048, 512, 512, 512, 512, 512, 512, 48, 1024, 1024, 1024, 1024, 1024, 1024]
_OFF = [0]
for _c in _COLS:
    _OFF.append(_OFF[-1] + _c)


def mixer_cols(j):
    r = lambda seg, a, n: list(range(_OFF[seg] + a, _OFF[seg] + a + n))
    cols = []
    cols += r(0, 4 * j * 128, 512)
    for seg in (1, 2, 3, 5):
        cols += r(seg, j * 128, 128)
    cols += r(4, j * 128, 128)
    cols += r(6, j * 128, 128)
    cols += r(7, j * 12, 12)
    cols += r(8, 2 * j * 128, 256)
    cols += r(9, 2 * j * 128, 256)
    cols += r(10, j * 256, 256)
    cols += r(11, 2 * j * 128, 256)
    cols += r(12, 2 * j * 128, 256)
    cols += r(13, 2 * j * 128, 256)
    return np.array(cols)


def mixer_consts(layer):
    import math
    invf = (1.0 / (500000.0 ** (np.arange(0, 32, 2, dtype=np.float32) / 32))).astype(np.float32)
    c_invf = np.concatenate([invf, invf]).reshape(32, 1).astype(np.float32)
    sw = np.zeros((128, 32), np.float32)
    for m in range(16):
        sw[m + 16, m] = -1.0
        sw[m, m + 16] = 1.0
    li = 0.8 - 0.6 * math.exp(-0.3 * layer)
    c_lam = np.array([[li, 1.0 - li]], np.float32)
    fb = np.zeros((128, 256), np.float32)
    for p in range(128):
        cr = p // 64
        for c in range(256):
            rel = c - 128
            if rel > cr:
                fb[p, c] = -1e30
            elif rel == cr or rel == cr - 1:
                fb[p, c] = 100.0
    return dict(c_invf=c_invf, c_sw=sw, c_lam=c_lam, c_fbig=fb)


_PROGS = {}


def _get_prog(name):
    if name not in _PROGS:
        _PROGS[name] = build_mixer() if name == 'mixer' else build_ffn()
    return _PROGS[name]


def kernel(x, positions, w_in, nsa_cmp_pos, nsa_cmp_w1, nsa_cmp_w2, diff_lambda, diff_subln_g,
           w_out, ln1_g, ln1_b, w_router, b_router, w_gate_up, b_gate_up, w_down, b_down, ln2_g, ln2_b):
    x = np.asarray(x, np.float32)
    positions = np.asarray(positions, np.int32)
    B, S, D = x.shape
    cores = list(range(8))
    for layer in range(2):
        ncA = _get_prog('mixer')
        consts = mixer_consts(layer)
        xTs = [np.ascontiguousarray(x[b].T) for b in range(B)]
        wAs = [np.ascontiguousarray(np.asarray(w_in[layer])[:, mixer_cols(j)]) for j in range(4)]
        in_maps = []
        for c in cores:
            b, j = c // 4, c % 4
            in_maps.append(dict(xT=xTs[b], wA=wAs[j], pos=np.ascontiguousarray(positions[b:b + 1]),
                                cpos=np.ascontiguousarray(nsa_cmp_pos[layer]), cw1=np.ascontiguousarray(nsa_cmp_w1[layer]),
                                cw2=np.ascontiguousarray(nsa_cmp_w2[layer]),
                                dlam=np.ascontiguousarray(np.asarray(diff_lambda[layer]).reshape(1, 512)),
                                subg=np.ascontiguousarray(np.asarray(diff_subln_g[layer]).reshape(1, 256)), **consts))
        resA = run_bass_kernel_spmd(ncA, in_maps, core_ids=cores)
        del xTs, wAs, in_maps
        mix_full = np.empty((B, S, 4096), np.float32)
        for c in cores:
            b, j = c // 4, c % 4
            m = resA.results[c]["mix"]
            mix_full[b, :, j * 512:(j + 1) * 512] = m[:, 0:512]
            mix_full[b, :, 2048 + j * 256:2048 + (j + 1) * 256] = m[:, 512:768]
            mix_full[b, :, 3072 + j * 256:3072 + (j + 1) * 256] = m[:, 768:1024]
        ncB = _get_prog('ffn')
        W = prep_ffn_weights(np.asarray(w_out[layer]), np.asarray(ln1_g[layer]), np.asarray(ln1_b[layer]), np.asarray(ln2_g[layer]),
                             np.asarray(ln2_b[layer]), np.asarray(w_router[layer]), np.asarray(b_router[layer]),
                             np.asarray(w_gate_up[layer]), np.asarray(b_gate_up[layer]), np.asarray(w_down[layer]),
                             np.asarray(b_down[layer]))
        in_maps = []
        for c in cores:
            b, s0 = c // 4, (c % 4) * TOK
            in_maps.append(dict(xres=np.ascontiguousarray(x[b, s0:s0 + TOK]), mixT=np.ascontiguousarray(mix_full[b, s0:s0 + TOK].T), **W))
        resB = run_bass_kernel_spmd(ncB, in_maps, core_ids=cores)
        del in_maps, W, mix_full
        xn = np.empty_like(x)
        for c in cores:
            b, s0 = c // 4, (c % 4) * TOK
            xn[b, s0:s0 + TOK] = resB.results[c]["y"]
        x = xn
    return x
```

```python
import numpy as np
import os
SKIP = os.environ.get('SKIP', '')
import concourse.bass as bass
import concourse.mybir as mybir
from concourse.bass_utils import run_bass_kernel_spmd
from contextlib import ExitStack

F32 = mybir.dt.float32
BF16 = mybir.dt.bfloat16
I32 = mybir.dt.int32
ALU = mybir.AluOpType
AF = mybir.ActivationFunctionType
AX = mybir.AxisListType

ENGS = ('pe', 'dve', 'act', 'pool', 'sp')


class Buf:
    __slots__ = ('name', 'w', 'r', 'excl')

    def __init__(self, name='', excl=False):
        self.name = name
        self.excl = excl
        self.w = None
        self.r = {}


class Prog:
    EPOCH = 16000
    NDMA = 32

    def __init__(self, nc, stack):
        self.nc = nc
        self.stack = stack
        self.ops = {e: [] for e in ENGS}
        self.cnt = {e: 0 for e in ENGS}
        self.esems = {e: [] for e in ENGS}
        self.waited = {e: {} for e in ENGS}
        self.dma_sems = [self.new_sem(f"dq{i}") for i in range(self.NDMA)]
        self.dma_val = [0] * self.NDMA
        self.dma_pool = {'sp': list(range(0, 20)), 'pool': list(range(20, 32)), 'act': []}
        self.dma_next = {'sp': 0, 'pool': 0}
        self.nwaits = 0
        self.alloc_stack = stack
        self.eobj = {'pe': nc.tensor, 'dve': nc.vector, 'act': nc.scalar, 'pool': nc.gpsimd, 'sp': nc.sync}

    def new_sem(self, name):
        return self.stack.enter_context(self.nc.semaphore(name))

    def sb(self, name, shape, dt):
        self._uid = getattr(self, '_uid', 0) + 1
        name = f"{name}_{self._uid}"
        return self.alloc_stack.enter_context(self.nc.sbuf_tensor(name, list(shape), dt))

    def ps(self, name, shape, dt=F32):
        self._uid = getattr(self, '_uid', 0) + 1
        name = f"{name}_{self._uid}"
        return self.alloc_stack.enter_context(self.nc.psum_tensor(name, list(shape), dt))

    def op(self, eng, fn, reads=(), writes=(), dma=False):
        xr = [b for b in reads if b.excl]
        if xr:
            reads = [b for b in reads if not b.excl]
            writes = list(writes) + [b for b in xr if b not in writes]
        deps = []
        for b in reads:
            if b.w is not None:
                deps.append(b.w)
        for b in writes:
            if b.w is not None:
                deps.append(b.w)
            for ev in b.r.values():
                deps.append(ev)
        if dma:
            pl = self.dma_pool[eng]
            i = pl[self.dma_next[eng] % len(pl)]
            self.dma_next[eng] += 1
            sem = self.dma_sems[i]
            prev = self.dma_val[i]
            if prev > 0:
                deps.append((sem, prev, 'dma'))
            self.dma_val[i] = prev + 16
            ev = (sem, prev + 16, 'dma')
            inc = 16
        else:
            k = self.cnt[eng]
            ep = k // self.EPOCH
            if ep >= len(self.esems[eng]):
                self.esems[eng].append(self.new_sem(f"{eng}{ep}"))
            sem = self.esems[eng][ep]
            self.cnt[eng] = k + 1
            ev = (sem, k - ep * self.EPOCH + 1, eng)
            inc = 1
        wd = self.waited[eng]
        waits = []
        for (s, v, e) in deps:
            if e == 'pe' and eng == 'pe' and not dma:
                continue
            if wd.get(s.num, 0) < v:
                wd[s.num] = v
                waits.append((s, v))
        self.nwaits += len(waits)
        eo = self.eobj[eng]
        for (s, v) in waits:
            eo.wait_ge(s, v)
        fn(eo).then_inc(sem, inc)
        for b in reads:
            old = b.r.get(sem.num)
            if old is None or old[1] < ev[1]:
                b.r[sem.num] = ev
        for b in writes:
            b.w = ev
            b.r = {}
        return ev


    def barrier(self):
        evs = []
        for e in ENGS:
            k = self.cnt[e]
            if k == 0:
                continue
            ep = (k - 1) // self.EPOCH
            evs.append((self.esems[e][ep], k - ep * self.EPOCH))
        for i, s in enumerate(self.dma_sems):
            if self.dma_val[i] > 0:
                evs.append((s, self.dma_val[i]))
        for e in ENGS:
            wd = self.waited[e]
            waits = []
            for (s, v) in evs:
                if wd.get(s.num, 0) < v:
                    wd[s.num] = v
                    waits.append((s, v))
            for (s, v) in waits:
                self.eobj[e].wait_ge(s, v)

    def push_scope(self):
        st = ExitStack()
        st.__enter__()
        self._saved = getattr(self, '_saved', [])
        self._saved.append(self.alloc_stack)
        self.alloc_stack = st
        return st

    def pop_scope(self):
        self.barrier()
        st = self.alloc_stack
        self.alloc_stack = self._saved.pop()
        st.__exit__(None, None, None)

    def dma(self, out, in_, reads=(), writes=(), eng='sp', **kw):
        return self.op(eng, lambda e: e.dma_start(out=out, in_=in_, **kw), reads, writes, dma=True)

    def finish(self):
        wd = self.waited['sp']
        waits = []
        for i, s in enumerate(self.dma_sems):
            v = self.dma_val[i]
            if v > 0 and wd.get(s.num, 0) < v:
                waits.append((s, v))
        for e in ENGS:
            if e == 'sp' or self.cnt[e] == 0:
                continue
            k = self.cnt[e]
            ep = (k - 1) // self.EPOCH
            s = self.esems[e][ep]
            v = k - ep * self.EPOCH
            if wd.get(s.num, 0) < v:
                waits.append((s, v))
        self.final_waits = waits

    def emit(self):
        self.finish()
        for (s, v) in self.final_waits:
            self.eobj['sp'].wait_ge(s, v)


S_LEN = 8192
D_MODEL = 4096
SCALE = 128 ** -0.5
NEGM = 30000.0
PI = 3.141592653589793


def _sel_causal(q0, k0):
    return dict(pattern=[[1, 512]], cm=-1, base=q0 - k0)


def attn_block(P, nm, qT, tiles, S_ps, bS, PT, bPT, O_ps, bO, dv1, q_reads):
    n = len(tiles)
    first = {}
    last = {}
    for i, t in enumerate(tiles):
        for s in t['subs']:
            first.setdefault(s, i)
            last[s] = i

    def emitS(i):
        t = tiles[i]
        sb_ = S_ps[i % len(S_ps)]
        b = bS[i % len(S_ps)]
        mm = t.get('mm')
        P.op('pe', lambda e: e.matmul(sb_[:, :], lhsT=t['kT'], rhs=qT, start=True, stop=(mm is None)),
             list(t['reads']) + list(q_reads), [b])
        if mm is not None:
            P.op('pe', lambda e: e.matmul(sb_[:, :], lhsT=mm[0], rhs=mm[1], start=False, stop=True),
                 list(mm[2]), [b])

    L = max(1, len(S_ps) - 1)
    for i in range(min(L, n)):
        emitS(i)
    for i in range(n):
        if i + L < n:
            emitS(i + L)
        t = tiles[i]
        sb_ = S_ps[i % len(S_ps)]
        b = bS[i % len(S_ps)]
        pt = PT[i % len(PT)]
        bp = bPT[i % len(PT)]
        P.op('act', lambda e: e.activation(out=pt[:, :], in_=sb_[:, :], func=AF.Exp, scale=SCALE), [b], [bp])
        sel = t.get('sel')
        if sel is not None:
            P.op('pool', lambda e: e.affine_select(out=pt[:, :], in_=pt[:, :], pattern=sel['pattern'],
                                                   compare_op=ALU.is_ge, fill=0.0, base=sel['base'],
                                                   channel_multiplier=sel['cm']), [bp], [bp])
        for s in t['subs']:
            P.op('pe', (lambda s: lambda e: e.matmul(O_ps[s][:, 0:dv1], lhsT=pt[:, s * 128:(s + 1) * 128],
                                                     rhs=t['v'], start=(first[s] == i), stop=(last[s] == i)))(s),
                 [bp] + list(t['reads']), [bO[s]])


def build_mixer(debug=False, stop_after=None, n_tt=16, n_ph=3, n_qt=16, mixers='NDM'):
    nc = bass.Bass("TRN2", target_bir_lowering=False)
    S = S_LEN
    xT = nc.dram_tensor("xT", [D_MODEL, S], F32, kind="ExternalInput").ap()
    wA = nc.dram_tensor("wA", [D_MODEL, 2828], F32, kind="ExternalInput").ap()
    pos = nc.dram_tensor("pos", [1, S], I32, kind="ExternalInput").ap()
    cpos = nc.dram_tensor("cpos", [2, 32, 128], F32, kind="ExternalInput").ap()
    cw1 = nc.dram_tensor("cw1", [2, 4096, 128], F32, kind="ExternalInput").ap()
    cw2 = nc.dram_tensor("cw2", [2, 128, 128], F32, kind="ExternalInput").ap()
    dlam = nc.dram_tensor("dlam", [1, 512], F32, kind="ExternalInput").ap()
    subg = nc.dram_tensor("subg", [1, 256], F32, kind="ExternalInput").ap()
    c_invf = nc.dram_tensor("c_invf", [32, 1], F32, kind="ExternalInput").ap()
    c_sw = nc.dram_tensor("c_sw", [128, 32], F32, kind="ExternalInput").ap()
    c_lam = nc.dram_tensor("c_lam", [1, 2], F32, kind="ExternalInput").ap()
    c_fbig = nc.dram_tensor("c_fbig", [128, 256], F32, kind="ExternalInput").ap()
    mix = nc.dram_tensor("mix", [S, 1024], F32, kind="ExternalOutput").ap()
    kd = "ExternalOutput" if debug else "Internal"
    cos_d = nc.dram_tensor("cos_d", [32, S], F32, kind=kd).ap()
    sin_d = nc.dram_tensor("sin_d", [32, S], F32, kind=kd).ap()
    pT_d = nc.dram_tensor("pT_d", [16, 128, S], BF16, kind=kd).ap()
    pV_d = nc.dram_tensor("pV_d", [3, S, 256], BF16, kind=kd).ap()
    gate_d = nc.dram_tensor("gate_d", [S, 12], F32, kind=kd).ap()
    b_cs = Buf()
    b_pT = [Buf() for _ in range(16)]
    b_pV = [Buf() for _ in range(3)]
    b_gate = Buf()

    with ExitStack() as st:
        P = Prog(nc, st)
        ident = P.sb("ident", [128, 128], F32); b_ident = Buf()
        P.op('pool', lambda e: e.memset(ident[:, :], 1.0), [], [b_ident])
        P.op('pool', lambda e: e.affine_select(out=ident[:, :], in_=ident[:, :], pattern=[[-1, 128]],
                                               compare_op=ALU.is_equal, fill=0.0, base=0, channel_multiplier=1),
             [b_ident], [b_ident])
        swm = P.sb("swm", [128, 32], BF16); b_swm = Buf()
        P.dma(swm[:, :], c_sw, writes=[b_swm], eng='pool')

        P.push_scope()
        invf = P.sb("invf", [32, 1], F32); b_invf = Buf()
        P.dma(invf[:, :], c_invf, writes=[b_invf])
        CH = 2048
        posi = P.sb("posi", [32, CH], I32); b_posi = Buf()
        ang = P.sb("ang", [32, CH], F32); b_ang = Buf()
        m1 = P.sb("m1", [32, CH], F32); b_m1 = Buf()
        tb = P.sb("tb", [32, CH], F32); b_tb = Buf()
        a2 = P.sb("a2", [32, CH], F32); b_a2 = Buf()
        for c in range(S // CH):
            sl = slice(c * CH, (c + 1) * CH)
            P.dma(posi[:, :], pos[:, sl].to_broadcast([32, CH]), writes=[b_posi])
            P.op('dve', lambda e: e.tensor_copy(out=ang[:, :], in_=posi[:, :]), [b_posi], [b_ang])
            P.op('dve', lambda e: e.tensor_scalar(out=ang[:, :], in0=ang[:, :], scalar1=invf[:, 0:1], scalar2=None,
                                                  op0=ALU.mult), [b_ang, b_invf], [b_ang])
            for (shift, dst) in ((0.0, sin_d), (0.5 * PI, cos_d)):
                P.op('dve', lambda e: e.tensor_scalar(out=a2[:, :], in0=ang[:, :], scalar1=shift, scalar2=None,
                                                      op0=ALU.add), [b_ang], [b_a2])
                P.op('dve', lambda e: e.tensor_scalar(out=m1[:, :], in0=a2[:, :], scalar1=1.0 / (2 * PI), scalar2=None,
                                                      op0=ALU.mult), [b_a2], [b_m1])
                P.op('dve', lambda e: e.tensor_copy(out=posi[:, :], in_=m1[:, :]), [b_m1], [b_posi])
                P.op('dve', lambda e: e.tensor_copy(out=m1[:, :], in_=posi[:, :]), [b_posi], [b_m1])
                P.op('dve', lambda e: e.scalar_tensor_tensor(out=m1[:, :], in0=m1[:, :], scalar=-2 * PI, in1=a2[:, :],
                                                             op0=ALU.mult, op1=ALU.add), [b_m1, b_a2], [b_m1])
                P.op('dve', lambda e: e.tensor_scalar(out=a2[:, :], in0=m1[:, :], scalar1=PI, scalar2=-2 * PI,
                                                      op0=ALU.is_gt, op1=ALU.mult), [b_m1], [b_a2])
                P.op('dve', lambda e: e.tensor_tensor(out=m1[:, :], in0=m1[:, :], in1=a2[:, :], op=ALU.add), [b_m1, b_a2], [b_m1])
                P.op('dve', lambda e: e.tensor_scalar(out=a2[:, :], in0=m1[:, :], scalar1=-PI, scalar2=2 * PI,
                                                      op0=ALU.is_lt, op1=ALU.mult), [b_m1], [b_a2])
                P.op('dve', lambda e: e.tensor_tensor(out=m1[:, :], in0=m1[:, :], in1=a2[:, :], op=ALU.add), [b_m1, b_a2], [b_m1])
                P.op('dve', lambda e: e.tensor_scalar(out=m1[:, :], in0=m1[:, :], scalar1=0.999999, scalar2=None,
                                                      op0=ALU.mult), [b_m1], [b_m1])
                P.op('act', lambda e: e.activation(out=tb[:, :], in_=m1[:, :], func=AF.Sin), [b_m1], [b_tb])
                P.dma(dst[:, sl], tb[:, :], reads=[b_tb], writes=[b_cs])
        P.pop_scope()
        if stop_after == 'rope':
            P.emit()
            return nc

        phases = [
            (0, 8, [1, 1, 1, 1, 1, 0, 1, 1], 256, True, 0, 0),
            (1292, 4, [1, 1, 1, 1], 256, False, 8, 1),
            (2060, 4, [1, 1, 1, 1], 256, False, 12, 2),
        ]
        xTv = xT.rearrange("(k p) t -> p k t", p=128)
        wAv = wA.rearrange("(k p) n -> p k n", p=128)
        for (col0, nT, ropef, nV, has_gate, pTb, pVi) in [p for p, mch in zip(phases, 'NDM') if mch in mixers][:n_ph]:
            ncols = nT * 128 + nV + (12 if has_gate else 0)
            nVg = nV + (12 if has_gate else 0)
            P.push_scope()
            wb = P.sb("wb", [128, 32, ncols], BF16); b_wb = Buf()
            for q in range(4):
                P.dma(wb[:, q * 8:(q + 1) * 8, :], wAv[:, q * 8:(q + 1) * 8, col0:col0 + ncols], writes=[b_wb], eng='pool')
            xb = [P.sb(f"xb{i}", [128, 32, 512], BF16) for i in range(2)]; b_xb = [Buf(), Buf()]
            cs = [P.sb(f"cs{i}", [32, 2, 512], F32) for i in range(2)]; b_csb = [Buf(), Buf()]
            stg = [P.sb(f"stg{i}", [128, 512], BF16) for i in range(3)]; b_stg = [Buf() for _ in range(3)]
            vst = [P.sb(f"vst{i}", [128, 256], BF16) for i in range(2)]; b_vst = [Buf(), Buf()]
            gst = [P.sb(f"gst{i}", [128, 12], F32) for i in range(2)]; b_gst = [Buf(), Buf()]
            t1 = P.sb("t1", [32, 512], F32); b_t1 = Buf()
            t2 = P.sb("t2", [32, 512], F32); b_t2 = Buf()
            pp = [P.ps(f"pp{i}", [128, 512]) for i in range(3)]; b_pp = [Buf(excl=True) for _ in range(3)]
            psw = [P.ps(f"psw{i}", [128, 512]) for i in range(2)]; b_psw = [Buf(excl=True), Buf(excl=True)]
            pv = [P.ps(f"pv{i}", [128, 512]) for i in range(2)]; b_pv = [Buf(excl=True), Buf(excl=True)]
            ic = 0
            iv = 0
            for tt in range(n_tt):
                t0 = tt * 512
                x_ = xb[tt % 2]; bx = b_xb[tt % 2]
                for q in range(2):
                    P.dma(x_[:, q * 16:(q + 1) * 16, :], xTv[:, q * 16:(q + 1) * 16, t0:t0 + 512], writes=[bx], eng='pool')
                c_ = cs[tt % 2]; bc = b_csb[tt % 2]
                P.dma(c_[:, 0, :], cos_d[:, t0:t0 + 512], reads=[b_cs], writes=[bc])
                P.dma(c_[:, 1, :], sin_d[:, t0:t0 + 512], reads=[b_cs], writes=[bc])
                for c in range(nT if 'T' not in SKIP else 0):
                    ps_ = pp[ic % 3]; bp = b_pp[ic % 3]
                    sg = stg[ic % 3]; bs = b_stg[ic % 3]
                    for k in range(32):
                        P.op('pe', lambda e: e.matmul(ps_[:, :], lhsT=wb[:, k, c * 128:(c + 1) * 128], rhs=x_[:, k, :],
                                                      start=(k == 0), stop=(k == 31)), [b_wb, bx], [bp])
                    P.op('act', lambda e: e.copy(out=sg[:, :], in_=ps_[:, :]), [bp], [bs])
                    if ropef[c] and 'R' not in SKIP:
                        sw_ = psw[ic % 2]; bw_ = b_psw[ic % 2]
                        if '1' not in SKIP:
                            P.op('pe', lambda e: e.matmul(sw_[0:32, :], lhsT=swm[:, :], rhs=sg[:, :], start=True, stop=True),
                                 [bs, b_swm], [bw_])
                        if '2' not in SKIP:
                            P.op('dve', lambda e: e.tensor_tensor(out=t1[:, :], in0=ps_[0:32, :], in1=c_[:, 0, :], op=ALU.mult),
                                 [bp, bc], [b_t1])
                        if '3' not in SKIP:
                            P.op('dve', lambda e: e.tensor_tensor(out=t2[:, :], in0=sw_[0:32, :], in1=c_[:, 1, :], op=ALU.mult),
                                 [bw_, bc], [b_t2])
                        if '4' not in SKIP:
                            P.op('dve', lambda e: e.tensor_tensor(out=sg[0:32, :], in0=t1[:, :], in1=t2[:, :], op=ALU.add),
                                 [b_t1, b_t2], [bs])
                    P.dma(pT_d[pTb + c, :, t0:t0 + 512], sg[:, :], reads=[bs], writes=[b_pT[pTb + c]])
                    ic += 1
                for sub in range(0 if 'V' not in SKIP else 4, 4):
                    pv_ = pv[iv % 2]; bpv = b_pv[iv % 2]
                    vs_ = vst[iv % 2]; bvs = b_vst[iv % 2]
                    for k in range(32):
                        P.op('pe', lambda e: e.matmul(pv_[:, 0:nVg], lhsT=x_[:, k, sub * 128:(sub + 1) * 128],
                                                      rhs=wb[:, k, nT * 128:nT * 128 + nVg],
                                                      start=(k == 0), stop=(k == 31)), [b_wb, bx], [bpv])
                    P.op('act', lambda e: e.copy(out=vs_[:, 0:nV], in_=pv_[:, 0:nV]), [bpv], [bvs])
                    r0 = t0 + sub * 128
                    P.dma(pV_d[pVi, r0:r0 + 128, :], vs_[:, :], reads=[bvs], writes=[b_pV[pVi]])
                    if has_gate and 'G' not in SKIP:
                        g_ = gst[iv % 2]; bg = b_gst[iv % 2]
                        P.op('act', lambda e: e.activation(out=g_[:, :], in_=pv_[:, nV:nV + 12], func=AF.Sigmoid), [bpv], [bg])
                        P.dma(gate_d[r0:r0 + 128, :], g_[:, :], reads=[bg], writes=[b_gate])
                    iv += 1
            P.pop_scope()

        if stop_after == 'proj':
            P.emit()
            return nc

        def ld_v(v_sb, b_v, pvi, c0, dv):
            src = pV_d[pvi].rearrange("(t p) c -> p t c", p=128)
            for q in range(4):
                P.dma(v_sb[:, q * 16:(q + 1) * 16, 0:dv], src[:, q * 16:(q + 1) * 16, c0:c0 + dv], reads=[b_pV[pvi]], writes=[b_v])
            P.op('pool', lambda e: e.memset(v_sb[:, :, dv:dv + 1], 1.0), [], [b_v])

        def causal_tiles(qt, kTs, b_k, v_sb, b_v, mm_fn=None):
            tl = []
            for kt in range(4 * qt + 4):
                a = kt - 4 * qt
                t = dict(kT=kTs[:, kt * 128:(kt + 1) * 128], v=v_sb[:, kt, :], reads=[b_k, b_v],
                         sel=_sel_causal(qt * 512, kt * 128) if a >= 0 else None,
                         subs=[s_ for s_ in range(4) if a <= s_])
                if mm_fn is not None:
                    t['mm'] = mm_fn(kt)
                tl.append(t)
            return tl

        def recip_col(rz, b_rz, src_ap, src_bufs):
            P.op('dve', lambda e: e.tensor_scalar(out=rz[:, 0:1], in0=src_ap, scalar1=1e-30, scalar2=None, op0=ALU.max),
                 src_bufs, [b_rz])
            P.op('dve', lambda e: e.reciprocal(out=rz[:, 0:1], in_=rz[:, 0:1]), [b_rz], [b_rz])

        qts = range(n_qt)

        if 'D' in mixers:
            P.push_scope()
            S_ps = [P.ps(f"S{i}", [128, 512]) for i in range(4)]; bS = [Buf(excl=True) for _ in range(4)]
            O_ps = [P.ps(f"O{i}", [128, 512]) for i in range(4)]; bO = [Buf(excl=True) for _ in range(4)]
            PT = [P.sb(f"PT{i}", [128, 512], BF16) for i in range(5)]; bPT = [Buf() for _ in range(5)]
            kT = [P.sb(f"dk{m}", [128, S], BF16) for m in range(2)]; b_kT = [Buf(), Buf()]
            for m in range(2):
                P.dma(kT[m][:, :], pT_d[10 + m], reads=[b_pT[10 + m]], writes=[b_kT[m]])
            v_sb = P.sb("dv", [128, 64, 257], BF16); b_v = Buf()
            ld_v(v_sb, b_v, 1, 0, 256)
            lv = P.sb("lv", [128, 512], F32); b_lv = Buf()
            P.dma(lv[:, :], dlam.to_broadcast([128, 512]), writes=[b_lv])
            lc = P.sb("lc", [128, 2], F32); b_lc = Buf()
            P.dma(lc[:, :], c_lam.to_broadcast([128, 2]), writes=[b_lc])
            gsc = P.sb("gsc", [128, 256], F32); b_gsc = Buf()
            P.dma(gsc[:, :], subg.to_broadcast([128, 256]), writes=[b_gsc])
            P.op('dve', lambda e: e.tensor_scalar(out=gsc[:, :], in0=gsc[:, :], scalar1=lc[:, 1:2], scalar2=None, op0=ALU.mult),
                 [b_gsc, b_lc], [b_gsc])
            pr = P.sb("pr", [128, 256], F32); b_pr = Buf()
            sm = P.sb("sm", [128, 4], F32); b_sm = Buf()
            P.op('dve', lambda e: e.tensor_tensor(out=pr[:, 0:128], in0=lv[:, 0:128], in1=lv[:, 128:256], op=ALU.mult), [b_lv], [b_pr])
            P.op('dve', lambda e: e.tensor_tensor(out=pr[:, 128:256], in0=lv[:, 256:384], in1=lv[:, 384:512], op=ALU.mult), [b_lv], [b_pr])
            P.op('dve', lambda e: e.reduce_sum(out=sm[:, 0:1], in_=pr[:, 0:128], axis=AX.X), [b_pr], [b_sm])
            P.op('dve', lambda e: e.reduce_sum(out=sm[:, 1:2], in_=pr[:, 128:256], axis=AX.X), [b_pr], [b_sm])
            P.op('act', lambda e: e.activation(out=sm[:, 0:2], in_=sm[:, 0:2], func=AF.Exp), [b_sm], [b_sm])
            P.op('dve', lambda e: e.tensor_tensor(out=sm[:, 2:3], in0=sm[:, 1:2], in1=sm[:, 0:1], op=ALU.subtract), [b_sm], [b_sm])
            P.op('dve', lambda e: e.tensor_tensor(out=sm[:, 2:3], in0=sm[:, 2:3], in1=lc[:, 0:1], op=ALU.subtract), [b_sm, b_lc], [b_sm])
            qsb = [P.sb(f"dq{i}", [128, 2, 512], BF16) for i in range(2)]; b_q = [Buf(), Buf()]
            o0 = P.sb("o0", [128, 4, 256], F32); b_o0 = Buf()
            ot = P.sb("ot", [128, 4, 256], F32); b_ot = Buf()
            ss4 = P.sb("ss4", [128, 4], F32); b_ss4 = Buf()
            sq = P.sb("sq", [128, 256], F32); b_sq = Buf()
            rz = P.sb("rz", [128, 4], F32); b_rz = Buf()
            outst = [P.sb(f"dout{i}", [128, 4, 256], F32) for i in range(2)]; b_out = [Buf(), Buf()]
            for qt in qts:
                q_ = qsb[qt % 2]; bq = b_q[qt % 2]
                for m in range(2):
                    P.dma(q_[:, m, :], pT_d[8 + m, :, qt * 512:(qt + 1) * 512], reads=[b_pT[8 + m]], writes=[bq])
                ost = outst[qt % 2]; bost = b_out[qt % 2]
                for m in range(2):
                    tl = causal_tiles(qt, kT[m], b_kT[m], v_sb, b_v)
                    attn_block(P, "d", q_[:, m, :], tl, S_ps, bS, PT, bPT, O_ps, bO, 257, [bq])
                    for s_ in range(4):
                        recip_col(rz, b_rz, O_ps[s_][:, 256:257], [bO[s_]])
                        if m == 0:
                            P.op('dve', lambda e: e.tensor_scalar(out=o0[:, s_, :], in0=O_ps[s_][:, 0:256], scalar1=rz[:, 0:1],
                                                                  scalar2=None, op0=ALU.mult), [bO[s_], b_rz], [b_o0])
                        else:
                            P.op('dve', lambda e: e.tensor_tensor(out=rz[:, 1:2], in0=rz[:, 0:1], in1=sm[:, 2:3], op=ALU.mult),
                                 [b_rz, b_sm], [b_rz])
                            P.op('dve', lambda e: e.scalar_tensor_tensor(out=ot[:, s_, :], in0=O_ps[s_][:, 0:256], scalar=rz[:, 1:2],
                                                                         in1=o0[:, s_, :], op0=ALU.mult, op1=ALU.add),
                                 [bO[s_], b_rz, b_o0], [b_ot])
                            P.op('dve', lambda e: e.tensor_tensor(out=sq[:, :], in0=ot[:, s_, :], in1=ot[:, s_, :], op=ALU.mult), [b_ot], [b_sq])
                            P.op('dve', lambda e: e.reduce_sum(out=ss4[:, s_:s_ + 1], in_=sq[:, :], axis=AX.X), [b_sq], [b_ss4])
                    if m == 1:
                        P.op('dve', lambda e: e.tensor_scalar(out=ss4[:, :], in0=ss4[:, :], scalar1=1.0 / 256, scalar2=1e-5,
                                                              op0=ALU.mult, op1=ALU.add), [b_ss4], [b_ss4])
                        P.op('act', lambda e: e.sqrt(out=ss4[:, :], in_=ss4[:, :]), [b_ss4], [b_ss4])
                        P.op('dve', lambda e: e.reciprocal(out=ss4[:, :], in_=ss4[:, :]), [b_ss4], [b_ss4])
                        for s_ in range(4):
                            P.op('dve', lambda e: e.scalar_tensor_tensor(out=ost[:, s_, :], in0=ot[:, s_, :], scalar=ss4[:, s_:s_ + 1],
                                                                         in1=gsc[:, :], op0=ALU.mult, op1=ALU.mult),
                                 [b_ot, b_ss4, b_gsc], [bost])
                P.dma(mix[qt * 512:(qt + 1) * 512, 512:768].rearrange("(s p) c -> p s c", p=128), ost[:, :, :], reads=[bost])
            P.pop_scope()
        if stop_after == 'diff':
            P.emit()
            return nc

        if 'M' in mixers:
            P.push_scope()
            S_ps = [P.ps(f"S{i}", [128, 512]) for i in range(2)]; bS = [Buf(excl=True) for _ in range(2)]
            O_ps = [P.ps(f"O{i}", [128, 512]) for i in range(4)]; bO = [Buf(excl=True) for _ in range(4)]
            aux = P.ps("aux", [128, 512]); b_aux = Buf(excl=True)
            aux2 = P.ps("aux2", [128, 512]); b_aux2 = Buf(excl=True)
            S_ps = S_ps + [aux, aux2]; bS = bS + [b_aux, b_aux2]
            PT = [P.sb(f"PT{i}", [128, 512], BF16) for i in range(5)]; bPT = [Buf() for _ in range(5)]
            kT = [P.sb(f"mk{m}", [128, S], BF16) for m in range(2)]; b_kT = [Buf(), Buf()]
            v_sb = [P.sb(f"mv{m}", [128, 64, 129], BF16) for m in range(2)]; b_v = [Buf(), Buf()]
            kmf = P.sb("kmf", [128, 32], F32); b_kmf = Buf()
            kmb = [P.sb(f"kmb{m}", [128, 32], BF16) for m in range(2)]; b_kmb = [Buf(), Buf()]
            for m in range(2):
                P.dma(kT[m][:, :], pT_d[14 + m], reads=[b_pT[14 + m]], writes=[b_kT[m]])
                ld_v(v_sb[m], b_v[m], 2, m * 128, 128)
                P.op('dve', lambda e: e.tensor_reduce(out=kmf[:, :], in_=kT[m][:, :].rearrange("p (b k) -> p b k", k=256),
                                                      axis=AX.X, op=ALU.add), [b_kT[m]], [b_kmf])
                P.op('dve', lambda e: e.tensor_scalar(out=kmb[m][:, :], in0=kmf[:, :], scalar1=1.0 / 256, scalar2=None, op0=ALU.mult),
                     [b_kmf], [b_kmb[m]])
            indm = P.sb("indm", [32, S], BF16); b_indm = Buf()
            P.op('pool', lambda e: e.memset(indm[:, :], 1.0), [], [b_indm])
            P.op('pool', lambda e: e.affine_select(out=indm[:, :], in_=indm[:, :], pattern=[[1, S]], compare_op=ALU.is_ge, fill=0.0,
                                                   base=0, channel_multiplier=-256), [b_indm], [b_indm])
            P.op('pool', lambda e: e.affine_select(out=indm[:, :], in_=indm[:, :], pattern=[[-1, S]], compare_op=ALU.is_ge, fill=0.0,
                                                   base=255, channel_multiplier=256), [b_indm], [b_indm])
            qsb = [P.sb(f"mq{i}", [128, 2, 512], BF16) for i in range(2)]; b_q = [Buf(), Buf()]
            sc4 = [P.sb(f"sc4{i}", [128, 32], F32) for i in range(4)]; b_sc4 = [Buf() for _ in range(4)]
            m84 = [P.sb(f"m84{i}", [128, 8], F32) for i in range(4)]; b_m84 = [Buf() for _ in range(4)]
            nm4 = [P.sb(f"nm4{i}", [128, 32], F32) for i in range(4)]; b_nm4 = [Buf() for _ in range(4)]
            negT = [P.sb(f"negT{i}", [32, 512], BF16) for i in range(2)]; b_negT = [Buf(), Buf()]
            rz = P.sb("rz", [128, 4], F32); b_rz = Buf()
            outst = [P.sb(f"mout{i}", [128, 4, 256], F32) for i in range(2)]; b_out = [Buf(), Buf()]
            for qt in qts:
                q_ = qsb[qt % 2]; bq = b_q[qt % 2]
                for m in range(2):
                    P.dma(q_[:, m, :], pT_d[12 + m, :, qt * 512:(qt + 1) * 512], reads=[b_pT[12 + m]], writes=[bq])
                ost = outst[qt % 2]; bost = b_out[qt % 2]
                for m in range(2):
                    ng = negT[m]; bng = b_negT[m]
                    R4 = range(4)
                    jbs = [(qt * 512 + s_ * 128) // 256 for s_ in R4]
                    for s_ in R4:
                        P.op('pe', lambda e: e.matmul(S_ps[s_][:, 0:32], lhsT=q_[:, m, s_ * 128:(s_ + 1) * 128], rhs=kmb[m][:, :],
                                                      start=True, stop=True), [bq, b_kmb[m]], [bS[s_]])
                    for s_ in R4:
                        P.op('dve', lambda e: e.tensor_copy(out=sc4[s_][:, :], in_=S_ps[s_][:, 0:32]), [bS[s_]], [b_sc4[s_]])
                    for s_ in R4:
                        P.op('dve', lambda e: e.memset(sc4[s_][:, jbs[s_]:32], -1e30), [], [b_sc4[s_]])
                    for s_ in R4:
                        P.op('dve', lambda e: e.max(out=m84[s_][:, :], in_=sc4[s_][:, :]), [b_sc4[s_]], [b_m84[s_]])
                    for s_ in R4:
                        P.op('dve', lambda e: e.tensor_scalar(out=nm4[s_][:, :], in0=sc4[s_][:, :], scalar1=m84[s_][:, 2:3], scalar2=None,
                                                              op0=ALU.is_ge), [b_sc4[s_], b_m84[s_]], [b_nm4[s_]])
                    for s_ in R4:
                        P.op('dve', lambda e: e.memset(nm4[s_][:, jbs[s_]:32], 0.0), [], [b_nm4[s_]])
                    for s_ in R4:
                        P.op('dve', lambda e: e.memset(nm4[s_][:, jbs[s_]:jbs[s_] + 1], 1.0), [], [b_nm4[s_]])
                    for s_ in R4:
                        P.op('dve', lambda e: e.tensor_scalar(out=nm4[s_][:, :], in0=nm4[s_][:, :], scalar1=NEGM, scalar2=-NEGM,
                                                              op0=ALU.mult, op1=ALU.add), [b_nm4[s_]], [b_nm4[s_]])
                    for s_ in R4:
                        P.op('pe', lambda e: e.transpose(out=S_ps[s_][0:32, 0:128], in_=nm4[s_][:, :], identity=ident[:, :]),
                             [b_nm4[s_], b_ident], [bS[s_]])
                    for s_ in R4:
                        P.op('act', lambda e: e.copy(out=ng[:, s_ * 128:(s_ + 1) * 128], in_=S_ps[s_][0:32, 0:128]), [bS[s_]], [bng])
                    tl = causal_tiles(qt, kT[m], b_kT[m], v_sb[m], b_v[m],
                                      mm_fn=lambda kt: (indm[:, kt * 128:(kt + 1) * 128], ng[:, :], [b_indm, bng]))
                    attn_block(P, "m", q_[:, m, :], tl, S_ps, bS, PT, bPT, O_ps, bO, 129, [bq])
                    for s_ in range(4):
                        recip_col(rz, b_rz, O_ps[s_][:, 128:129], [bO[s_]])
                        P.op('dve', lambda e: e.tensor_scalar(out=ost[:, s_, m * 128:(m + 1) * 128], in0=O_ps[s_][:, 0:128],
                                                              scalar1=rz[:, 0:1], scalar2=None, op0=ALU.mult), [bO[s_], b_rz], [bost])
                P.dma(mix[qt * 512:(qt + 1) * 512, 768:1024].rearrange("(s p) c -> p s c", p=128), ost[:, :, :], reads=[bost])
            P.pop_scope()
        if stop_after == 'moba':
            P.emit()
            return nc

        if 'N' in mixers:
            P.push_scope()
            S_ps = [P.ps(f"S{i}", [128, 512]) for i in range(2)]; bS = [Buf(excl=True) for _ in range(2)]
            O_ps = [P.ps(f"O{i}", [128, 512]) for i in range(4)]; bO = [Buf(excl=True) for _ in range(4)]
            aux = P.ps("aux", [128, 512]); b_aux = Buf(excl=True)
            aux2 = P.ps("aux2", [128, 512]); b_aux2 = Buf(excl=True)
            S_ps = S_ps + [aux, aux2]; bS = bS + [b_aux, b_aux2]
            PT = [P.sb(f"PT{i}", [128, 512], BF16) for i in range(5)]; bPT = [Buf() for _ in range(5)]
            kcT = P.sb("kcT", [128, 512], BF16); b_kcT = Buf()
            vc = P.sb("vc", [128, 4, 129], BF16); b_vc = Buf()
            P.push_scope()
            for which in range(2):
                cin = P.sb("cin", [128, S], BF16); b_cin = Buf()
                P.dma(cin[:, :], pT_d[4 + which], reads=[b_pT[4 + which]], writes=[b_cin])
                w1 = P.sb("w1", [128, 32, 128], BF16); b_w1 = Buf()
                P.dma(w1[:, :, :], cw1[which].rearrange("(l d) h -> d l h", d=128), writes=[b_w1], eng='pool')
                w2 = P.sb("w2", [128, 128], BF16); b_w2 = Buf()
                P.dma(w2[:, :], cw2[which], writes=[b_w2], eng='pool')
                posT = P.sb("posT", [128, 32], BF16); b_posT = Buf()
                P.dma(posT[:, :], cpos[which].rearrange("l d -> d l"), writes=[b_posT], eng='pool', allow_slow_non_contiguous=True)
                for l in range(32):
                    P.op('pe', lambda e: e.matmul(aux[:, 0:511], lhsT=w1[:, l, :], rhs=cin[:, l:l + 16 * 510 + 1:16],
                                                  start=(l == 0), stop=(l == 31)), [b_w1, b_cin], [b_aux])
                for l in range(32):
                    P.op('pe', lambda e: e.matmul(aux2[:, 0:1], lhsT=w1[:, l, :], rhs=posT[:, l:l + 1],
                                                  start=(l == 0), stop=(l == 31)), [b_w1, b_posT], [b_aux2])
                pb = P.sb("pb", [128, 1], F32); b_pb = Buf()
                P.op('dve', lambda e: e.tensor_copy(out=pb[:, :], in_=aux2[:, 0:1]), [b_aux2], [b_pb])
                u = P.sb("u", [128, 512], F32); b_u = Buf()
                w_ = P.sb("w_", [128, 512], F32); b_w_ = Buf()
                gel = P.sb("gel", [128, 512], BF16); b_gel = Buf()
                P.op('dve', lambda e: e.memset(gel[:, 511:512], 0.0), [], [b_gel])
                P.op('dve', lambda e: e.tensor_scalar(out=u[:, 0:511], in0=aux[:, 0:511], scalar1=pb[:, 0:1], scalar2=None, op0=ALU.add),
                     [b_aux, b_pb], [b_u])
                P.op('dve', lambda e: e.tensor_tensor(out=w_[:, 0:511], in0=u[:, 0:511], in1=u[:, 0:511], op=ALU.mult), [b_u], [b_w_])
                P.op('dve', lambda e: e.tensor_scalar(out=w_[:, 0:511], in0=w_[:, 0:511], scalar1=0.044715, scalar2=1.0,
                                                      op0=ALU.mult, op1=ALU.add), [b_w_], [b_w_])
                P.op('dve', lambda e: e.tensor_tensor(out=w_[:, 0:511], in0=w_[:, 0:511], in1=u[:, 0:511], op=ALU.mult), [b_w_, b_u], [b_w_])
                P.op('act', lambda e: e.activation(out=w_[:, 0:511], in_=w_[:, 0:511], func=AF.Tanh, scale=0.7978845608028654),
                     [b_w_], [b_w_])
                P.op('dve', lambda e: e.scalar_tensor_tensor(out=w_[:, 0:511], in0=w_[:, 0:511], scalar=1.0, in1=u[:, 0:511],
                                                             op0=ALU.add, op1=ALU.mult), [b_w_, b_u], [b_w_])
                P.op('dve', lambda e: e.tensor_scalar(out=gel[:, 0:511], in0=w_[:, 0:511], scalar1=0.5, scalar2=None, op0=ALU.mult),
                     [b_w_], [b_gel])
                if which == 0:
                    P.op('pe', lambda e: e.matmul(aux[:, 0:512], lhsT=w2[:, :], rhs=gel[:, :], start=True, stop=True), [b_w2, b_gel], [b_aux])
                    P.op('act', lambda e: e.copy(out=kcT[:, :], in_=aux[:, 0:512]), [b_aux], [b_kcT])
                else:
                    for nt in range(4):
                        P.op('pe', lambda e: e.matmul(aux[:, 0:128], lhsT=gel[:, nt * 128:(nt + 1) * 128], rhs=w2[:, :], start=True, stop=True),
                             [b_w2, b_gel], [b_aux])
                        P.op('act', lambda e: e.copy(out=vc[:, nt, 0:128], in_=aux[:, 0:128]), [b_aux], [b_vc])
                    P.op('pool', lambda e: e.memset(vc[:, :, 128:129], 1.0), [], [b_vc])
            P.pop_scope()
            ksT = P.sb("ksT", [128, S], BF16); b_ksT = Buf()
            kwT = P.sb("kwT", [128, S], BF16); b_kwT = Buf()
            P.dma(ksT[:, :], pT_d[6], reads=[b_pT[6]], writes=[b_ksT])
            P.dma(kwT[:, :], pT_d[7], reads=[b_pT[7]], writes=[b_kwT])
            vs = P.sb("vs", [128, 64, 129], BF16); b_vs = Buf()
            vw = P.sb("vw", [128, 64, 129], BF16); b_vw = Buf()
            ld_v(vs, b_vs, 0, 0, 128)
            ld_v(vw, b_vw, 0, 128, 128)
            indb = P.sb("indb", [128, S], BF16); b_indb = Buf()
            P.op('pool', lambda e: e.memset(indb[:, :], 1.0), [], [b_indb])
            P.op('pool', lambda e: e.affine_select(out=indb[:, :], in_=indb[:, :], pattern=[[1, S]], compare_op=ALU.is_ge, fill=0.0,
                                                   base=0, channel_multiplier=-64), [b_indb], [b_indb])
            P.op('pool', lambda e: e.affine_select(out=indb[:, :], in_=indb[:, :], pattern=[[-1, S]], compare_op=ALU.is_ge, fill=0.0,
                                                   base=63, channel_multiplier=64), [b_indb], [b_indb])
            fbig = P.sb("fbig", [128, 256], F32); b_fbig = Buf()
            P.dma(fbig[:, :], c_fbig, writes=[b_fbig])
            qsb = [P.sb(f"nq{i}", [128, 4, 512], BF16) for i in range(2)]; b_q = [Buf(), Buf()]
            gsb = [P.sb(f"ng{i}", [128, 4, 12], F32) for i in range(2)]; b_g = [Buf(), Buf()]
            E4 = [P.sb(f"E4{i}", [128, 512], F32) for i in range(4)]; b_E4 = [Buf() for _ in range(4)]
            pg4 = [P.sb(f"pg4{i}", [128, 512], F32) for i in range(4)]; b_pg4 = [Buf() for _ in range(4)]
            imp4 = [P.sb(f"imp4{i}", [128, 128], F32) for i in range(4)]; b_imp4 = [Buf() for _ in range(4)]
            wk4 = [P.sb(f"wk4{i}", [128, 128], F32) for i in range(4)]; b_wk4 = [Buf() for _ in range(4)]
            m84 = [P.sb(f"m84{i}", [128, 16], F32) for i in range(4)]; b_m84 = [Buf() for _ in range(4)]
            rz4 = [P.sb(f"rz4{i}", [128, 2], F32) for i in range(4)]; b_rz4 = [Buf() for _ in range(4)]
            pg = P.sb("pg", [128, 512], F32); b_pg = Buf()
            imp = P.sb("imp", [128, 128], F32); b_imp = Buf()
            wk = P.sb("wk", [128, 128], F32); b_wk = Buf()
            m8 = P.sb("m8", [128, 16], F32); b_m8 = Buf()
            rz = P.sb("rz", [128, 4], F32); b_rz = Buf()
            negT = P.sb("negT", [128, 512], BF16); b_negT = Buf()
            outst = [P.sb(f"nout{i}", [128, 4, 512], F32) for i in range(2)]; b_out = [Buf(), Buf()]
            for qt in qts:
                q_ = qsb[qt % 2]; bq = b_q[qt % 2]
                g_ = gsb[qt % 2]; bg = b_g[qt % 2]
                for g in range(4):
                    P.dma(q_[:, g, :], pT_d[g, :, qt * 512:(qt + 1) * 512], reads=[b_pT[g]], writes=[bq])
                P.dma(g_[:, :, :], gate_d[qt * 512:(qt + 1) * 512, :].rearrange("(s p) c -> p s c", p=128), reads=[b_gate], writes=[bg])
                ost = outst[qt % 2]; bost = b_out[qt % 2]
                R4 = range(4)
                q0s = [qt * 512 + s_ * 128 for s_ in R4]
                for g in range(4):
                    for s_ in R4:
                        P.op('pe', lambda e: e.matmul(S_ps[s_][:, :], lhsT=q_[:, g, s_ * 128:(s_ + 1) * 128], rhs=kcT[:, :], start=True, stop=True),
                             [bq, b_kcT], [bS[s_]])
                    for s_ in R4:
                        P.op('act', lambda e: e.activation(out=E4[s_][:, :], in_=S_ps[s_][:, :], func=AF.Exp, scale=SCALE), [bS[s_]], [b_E4[s_]])
                    for s_ in R4:
                        P.op('pool', lambda e: e.affine_select(out=E4[s_][:, :], in_=E4[s_][:, :], pattern=[[-16, 512]], compare_op=ALU.is_ge,
                                                               fill=0.0, base=q0s[s_] - 31, channel_multiplier=1), [b_E4[s_]], [b_E4[s_]])
                    for s_ in R4:
                        P.op('dve', lambda e: e.reduce_sum(out=rz4[s_][:, 0:1], in_=E4[s_][:, :], axis=AX.X), [b_E4[s_]], [b_rz4[s_]])
                    for s_ in R4:
                        P.op('dve', lambda e: e.tensor_scalar(out=rz4[s_][:, 0:1], in0=rz4[s_][:, 0:1], scalar1=1e-30, scalar2=None, op0=ALU.max),
                             [b_rz4[s_]], [b_rz4[s_]])
                    for s_ in R4:
                        P.op('dve', lambda e: e.reciprocal(out=rz4[s_][:, 0:1], in_=rz4[s_][:, 0:1]), [b_rz4[s_]], [b_rz4[s_]])
                    for s_ in R4:
                        if g == 0:
                            P.op('dve', lambda e: e.tensor_scalar(out=pg4[s_][:, :], in0=E4[s_][:, :], scalar1=rz4[s_][:, 0:1], scalar2=None,
                                                                  op0=ALU.mult), [b_E4[s_], b_rz4[s_]], [b_pg4[s_]])
                        else:
                            P.op('dve', lambda e: e.scalar_tensor_tensor(out=pg4[s_][:, :], in0=E4[s_][:, :], scalar=rz4[s_][:, 0:1],
                                                                         in1=pg4[s_][:, :], op0=ALU.mult, op1=ALU.add),
                                 [b_E4[s_], b_rz4[s_], b_pg4[s_]], [b_pg4[s_]])
                for s_ in R4:
                    P.op('dve', lambda e: e.tensor_reduce(out=imp4[s_][:, :], in_=pg4[s_][:, :].rearrange("p (b k) -> p b k", k=4), axis=AX.X,
                                                          op=ALU.add), [b_pg4[s_]], [b_imp4[s_]])
                for s_ in R4:
                    P.op('dve', lambda e: e.tensor_tensor(out=imp4[s_][:, 1:128], in0=imp4[s_][:, 1:128], in1=pg4[s_][:, 3:508:4], op=ALU.add),
                         [b_imp4[s_], b_pg4[s_]], [b_imp4[s_]])
                for s_ in R4:
                    st0 = 128 - q0s[s_] // 64
                    P.op('dve', lambda e: e.tensor_tensor(out=imp4[s_][:, :], in0=imp4[s_][:, :], in1=fbig[:, st0:st0 + 128], op=ALU.add),
                         [b_imp4[s_], b_fbig], [b_imp4[s_]])
                for s_ in R4:
                    P.op('dve', lambda e: e.tensor_scalar(out=imp4[s_][:, 0:1], in0=imp4[s_][:, 0:1], scalar1=100.0, scalar2=None, op0=ALU.add),
                         [b_imp4[s_]], [b_imp4[s_]])
                for s_ in R4:
                    P.op('dve', lambda e: e.max(out=m84[s_][:, 0:8], in_=imp4[s_][:, :]), [b_imp4[s_]], [b_m84[s_]])
                for s_ in R4:
                    P.op('dve', lambda e: e.match_replace(out=wk4[s_][:, :], in_to_replace=m84[s_][:, 0:8], in_values=imp4[s_][:, :],
                                                          imm_value=-1e30), [b_imp4[s_], b_m84[s_]], [b_wk4[s_]])
                for s_ in R4:
                    P.op('dve', lambda e: e.max(out=m84[s_][:, 8:16], in_=wk4[s_][:, :]), [b_wk4[s_]], [b_m84[s_]])
                for s_ in R4:
                    P.op('dve', lambda e: e.tensor_scalar(out=wk4[s_][:, :], in0=imp4[s_][:, :], scalar1=m84[s_][:, 15:16], scalar2=NEGM,
                                                          op0=ALU.is_ge, op1=ALU.mult), [b_imp4[s_], b_m84[s_]], [b_wk4[s_]])
                for s_ in R4:
                    P.op('dve', lambda e: e.tensor_scalar(out=wk4[s_][:, :], in0=wk4[s_][:, :], scalar1=-NEGM, scalar2=None, op0=ALU.add),
                         [b_wk4[s_]], [b_wk4[s_]])
                for s_ in R4:
                    P.op('pe', lambda e: e.transpose(out=S_ps[s_][:, 0:128], in_=wk4[s_][:, :], identity=ident[:, :]), [b_wk4[s_], b_ident], [bS[s_]])
                for s_ in R4:
                    P.op('act', lambda e: e.copy(out=negT[:, s_ * 128:(s_ + 1) * 128], in_=S_ps[s_][:, 0:128]), [bS[s_]], [b_negT])
                for g in range(4):
                    qg = q_[:, g, :]
                    for br in range(3):
                        if br == 0:
                            tl = []
                            for nt in range(4):
                                if qt * 512 + 511 < 2048 * nt + 31:
                                    continue
                                full = qt * 512 >= 16 * (nt * 128 + 127) + 31
                                tl.append(dict(kT=kcT[:, nt * 128:(nt + 1) * 128], v=vc[:, nt, :], reads=[b_kcT, b_vc],
                                               sel=None if full else dict(pattern=[[1, 512]], cm=-16, base=qt * 512 - 2048 * nt - 31),
                                               subs=[x_ for x_ in range(4) if qt * 512 + x_ * 128 + 127 >= 2048 * nt + 31]))
                        elif br == 1:
                            tl = causal_tiles(qt, ksT, b_ksT, vs, b_vs,
                                              mm_fn=lambda kt: (indb[:, kt * 128:(kt + 1) * 128], negT[:, :], [b_indb, b_negT]))
                        else:
                            tl = []
                            for a in range(8):
                                kt = 4 * qt - 4 + a
                                if kt < 0:
                                    continue
                                if a < 4:
                                    sel = dict(pattern=[[-1, 512]], cm=1, base=kt * 128 - qt * 512 + 511)
                                    subs = [x_ for x_ in range(4) if a >= x_]
                                else:
                                    sel = _sel_causal(qt * 512, kt * 128)
                                    subs = [x_ for x_ in range(4) if a - 4 <= x_]
                                tl.append(dict(kT=kwT[:, kt * 128:(kt + 1) * 128], v=vw[:, kt, :], reads=[b_kwT, b_vw], sel=sel, subs=subs))
                        attn_block(P, "n", qg, tl, S_ps, bS, PT, bPT, O_ps, bO, 129, [bq])
                        for s_ in range(4):
                            recip_col(rz, b_rz, O_ps[s_][:, 128:129], [bO[s_]])
                            P.op('dve', lambda e: e.tensor_tensor(out=rz[:, 1:2], in0=rz[:, 0:1], in1=g_[:, s_, 3 * g + br:3 * g + br + 1],
                                                                  op=ALU.mult), [b_rz, bg], [b_rz])
                            dst = ost[:, s_, g * 128:(g + 1) * 128]
                            if br == 0:
                                P.op('dve', lambda e: e.tensor_scalar(out=dst, in0=O_ps[s_][:, 0:128], scalar1=rz[:, 1:2], scalar2=None,
                                                                      op0=ALU.mult), [bO[s_], b_rz], [bost])
                            else:
                                P.op('dve', lambda e: e.scalar_tensor_tensor(out=dst, in0=O_ps[s_][:, 0:128], scalar=rz[:, 1:2], in1=dst,
                                                                             op0=ALU.mult, op1=ALU.add), [bO[s_], b_rz, bost], [bost])
                P.dma(mix[qt * 512:(qt + 1) * 512, 0:512].rearrange("(s p) c -> p s c", p=128), ost[:, :, :], reads=[bost])
            P.pop_scope()
        P.emit()
        return nc


ALPHA = 4 ** 0.25
TOK = 2048


def build_ffn(n_pass=4, n_exp=32, debug=False):
    nc = bass.Bass("TRN2", target_bir_lowering=False)
    xres = nc.dram_tensor("xres", [TOK, 4096], F32, kind="ExternalInput").ap()
    mixT = nc.dram_tensor("mixT", [4096, TOK], F32, kind="ExternalInput").ap()
    wout = nc.dram_tensor("wout", [4096, 4096], F32, kind="ExternalInput").ap()
    lnp = nc.dram_tensor("lnp", [4, 4096], F32, kind="ExternalInput").ap()
    wr = nc.dram_tensor("wr", [4096, 32], F32, kind="ExternalInput").ap()
    br = nc.dram_tensor("br", [1, 32], F32, kind="ExternalInput").ap()
    wgu = nc.dram_tensor("wgu", [32, 4, 128, 32 * 256], F32, kind="ExternalInput").ap()
    bgu = nc.dram_tensor("bgu", [128, 256], F32, kind="ExternalInput").ap()
    wdn = nc.dram_tensor("wdn", [32, 4, 128, 4 * 1024], F32, kind="ExternalInput").ap()
    bdn = nc.dram_tensor("bdn", [32, 4096], F32, kind="ExternalInput").ap()
    y = nc.dram_tensor("y", [TOK, 4096], F32, kind="ExternalOutput").ap()
    dbg = nc.dram_tensor("dbg", [TOK, 32], F32, kind="ExternalOutput").ap() if debug else None
    with ExitStack() as st:
        P = Prog(nc, st)
        ident = P.sb("ident", [128, 128], F32); b_ident = Buf()
        P.op('pool', lambda e: e.memset(ident[:, :], 1.0), [], [b_ident])
        P.op('pool', lambda e: e.affine_select(out=ident[:, :], in_=ident[:, :], pattern=[[-1, 128]],
                                               compare_op=ALU.is_equal, fill=0.0, base=0, channel_multiplier=1),
             [b_ident], [b_ident])
        bgs = P.sb("bgs", [128, 256], F32); b_bgs = Buf()
        P.dma(bgs[:, :], bgu, writes=[b_bgs])
        brs = P.sb("brs", [128, 32], F32); b_brs = Buf()
        P.dma(brs[:, :], br.to_broadcast([128, 32]), writes=[b_brs])
        wrs = P.sb("wrs", [128, 32, 32], F32); b_wrs = Buf()
        P.dma(wrs[:, :, :], wr.rearrange("(k p) e -> p k e", p=128), writes=[b_wrs])
        acc = P.sb("acc", [128, 4, 4096], F32); b_acc = [Buf() for _ in range(4)]
        x1T = P.sb("x1T", [128, 32, 512], BF16); b_x1T = Buf()
        G = P.sb("G", [128, 4, 32], F32); b_G = Buf()
        moutv = mixT.rearrange("(k p) t -> p k t", p=128)
        woutv = wout.rearrange("(k p) n -> p k n", p=128)

        def layer_norm(sub, gi, stat, b_stat, sq, b_sq, gb, b_gb):
            a_ = acc[:, sub, :]
            ba = b_acc[sub]
            P.op('dve', lambda e: e.reduce_sum(out=stat[:, 0:1], in_=a_, axis=AX.X), [ba], [b_stat])
            P.op('dve', lambda e: e.tensor_scalar(out=stat[:, 0:1], in0=stat[:, 0:1], scalar1=-1.0 / 4096, scalar2=None, op0=ALU.mult),
                 [b_stat], [b_stat])
            P.op('dve', lambda e: e.tensor_scalar(out=a_, in0=a_, scalar1=stat[:, 0:1], scalar2=None, op0=ALU.add), [ba, b_stat], [ba])
            P.op('act', lambda e: e.activation(out=sq[:, :], in_=a_, func=AF.Square, accum_out=stat[:, 1:2]), [ba, b_stat], [b_sq, b_stat])
            P.op('dve', lambda e: e.tensor_scalar(out=stat[:, 1:2], in0=stat[:, 1:2], scalar1=1.0 / 4096, scalar2=1e-5,
                                                  op0=ALU.mult, op1=ALU.add), [b_stat], [b_stat])
            P.op('act', lambda e: e.sqrt(out=stat[:, 1:2], in_=stat[:, 1:2]), [b_stat], [b_stat])
            P.op('dve', lambda e: e.reciprocal(out=stat[:, 1:2], in_=stat[:, 1:2]), [b_stat], [b_stat])
            for ch in range(4):
                cs_ = slice(ch * 1024, (ch + 1) * 1024)
                P.op('dve', lambda e: e.scalar_tensor_tensor(out=acc[:, sub, cs_], in0=acc[:, sub, cs_], scalar=stat[:, 1:2],
                                                             in1=gb[:, 0, cs_], op0=ALU.mult, op1=ALU.mult), [ba, b_stat, b_gb], [ba])
                P.op('pool', lambda e: e.tensor_tensor(out=acc[:, sub, cs_], in0=acc[:, sub, cs_], in1=gb[:, 1, cs_], op=ALU.add),
                     [ba, b_gb], [ba])

        for ps_i in range(n_pass):
            t0 = ps_i * 512
            P.push_scope()
            mT = P.sb("mT", [128, 32, 512], BF16); b_mT = Buf()
            for q in range(2):
                P.dma(mT[:, q * 16:(q + 1) * 16, :], moutv[:, q * 16:(q + 1) * 16, t0:t0 + 512], writes=[b_mT], eng='pool')
            wo = [P.sb(f"wo{i}", [128, 32, 256], BF16) for i in range(2)]; b_wo = [Buf(), Buf()]
            xr = [P.sb(f"xr{i}", [128, 4, 256], F32) for i in range(2)]; b_xr = [Buf(), Buf()]
            pso = [P.ps(f"pso{i}", [128, 512]) for i in range(2)]; b_pso = [Buf(excl=True), Buf(excl=True)]
            io = 0
            for cc in range(16):
                w_ = wo[cc % 2]; bw = b_wo[cc % 2]
                P.dma(w_[:, :, :], woutv[:, :, cc * 256:(cc + 1) * 256], writes=[bw], eng='pool')
                x_ = xr[cc % 2]; bx = b_xr[cc % 2]
                P.dma(x_[:, :, :], xres[t0:t0 + 512, cc * 256:(cc + 1) * 256].rearrange("(s p) c -> p s c", p=128), writes=[bx])
                for sub in range(4):
                    po = pso[io % 2]; bpo = b_pso[io % 2]
                    for k in range(32):
                        P.op('pe', lambda e: e.matmul(po[:, 0:256], lhsT=mT[:, k, sub * 128:(sub + 1) * 128], rhs=w_[:, k, :],
                                                      start=(k == 0), stop=(k == 31)), [b_mT, bw], [bpo])
                    P.op('dve', lambda e: e.scalar_tensor_tensor(out=acc[:, sub, cc * 256:(cc + 1) * 256], in0=x_[:, sub, :], scalar=ALPHA,
                                                                 in1=po[:, 0:256], op0=ALU.mult, op1=ALU.add), [bx, bpo], [b_acc[sub]])
                    io += 1
            P.pop_scope()
            P.push_scope()
            gb = P.sb("gb", [128, 2, 4096], F32); b_gb = Buf()
            P.dma(gb[:, 0, :], lnp[0:1, :].to_broadcast([128, 4096]), writes=[b_gb])
            P.dma(gb[:, 1, :], lnp[1:2, :].to_broadcast([128, 4096]), writes=[b_gb])
            ptr = [P.ps(f"ptr{i}", [128, 512]) for i in range(2)]; b_ptr = [Buf(excl=True), Buf(excl=True)]
            plg = P.ps("plg", [128, 512]); b_plg = Buf(excl=True)
            pbd = [P.ps(f"pbd{i}", [128, 512]) for i in range(2)]; b_pbd = [Buf(excl=True), Buf(excl=True)]
            stat = P.sb("stat", [128, 2], F32); b_stat = Buf()
            sq = P.sb("sq", [128, 4096], BF16); b_sq = Buf()
            xTf = P.sb("xTf", [128, 32, 128], F32); b_xTf = Buf()
            lg = P.sb("lg", [128, 32], F32); b_lg = Buf()
            m8 = P.sb("m8", [128, 8], F32); b_m8 = Buf()
            ex = P.sb("ex", [128, 32], F32); b_ex = Buf()
            GT = P.sb("GT", [32, 128], F32); b_GT = Buf()
            bds = P.sb("bds", [32, 4096], F32); b_bds = Buf()
            P.dma(bds[:, :], bdn, writes=[b_bds])
            for sub in range(4):
                layer_norm(sub, 0, stat, b_stat, sq, b_sq, gb, b_gb)
                for k4 in range(8):
                    pt_ = ptr[k4 % 2]; bpt = b_ptr[k4 % 2]
                    for i4 in range(4):
                        k = k4 * 4 + i4
                        P.op('pe', lambda e: e.transpose(out=pt_[:, i4 * 128:(i4 + 1) * 128], in_=acc[:, sub, k * 128:(k + 1) * 128],
                                                         identity=ident[:, :]), [b_acc[sub], b_ident], [bpt])
                    P.op('act', lambda e: e.copy(out=x1T[:, k4 * 4:(k4 + 1) * 4, sub * 128:(sub + 1) * 128],
                                                 in_=pt_[:, :].rearrange("p (a b) -> p a b", b=128)), [bpt], [b_x1T])
                    P.op('dve', lambda e: e.tensor_copy(out=xTf[:, k4 * 4:(k4 + 1) * 4, :], in_=pt_[:, :].rearrange("p (a b) -> p a b", b=128)),
                         [bpt], [b_xTf])
                for k in range(32):
                    P.op('pe', lambda e: e.matmul(plg[:, 0:32], lhsT=xTf[:, k, :], rhs=wrs[:, k, :], start=(k == 0), stop=(k == 31)),
                         [b_xTf, b_wrs], [b_plg])
                P.op('dve', lambda e: e.tensor_tensor(out=lg[:, :], in0=plg[:, 0:32], in1=brs[:, :], op=ALU.add), [b_plg, b_brs], [b_lg])
                P.op('dve', lambda e: e.max(out=m8[:, :], in_=lg[:, :]), [b_lg], [b_m8])
                P.op('dve', lambda e: e.tensor_scalar(out=ex[:, :], in0=lg[:, :], scalar1=m8[:, 0:1], scalar2=None, op0=ALU.subtract),
                     [b_lg, b_m8], [b_ex])
                P.op('act', lambda e: e.activation(out=ex[:, :], in_=ex[:, :], func=AF.Exp), [b_ex], [b_ex])
                P.op('dve', lambda e: e.scalar_tensor_tensor(out=ex[:, :], in0=lg[:, :], scalar=m8[:, 3:4], in1=ex[:, :],
                                                             op0=ALU.is_ge, op1=ALU.mult), [b_lg, b_m8, b_ex], [b_ex])
                P.op('dve', lambda e: e.reduce_sum(out=m8[:, 4:5], in_=ex[:, :], axis=AX.X), [b_ex], [b_m8])
                P.op('dve', lambda e: e.reciprocal(out=m8[:, 4:5], in_=m8[:, 4:5]), [b_m8], [b_m8])
                P.op('dve', lambda e: e.tensor_scalar(out=G[:, sub, :], in0=ex[:, :], scalar1=m8[:, 4:5], scalar2=None, op0=ALU.mult),
                     [b_ex, b_m8], [b_G])
                if dbg is not None:
                    P.dma(dbg[t0 + sub * 128:t0 + (sub + 1) * 128, :], G[:, sub, :], reads=[b_G])
                P.op('pe', lambda e: e.transpose(out=plg[0:32, 128:256], in_=G[:, sub, :], identity=ident[:, :]), [b_G, b_ident], [b_plg])
                P.op('dve', lambda e: e.tensor_copy(out=GT[:, :], in_=plg[0:32, 128:256]), [b_plg], [b_GT])
                for ch in range(8):
                    pb_ = pbd[ch % 2]; bpb = b_pbd[ch % 2]
                    P.op('pe', lambda e: e.matmul(pb_[:, :], lhsT=GT[:, :], rhs=bds[:, ch * 512:(ch + 1) * 512], start=True, stop=True),
                         [b_GT, b_bds], [bpb])
                    P.op('dve', lambda e: e.scalar_tensor_tensor(out=acc[:, sub, ch * 512:(ch + 1) * 512], in0=acc[:, sub, ch * 512:(ch + 1) * 512],
                                                                 scalar=ALPHA, in1=pb_[:, :], op0=ALU.mult, op1=ALU.add),
                         [b_acc[sub], bpb], [b_acc[sub]])
            P.pop_scope()
            P.push_scope()
            NWB = 3
            wg = [P.sb(f"wg{i}", [128, 32, 256], BF16) for i in range(NWB)]; b_wg = [Buf() for _ in range(NWB)]
            wd = [P.sb(f"wd{i}", [128, 4, 1024], BF16) for i in range(NWB)]; b_wd = [Buf() for _ in range(NWB)]
            pg_ = [P.ps(f"pg{i}", [128, 512]) for i in range(4)]; b_pg = [Buf(excl=True) for _ in range(4)]
            pd_ = [P.ps(f"pd{i}", [128, 512]) for i in range(3)]; b_pd = [Buf(excl=True) for _ in range(3)]
            actT = [P.sb(f"actT{i}", [128, 4, 512], BF16) for i in range(2)]; b_act = [Buf(), Buf()]
            gl = [P.sb(f"gl{i}", [128, 512], F32) for i in range(2)]; b_gl = [Buf(), Buf()]
            sg_ = [P.sb(f"sg{i}", [128, 512], F32) for i in range(2)]; b_sg = [Buf(), Buf()]
            ln_ = [P.sb(f"ln{i}", [128, 512], F32) for i in range(2)]; b_ln = [Buf(), Buf()]
            ig = 0
            idn = 0
            iw = 0
            for ex_i in range(n_exp):
                a_ = actT[ex_i % 2]; ba = b_act[ex_i % 2]
                for f in range(4):
                    w_ = wg[ig % NWB]; bw = b_wg[ig % NWB]
                    P.dma(w_[:, :, :], wgu[ex_i, f].rearrange("p (k c) -> p k c", c=256), writes=[bw], eng='pool')
                    pgl = pg_[(ig % 2) * 2]; bpgl = b_pg[(ig % 2) * 2]
                    pln = pg_[(ig % 2) * 2 + 1]; bpln = b_pg[(ig % 2) * 2 + 1]
                    for k in range(32):
                        P.op('pe', lambda e: e.matmul(pgl[:, :], lhsT=w_[:, k, 0:128], rhs=x1T[:, k, :], start=(k == 0), stop=(k == 31)),
                             [bw, b_x1T], [bpgl])
                    for k in range(32):
                        P.op('pe', lambda e: e.matmul(pln[:, :], lhsT=w_[:, k, 128:256], rhs=x1T[:, k, :], start=(k == 0), stop=(k == 31)),
                             [bw, b_x1T], [bpln])
                    g_ = gl[ig % 2]; bg = b_gl[ig % 2]
                    s_ = sg_[ig % 2]; bs = b_sg[ig % 2]
                    l_ = ln_[ig % 2]; bl = b_ln[ig % 2]
                    bc = (ex_i * 4 + f) * 2
                    P.op('dve', lambda e: e.tensor_scalar(out=g_[:, :], in0=pgl[:, :], scalar1=bgs[:, bc:bc + 1], scalar2=7.0,
                                                          op0=ALU.add, op1=ALU.min), [bpgl, b_bgs], [bg])
                    P.op('act', lambda e: e.activation(out=s_[:, :], in_=g_[:, :], func=AF.Sigmoid, scale=1.702), [bg], [bs])
                    P.op('dve', lambda e: e.tensor_scalar(out=l_[:, :], in0=pln[:, :], scalar1=bgs[:, bc + 1:bc + 2], scalar2=7.0,
                                                          op0=ALU.add, op1=ALU.min), [bpln, b_bgs], [bl])
                    P.op('dve', lambda e: e.tensor_scalar(out=l_[:, :], in0=l_[:, :], scalar1=-7.0, scalar2=1.0,
                                                          op0=ALU.max, op1=ALU.add), [bl], [bl])
                    P.op('dve', lambda e: e.tensor_tensor(out=g_[:, :], in0=g_[:, :], in1=s_[:, :], op=ALU.mult), [bg, bs], [bg])
                    P.op('dve', lambda e: e.tensor_tensor(out=a_[:, f, :], in0=g_[:, :], in1=l_[:, :], op=ALU.mult), [bg, bl], [ba])
                    ig += 1
                for cc in range(4):
                    d_ = wd[iw % NWB]; bd = b_wd[iw % NWB]
                    P.dma(d_[:, :, :], wdn[ex_i, cc].rearrange("p (f c) -> p f c", c=1024), writes=[bd], eng='pool')
                    iw += 1
                    for sub in range(4):
                        for hf in range(2):
                            pp_ = pd_[idn % 3]; bpp = b_pd[idn % 3]
                            for ft in range(4):
                                P.op('pe', lambda e: e.matmul(pp_[:, :], lhsT=a_[:, ft, sub * 128:(sub + 1) * 128],
                                                              rhs=d_[:, ft, hf * 512:(hf + 1) * 512], start=(ft == 0), stop=(ft == 3)),
                                     [ba, bd], [bpp])
                            c0 = cc * 1024 + hf * 512
                            P.op('dve', lambda e: e.scalar_tensor_tensor(out=acc[:, sub, c0:c0 + 512], in0=pp_[:, :],
                                                                         scalar=G[:, sub, ex_i:ex_i + 1], in1=acc[:, sub, c0:c0 + 512],
                                                                         op0=ALU.mult, op1=ALU.add), [bpp, b_G, b_acc[sub]], [b_acc[sub]])
                            idn += 1
            P.pop_scope()
            P.push_scope()
            gb = P.sb("gb2", [128, 2, 4096], F32); b_gb = Buf()
            P.dma(gb[:, 0, :], lnp[2:3, :].to_broadcast([128, 4096]), writes=[b_gb])
            P.dma(gb[:, 1, :], lnp[3:4, :].to_broadcast([128, 4096]), writes=[b_gb])
            stat = P.sb("stat2", [128, 2], F32); b_stat = Buf()
            sq = P.sb("sq2", [128, 4096], BF16); b_sq = Buf()
            for sub in range(4):
                layer_norm(sub, 2, stat, b_stat, sq, b_sq, gb, b_gb)
                P.dma(y[t0 + sub * 128:t0 + (sub + 1) * 128, :], acc[:, sub, :], reads=[b_acc[sub]])
            P.pop_scope()
        P.emit()
    return nc


def prep_ffn_weights(w_out, ln1_g, ln1_b, ln2_g, ln2_b, w_router, b_router, w_gate_up, b_gate_up, w_down, b_down):
    wg = w_gate_up.reshape(32, 32, 128, 4, 128, 2)
    wg = np.ascontiguousarray(wg.transpose(0, 3, 2, 1, 5, 4)).reshape(32, 4, 128, 32 * 256)
    bg = b_gate_up.reshape(32, 4, 128, 2)
    bg = np.ascontiguousarray(bg.transpose(2, 0, 1, 3)).reshape(128, 256)
    wd = w_down.reshape(32, 4, 128, 4, 1024)
    wd = np.ascontiguousarray(wd.transpose(0, 3, 2, 1, 4)).reshape(32, 4, 128, 4 * 1024)
    return dict(wout=np.ascontiguousarray(w_out), lnp=np.ascontiguousarray(np.stack([ln1_g, ln1_b, ln2_g, ln2_b])),
                wr=np.ascontiguousarray(w_router), br=np.ascontiguousarray(b_router.reshape(1, 32)),
                wgu=wg, bgu=bg, wdn=wd, bdn=np.ascontiguousarray(b_down))


_COLS = [2048, 512, 512, 512, 512, 512, 512, 48, 1024, 1024, 1024, 1024, 1024, 1024]
_OFF = [0]
for _c in _COLS:
    _OFF.append(_OFF[-1] + _c)


def mixer_cols(j):
    r = lambda seg, a, n: list(range(_OFF[seg] + a, _OFF[seg] + a + n))
    cols = []
    cols += r(0, 4 * j * 128, 512)
    for seg in (1, 2, 3, 5):
        cols += r(seg, j * 128, 128)
    cols += r(4, j * 128, 128)
    cols += r(6, j * 128, 128)
    cols += r(7, j * 12, 12)
    cols += r(8, 2 * j * 128, 256)
    cols += r(9, 2 * j * 128, 256)
    cols += r(10, j * 256, 256)
    cols += r(11, 2 * j * 128, 256)
    cols += r(12, 2 * j * 128, 256)
    cols += r(13, 2 * j * 128, 256)
    return np.array(cols)


def mixer_consts(layer):
    import math
    invf = (1.0 / (500000.0 ** (np.arange(0, 32, 2, dtype=np.float32) / 32))).astype(np.float32)
    c_invf = np.concatenate([invf, invf]).reshape(32, 1).astype(np.float32)
    sw = np.zeros((128, 32), np.float32)
    for m in range(16):
        sw[m + 16, m] = -1.0
        sw[m, m + 16] = 1.0
    li = 0.8 - 0.6 * math.exp(-0.3 * layer)
    c_lam = np.array([[li, 1.0 - li]], np.float32)
    fb = np.zeros((128, 256), np.float32)
    for p in range(128):
        cr = p // 64
        for c in range(256):
            rel = c - 128
            if rel > cr:
                fb[p, c] = -1e30
            elif rel == cr or rel == cr - 1:
                fb[p, c] = 100.0
    return dict(c_invf=c_invf, c_sw=sw, c_lam=c_lam, c_fbig=fb)


_PROGS = {}


def _get_prog(name):
    if name not in _PROGS:
        _PROGS[name] = build_mixer() if name == 'mixer' else build_ffn()
    return _PROGS[name]


def kernel(x, positions, w_in, nsa_cmp_pos, nsa_cmp_w1, nsa_cmp_w2, diff_lambda, diff_subln_g,
           w_out, ln1_g, ln1_b, w_router, b_router, w_gate_up, b_gate_up, w_down, b_down, ln2_g, ln2_b):
    x = np.asarray(x, np.float32)
    positions = np.asarray(positions, np.int32)
    B, S, D = x.shape
    cores = list(range(8))
    for layer in range(2):
        ncA = _get_prog('mixer')
        consts = mixer_consts(layer)
        xTs = [np.ascontiguousarray(x[b].T) for b in range(B)]
        wAs = [np.ascontiguousarray(np.asarray(w_in[layer])[:, mixer_cols(j)]) for j in range(4)]
        in_maps = []
        for c in cores:
            b, j = c // 4, c % 4
            in_maps.append(dict(xT=xTs[b], wA=wAs[j], pos=np.ascontiguousarray(positions[b:b + 1]),
                                cpos=np.ascontiguousarray(nsa_cmp_pos[layer]), cw1=np.ascontiguousarray(nsa_cmp_w1[layer]),
                                cw2=np.ascontiguousarray(nsa_cmp_w2[layer]),
                                dlam=np.ascontiguousarray(np.asarray(diff_lambda[layer]).reshape(1, 512)),
                                subg=np.ascontiguousarray(np.asarray(diff_subln_g[layer]).reshape(1, 256)), **consts))
        resA = run_bass_kernel_spmd(ncA, in_maps, core_ids=cores)
        del xTs, wAs, in_maps
        mix_full = np.empty((B, S, 4096), np.float32)
        for c in cores:
            b, j = c // 4, c % 4
            m = resA.results[c]["mix"]
            mix_full[b, :, j * 512:(j + 1) * 512] = m[:, 0:512]
            mix_full[b, :, 2048 + j * 256:2048 + (j + 1) * 256] = m[:, 512:768]
            mix_full[b, :, 3072 + j * 256:3072 + (j + 1) * 256] = m[:, 768:1024]
        ncB = _get_prog('ffn')
        W = prep_ffn_weights(np.asarray(w_out[layer]), np.asarray(ln1_g[layer]), np.asarray(ln1_b[layer]), np.asarray(ln2_g[layer]),
                             np.asarray(ln2_b[layer]), np.asarray(w_router[layer]), np.asarray(b_router[layer]),
                             np.asarray(w_gate_up[layer]), np.asarray(b_gate_up[layer]), np.asarray(w_down[layer]),
                             np.asarray(b_down[layer]))
        in_maps = []
        for c in cores:
            b, s0 = c // 4, (c % 4) * TOK
            in_maps.append(dict(xres=np.ascontiguousarray(x[b, s0:s0 + TOK]), mixT=np.ascontiguousarray(mix_full[b, s0:s0 + TOK].T), **W))
        resB = run_bass_kernel_spmd(ncB, in_maps, core_ids=cores)
        del in_maps, W, mix_full
        xn = np.empty_like(x)
        for c in cores:
            b, s0 = c // 4, (c % 4) * TOK
            xn[b, s0:s0 + TOK] = resB.results[c]["y"]
        x = xn
    return x
```
